# Optimizing a Trainium2 kernel written in Bass

```python
import math
import jax, jax.numpy as jnp
from jax import lax
import numpy as np

D_MODEL = 2048
BATCH = 8
SEQ = 4096
DEPTH = 4

M_HEADS = 4
M_DQK = 128
M_DV = 256
M_CHUNK = 64
M_CONV = 4
M_QK_W = 2 * M_HEADS * M_DQK
M_V_W = M_HEADS * M_DV
A_HEADS = 8
A_DH = 128
A_DV = 2 * A_DH
A_Q_W = A_HEADS * 2 * A_DH
A_V_W = A_HEADS * A_DV
Q_BLOCK = 128
ROPE_THETA = 500000.0
ROPE_DIM = A_DH // 4
D_FF = -(-8 * D_MODEL // (3 * 256)) * 256
EPS = 1e-6
COL_SIZES = (M_QK_W, M_V_W, M_V_W, M_HEADS, M_HEADS, A_Q_W, A_Q_W, A_V_W, D_MODEL, D_MODEL)
D_IN = sum(COL_SIZES)

kernel_name = 'hybrid_mlstm_diffattn_block'


def rms_norm(x, g):
    xf = x.astype(jnp.float32)
    y = xf * lax.rsqrt(jnp.mean(xf * xf, axis=-1, keepdims=True) + EPS)
    return (y * g.astype(jnp.float32)).astype(x.dtype)


def split_cols(t, sizes):
    out, start = [], 0
    for s in sizes:
        out.append(t[..., start:start + s])
        start += s
    return out


def causal_dwconv(x, w, b):
    y = lax.conv_general_dilated(x, w[:, None, :].astype(x.dtype), window_strides=(1,),
                                 padding=((M_CONV - 1, 0),),
                                 dimension_numbers=('NWC', 'WIO', 'NWC'),
                                 feature_group_count=x.shape[-1])
    return y + b.astype(x.dtype)


def partial_rope(x, positions):
    half = ROPE_DIM // 2
    inv = jnp.power(ROPE_THETA, -jnp.arange(half, dtype=jnp.float32) * (2.0 / ROPE_DIM))
    ang = positions.astype(jnp.float32)[..., None] * inv
    cos = jnp.cos(ang)[:, :, None, None, :]
    sin = jnp.sin(ang)[:, :, None, None, :]
    xf = x.astype(jnp.float32)
    x1, x2, xp = xf[..., :half], xf[..., half:ROPE_DIM], xf[..., ROPE_DIM:]
    out = jnp.concatenate([x1 * cos - x2 * sin, x2 * cos + x1 * sin, xp], axis=-1)
    return out.astype(x.dtype)


def mlstm_chunkwise(q, k, v, log_i, log_f):
    B, S, H, dk = q.shape
    dv = v.shape[-1]
    L = M_CHUNK
    nc = S // L

    def to_chunks(t):
        t = t.reshape((B, nc, L, H) + t.shape[3:])
        return jnp.moveaxis(t, (1, 3), (0, 2))

    causal = jnp.tril(jnp.ones((L, L), dtype=bool))

    def step(carry, xs):
        C, n, m = carry
        qc, kc, vc, lic, lfc = xs
        b = jnp.cumsum(lfc, axis=-1)
        dmat = jnp.where(causal, b[..., :, None] - b[..., None, :] + lic[..., None, :], -jnp.inf)
        m_inter = b + m[..., None]
        m_t = jnp.maximum(jnp.max(dmat, axis=-1), m_inter)
        w_intra = jnp.exp(dmat - m_t[..., None])
        w_inter = jnp.exp(m_inter - m_t)
        s = jnp.einsum('bhtd,bhsd->bhts', qc, kc) * w_intra
        num = jnp.einsum('bhts,bhsv->bhtv', s, vc) + w_inter[..., None] * jnp.einsum('bhtd,bhdv->bhtv', qc, C)
        den = jnp.sum(s, axis=-1) + w_inter * jnp.einsum('bhtd,bhd->bht', qc, n)
        h = num / jnp.maximum(jnp.abs(den), jnp.exp(-m_t))[..., None]
        g = b[..., -1]
        a = g[..., None] - b + lic
        m_new = jnp.maximum(g + m, jnp.max(a, axis=-1))
        decay = jnp.exp(g + m - m_new)
        w_s = jnp.exp(a - m_new[..., None])
        C = decay[..., None, None] * C + jnp.einsum('bhs,bhsd,bhsv->bhdv', w_s, kc, vc)
        n = decay[..., None] * n + jnp.einsum('bhs,bhsd->bhd', w_s, kc)
        return (C, n, m_new), h

    init = (jnp.zeros((B, H, dk, dv), jnp.float32), jnp.zeros((B, H, dk), jnp.float32),
            jnp.zeros((B, H), jnp.float32))
    xs = (to_chunks(q), to_chunks(k), to_chunks(v), to_chunks(log_i), to_chunks(log_f))
    _, h = lax.scan(step, init, xs)
    return jnp.moveaxis(h, (0, 2), (1, 3)).reshape(B, S, H, dv)


def diff_attention(q, k, v, lam, lam_init, g_sub):
    B, S, H, _, dh = q.shape
    dv = v.shape[-1]
    nq = S // Q_BLOCK
    scale = dh ** -0.5
    qh = jnp.moveaxis(q, 1, 3)
    kh = jnp.moveaxis(k, 1, 3)
    vh = jnp.moveaxis(v, 1, 2)
    qb = jnp.moveaxis(qh.reshape(B, H, 2, nq, Q_BLOCK, dh), 3, 0)
    kpos = jnp.arange(S)

    def block(args):
        qblk, i = args
        s = jnp.einsum('bhcqd,bhckd->bhcqk', qblk, kh).astype(jnp.float32) * scale
        qpos = i * Q_BLOCK + jnp.arange(Q_BLOCK)
        mask = kpos[None, :] <= qpos[:, None]
        p = jax.nn.softmax(jnp.where(mask, s, -jnp.inf), axis=-1)
        attn = p[:, :, 0] - lam * p[:, :, 1]
        return jnp.einsum('bhqk,bhkv->bhqv', attn.astype(vh.dtype), vh)

    o = lax.map(block, (qb, jnp.arange(nq)))
    o = jnp.moveaxis(o, 0, 2).reshape(B, H, S, dv)
    o = rms_norm(o, g_sub) * (1.0 - lam_init)
    return jnp.moveaxis(o, 1, 2).reshape(B, S, H * dv)


def setup_inputs(seed: int = 0) -> dict:
    key = jax.random.key(seed)
    ks = jax.random.split(key, 24)
    nrm = lambda k, shape, s: jax.random.normal(k, shape, jnp.float32) * s
    x = jax.random.normal(ks[0], (BATCH, SEQ, D_MODEL), jnp.float32)
    positions = jnp.broadcast_to(jnp.arange(SEQ, dtype=jnp.int32), (BATCH, SEQ))
    return {
        'x': x,
        'positions': positions,
        'g_mix': 1.0 + nrm(ks[1], (DEPTH, D_MODEL), 0.01),
        'w_in': nrm(ks[2], (DEPTH, D_MODEL, D_IN), D_MODEL ** -0.5),
        'conv_w': nrm(ks[3], (DEPTH, M_CONV, M_QK_W), M_CONV ** -0.5),
        'conv_b': nrm(ks[4], (DEPTH, M_QK_W), 0.01),
        'i_bias': nrm(ks[5], (DEPTH, M_HEADS), 0.1),
        'f_bias': jnp.linspace(3.0, 6.0, M_HEADS, dtype=jnp.float32)[None, :] + nrm(ks[6], (DEPTH, M_HEADS), 0.1),
        'g_mhead': 1.0 + nrm(ks[7], (DEPTH, M_V_W), 0.01),
        'lambda_q1': nrm(ks[8], (DEPTH, A_DH), 0.1),
        'lambda_k1': nrm(ks[9], (DEPTH, A_DH), 0.1),
        'lambda_q2': nrm(ks[10], (DEPTH, A_DH), 0.1),
        'lambda_k2': nrm(ks[11], (DEPTH, A_DH), 0.1),
        'g_sub': 1.0 + nrm(ks[12], (DEPTH, A_DV), 0.01),
        'p_m': nrm(ks[13], (DEPTH, M_V_W, D_MODEL), M_V_W ** -0.5),
        'p_a': nrm(ks[14], (DEPTH, A_V_W, D_MODEL), A_V_W ** -0.5),
        'w_out': nrm(ks[15], (DEPTH, D_MODEL, D_MODEL), D_MODEL ** -0.5),
        'g_ffn': 1.0 + nrm(ks[16], (DEPTH, D_MODEL), 0.01),
        'w_gate': nrm(ks[17], (DEPTH, D_MODEL, D_FF), D_MODEL ** -0.5),
        'w_up': nrm(ks[18], (DEPTH, D_MODEL, D_FF), D_MODEL ** -0.5),
        'w_down': nrm(ks[19], (DEPTH, D_FF, D_MODEL), D_FF ** -0.5),
        'g_final': 1.0 + nrm(ks[20], (D_MODEL,), 0.01),
    }


def reference(x, positions, g_mix, w_in, conv_w, conv_b, i_bias, f_bias, g_mhead,
              lambda_q1, lambda_k1, lambda_q2, lambda_k2, g_sub, p_m, p_a, w_out,
              g_ffn, w_gate, w_up, w_down, g_final):
    B, S, _ = x.shape
    for l in range(DEPTH):
        lam_init = 0.8 - 0.6 * math.exp(-0.3 * l)
        h = rms_norm(x, g_mix[l])
        proj = h @ w_in[l]
        m_qk, m_v, m_o, m_i, m_f, a_q, a_k, a_v, gate_m, gate_a = split_cols(proj, COL_SIZES)

        m_qk = jax.nn.silu(causal_dwconv(m_qk, conv_w[l], conv_b[l]))
        m_q = m_qk[..., :M_QK_W // 2].reshape(B, S, M_HEADS, M_DQK).astype(jnp.float32)
        m_k = m_qk[..., M_QK_W // 2:].reshape(B, S, M_HEADS, M_DQK).astype(jnp.float32) * (M_DQK ** -0.5)
        m_vv = m_v.reshape(B, S, M_HEADS, M_DV).astype(jnp.float32)
        log_i = (m_i + i_bias[l]).astype(jnp.float32)
        log_f = jax.nn.log_sigmoid((m_f + f_bias[l]).astype(jnp.float32))
        h_m = mlstm_chunkwise(m_q, m_k, m_vv, log_i, log_f)
        h_m = rms_norm(h_m, g_mhead[l].reshape(M_HEADS, M_DV)).reshape(B, S, M_V_W).astype(x.dtype)
        h_m = jax.nn.sigmoid(m_o) * h_m

        q = partial_rope(a_q.reshape(B, S, A_HEADS, 2, A_DH), positions)
        k = partial_rope(a_k.reshape(B, S, A_HEADS, 2, A_DH), positions)
        lam = (jnp.exp(jnp.sum(lambda_q1[l].astype(jnp.float32) * lambda_k1[l].astype(jnp.float32)))
               - jnp.exp(jnp.sum(lambda_q2[l].astype(jnp.float32) * lambda_k2[l].astype(jnp.float32)))
               + lam_init)
        h_a = diff_attention(q, k, a_v.reshape(B, S, A_HEADS, A_DV), lam, lam_init, g_sub[l])

        y = jax.nn.sigmoid(gate_m) * (h_m @ p_m[l]) + jax.nn.sigmoid(gate_a) * (h_a @ p_a[l])
        x = x + y @ w_out[l]

        h = rms_norm(x, g_ffn[l])
        x = x + (jax.nn.silu(h @ w_gate[l]) * (h @ w_up[l])) @ w_down[l]
    return rms_norm(x, g_final)
```

```python
import math
from contextlib import ExitStack

import numpy as np
import ml_dtypes

import concourse.bass as bass
import concourse.mybir as mybir
from concourse.bass_utils import run_bass_kernel_spmd

F32 = mybir.dt.float32
BF16 = mybir.dt.bfloat16
I32 = mybir.dt.int32
U8 = mybir.dt.uint8
AF = mybir.ActivationFunctionType
ALU = mybir.AluOpType

D = 2048
KCD = 16
DFF = 5632
KCF = 44
DEPTH = 4
SEQ = 4096
EPS = 1e-6
NSMALL = 96
ROPE_THETA = 500000.0


class Op:
    __slots__ = ("eng", "fn", "deps", "needed", "sigval", "dma", "sem", "val", "slot")


class OpSet:
    __slots__ = ("d",)

    def __init__(self):
        self.d = {}

    def add(self, o):
        self.d[(o.eng, o.slot) if o.dma else o.eng] = o

    def ops(self):
        return self.d.values()

    def __bool__(self):
        return bool(self.d)


class Res:
    __slots__ = ("ws", "rs", "prs", "name")

    def __init__(self, name=""):
        self.ws = OpSet()
        self.rs = OpSet()
        self.prs = OpSet()
        self.name = name


class Sched:
    ENGS = ("pe", "act", "dve", "pool", "sp")
    QUEUES = ("sp", "pool", "act")

    def __init__(self, nc, es, K=4):
        self.nc = nc
        self.K = K
        self.streams = {e: [] for e in self.ENGS}
        self.engsem = {e: es.enter_context(nc.semaphore("sem_" + e)) for e in self.ENGS}
        self.ring = {q: [es.enter_context(nc.semaphore("ring_%s_%d" % (q, i))) for i in range(K)] for q in self.QUEUES}
        self.ndma = {q: 0 for q in self.QUEUES}
        self.ringlast = {q: [None] * K for q in self.QUEUES}
        self.lastc = {e: None for e in self.ENGS}
        self.barrier = {}

    def op(self, eng, fn, reads=(), writes=(), pwrites=(), dma=False):
        o = Op()
        o.eng = eng
        o.fn = fn
        o.dma = dma
        o.needed = False
        o.sigval = None
        o.slot = None
        o.sem = None
        o.val = None
        deps = set()
        if eng in self.barrier:
            deps |= self.barrier.pop(eng)
        if dma:
            n = self.ndma[eng]
            self.ndma[eng] = n + 1
            slot = n % self.K
            o.slot = slot
            o.sem = self.ring[eng][slot]
            o.val = 16 * (n // self.K + 1)
            prev = self.ringlast[eng][slot]
            if prev is not None:
                deps.add(prev)
            self.ringlast[eng][slot] = o
        for r in reads:
            deps.update(r.ws.ops())
        for w in writes:
            deps.update(w.ws.ops())
            deps.update(w.rs.ops())
            if not w.rs:
                deps.update(w.prs.ops())
        for w in pwrites:
            if w.rs:
                deps.update(w.rs.ops())
            else:
                deps.update(w.prs.ops())
        if eng == "pe":
            deps = {d for d in deps if d.dma or d.eng != "pe"}
        deps.discard(o)
        for d in deps:
            if not d.dma:
                d.needed = True
        o.deps = deps
        for r in reads:
            r.rs.add(o)
        for w in writes:
            if w.rs:
                w.prs = w.rs
                w.rs = OpSet()
            w.ws = OpSet()
            w.ws.add(o)
        for w in pwrites:
            if w.rs:
                w.prs = w.rs
                w.rs = OpSet()
                w.ws = OpSet()
            w.ws.add(o)
        self.streams[eng].append(o)
        if not dma:
            self.lastc[eng] = o
        return o

    def barrier_all(self):
        b = set()
        for e, o in self.lastc.items():
            if o is not None:
                b.add(o)
        for q in self.QUEUES:
            for o in self.ringlast[q]:
                if o is not None:
                    b.add(o)
        for e in self.ENGS:
            self.barrier[e] = set(b) | self.barrier.get(e, set())

    def emit(self, block):
        nc = self.nc
        for e, st in self.streams.items():
            c = 0
            for o in st:
                if (not o.dma) and o.needed:
                    c += 1
                    o.sigval = c
        engmap = {"pe": block.tensor, "act": block.scalar, "dve": block.vector, "pool": block.gpsimd, "sp": block.sync}
        nceng = {"pe": nc.tensor, "act": nc.scalar, "dve": nc.vector, "pool": nc.gpsimd, "sp": nc.sync}
        finals = []
        for q in self.QUEUES:
            for o in self.ringlast[q]:
                if o is not None:
                    finals.append((o.sem, o.val))
        engsem = self.engsem

        def mk(e, st):
            def body(_e):
                eng = nceng[e]
                known = {}
                for o in st:
                    waits = {}
                    for d in o.deps:
                        if d.dma:
                            sem, v = d.sem, d.val
                        else:
                            sem, v = engsem[d.eng], d.sigval
                        k = id(sem)
                        if k not in waits or waits[k][1] < v:
                            waits[k] = (sem, v)
                    for k, (sem, v) in waits.items():
                        if known.get(k, 0) < v:
                            eng.wait_ge(sem, v)
                            known[k] = v
                    ins = o.fn()
                    if o.dma:
                        ins.then_inc(o.sem, 16)
                    elif o.needed:
                        ins.then_inc(engsem[e], 1)
                if e == "sp":
                    for sem, v in finals:
                        eng.wait_ge(sem, v)
            return body

        for e, st in self.streams.items():
            engmap[e](mk(e, st))


class Carver:
    def __init__(self, base, size, start=0):
        self.base = base
        self.size = size
        self.start = start
        self.off = start
        self.gen = 0

    def reset(self):
        self.off = self.start
        self.gen += 1

    def alloc(self, n, dt):
        nb = {F32: 4, BF16: 2, I32: 4, U8: 1}[dt]
        off = (self.off + 63) // 64 * 64
        assert off + n * nb <= self.size, ("SBUF overflow", off, n * nb, self.size)
        self.off = off + n * nb
        return self.base[:, off:off + n * nb].bitcast(dt)


def build(S, depth, debug=False, upto=None):
    nc = bass.Bass("TRN2", target_bir_lowering=False)
    TB = S // 128
    TC = S // 512
    dbg_outs = []

    def din(name, shape, dt):
        return nc.dram_tensor(name, list(shape), dt, kind="ExternalInput").ap()

    def dscr(name, shape, dt):
        if debug:
            dbg_outs.append(name)
            return nc.dram_tensor(name, list(shape), dt, kind="ExternalOutput").ap()
        return nc.dram_tensor(name, list(shape), dt, kind="Internal").ap()

    x_in = din("x", [S, D], F32)
    pos_in = din("pos", [1, S], I32)
    small_in = din("small", [depth, 128, NSMALL], F32)
    gfin_in = din("gfin", [128, D], F32)
    cf_in = din("cf", [128, 1024], F32)
    cb_in = din("cb", [128, 512], BF16)
    wfm_in = din("wfm", [depth, 80, 128, KCD, 128], F32)
    wg_in = din("wg", [depth, 1, 128, KCD, 128], F32)
    wtm_in = din("wtm", [depth, 6, 128, KCD, 512], F32)
    pm_in = din("pm", [depth, 16, 128, 8, 128], F32)
    pa_in = din("pa", [depth, 16, 128, 16, 128], F32)
    wo_in = din("wo", [depth, 4, 128, KCD, 512], F32)
    wga_in = din("wga", [depth, 44, 128, KCD, 128], F32)
    wup_in = din("wup", [depth, 44, 128, KCD, 128], F32)
    wdn_in = din("wdn", [depth, 8, 128, KCF, 256], F32)
    out_d = nc.dram_tensor("out", [S, D], F32, kind="ExternalOutput").ap()

    hT_d = dscr("hT", [KCD, 128, S], BF16)
    mqk_d = dscr("mqk", [8, 128, S], F32)
    sgo_d = dscr("sgo", [8, 128, S], F32)
    gi_d = dscr("gi", [4, S], F32)
    gf_d = dscr("gf", [4, S], F32)
    aq_d = dscr("aq", [16, 128, S], BF16)
    ak_d = dscr("ak", [16, 128, S], BF16)
    sgm_d = dscr("sgm", [16, 128, S], BF16)
    sga_d = dscr("sga", [16, 128, S], BF16)
    mv_d = dscr("mv", [S, 1024], BF16)
    av_d = dscr("av", [S, 2048], BF16)
    qkt_d = dscr("qkt", [8, 128, S], BF16)
    egb_d = dscr("egb", [128, 4 * TB], F32)
    hraw_d = dscr("hraw", [8, 128, S], F32)
    hmT_d = dscr("hmT", [8, 128, S], BF16)
    haT_d = dscr("haT", [16, 128, S], BF16)
    yT_d = dscr("yT", [16, 128, S], BF16)
    actT_d = dscr("actT", [KCF, 128, S], BF16)
    xa_d = dscr("xa", [S, D], F32)
    xb_d = dscr("xb", [S, D], F32)
    cs_d = dscr("cs", [2, 32, S], F32)

    R = {}
    for n in ("hT", "mqk", "sgo", "gi", "gf", "aq", "ak", "sgm", "sga", "mv", "av", "qkt", "egb", "hraw", "hmT", "haT",
              "yT", "actT", "xa", "xb", "cs", "out", "xin"):
        R[n] = Res(n)

    es = ExitStack()
    with es:
        sb = es.enter_context(nc.sbuf_tensor("sb", [128, 196608], U8))
        pst = es.enter_context(nc.psum_tensor("ps", [128, 16384], U8))
        sc = Sched(nc, es, K=4)

        def bank(i, dt=F32):
            return pst[:, i * 2048:(i + 1) * 2048].bitcast(dt)

        PSR = [Res("ps%d" % i) for i in range(8)]

        PERS = 12288
        pc = Carver(sb, PERS, 0)
        wk = Carver(sb, 196608, PERS)

        def DMA(q, out, in_, reads=(), writes=(), pwrites=()):
            eng = {"sp": nc.sync, "pool": nc.gpsimd, "act": nc.scalar}[q]
            n = out.shape[-1]
            if n > 2048 and tuple(out.shape) == tuple(in_.shape):
                r = None
                for c0 in range(0, n, 2048):
                    c1 = min(n, c0 + 2048)
                    idx = tuple([slice(None)] * (len(out.shape) - 1) + [slice(c0, c1)])
                    o_, i_ = out[idx], in_[idx]
                    r = sc.op(q, lambda o_=o_, i_=i_: eng.dma_start(out=o_, in_=i_), reads, writes, pwrites, dma=True)
                return r
            return sc.op(q, lambda: eng.dma_start(out=out, in_=in_), reads, writes, pwrites, dma=True)

        def PE(fn, reads=(), writes=()):
            return sc.op("pe", fn, reads, writes)

        def MM(out, lhsT, rhs, start, stop, reads=(), writes=()):
            return sc.op("pe", lambda: nc.tensor.matmul(out, lhsT, rhs, start=start, stop=stop), reads, writes)

        def ACT(out, in_, func, reads=(), writes=(), bias=None, scale=None, accum_out=None):
            kw = {}
            if bias is not None:
                kw["bias"] = bias
            if scale is not None:
                kw["scale"] = scale
            if accum_out is not None:
                kw["accum_out"] = accum_out
            return sc.op("act", lambda: nc.scalar.activation(out=out, in_=in_, func=func, **kw), reads, writes)

        def TS(eng, out, in0, s1, s2, op0, op1=None, reads=(), writes=()):
            e = {"dve": nc.vector, "pool": nc.gpsimd}[eng]
            if op1 is None:
                return sc.op(eng, lambda: e.tensor_scalar(out=out, in0=in0, scalar1=s1, scalar2=None, op0=op0), reads, writes)
            return sc.op(eng, lambda: e.tensor_scalar(out=out, in0=in0, scalar1=s1, scalar2=s2, op0=op0, op1=op1), reads, writes)

        def TT(eng, out, in0, in1, op, reads=(), writes=()):
            e = {"dve": nc.vector, "pool": nc.gpsimd}[eng]
            return sc.op(eng, lambda: e.tensor_tensor(out=out, in0=in0, in1=in1, op=op), reads, writes)

        def STT(out, in0, scalar, in1, op0, op1, reads=(), writes=()):
            return sc.op("dve", lambda: nc.vector.scalar_tensor_tensor(out=out, in0=in0, scalar=scalar, in1=in1, op0=op0, op1=op1), reads, writes)

        def COPY(eng, out, in_, reads=(), writes=()):
            if eng == "act":
                return sc.op("act", lambda: nc.scalar.copy(out=out, in_=in_), reads, writes)
            e = {"dve": nc.vector, "pool": nc.gpsimd}[eng]
            return sc.op(eng, lambda: e.tensor_copy(out=out, in_=in_), reads, writes)

        cf = pc.alloc(1024, F32)
        cbt = pc.alloc(512, BF16)
        small = pc.alloc(NSMALL, F32)
        lamt = pc.alloc(8, F32)
        Rcf, Rcb, Rsmall, Rlam = Res("cf"), Res("cb"), Res("small"), Res("lam")
        DMA("sp", cf, cf_in, writes=[Rcf])
        DMA("sp", cbt, cb_in, writes=[Rcb])
        ones_f = cf[:, 0:128]
        sel_f = cf[:, 128:640].rearrange("p (h c) -> p h c", c=128)
        invf = cf[0:32, 640:641]
        mhalf = cf[:, 641:642]
        c025 = cf[0:32, 642:643]
        ones4 = cf[0:4, 768:896]
        ident_b = cbt[:, 0:128]
        ones_b = cbt[:, 128:256]
        tri_b = cbt[:, 256:384]
        rt_b = cbt[:, 384:512]

        wk.reset()
        posi = wk.alloc(S, I32)
        ang = wk.alloc(S, F32)
        t1 = wk.alloc(S, F32)
        t2 = wk.alloc(S, F32)
        ti = wk.alloc(S, I32)
        Rp, Ra, Rt1, Rt2, Rti = Res(), Res(), Res(), Res(), Res()
        DMA("sp", posi[0:32, :], pos_in.partition_broadcast(32), writes=[Rp])
        COPY("dve", ang[0:32, :], posi[0:32, :], reads=[Rp], writes=[Ra])
        TS("dve", ang[0:32, :], ang[0:32, :], invf, None, ALU.mult, reads=[Ra, Rcf], writes=[Ra])
        for which in (0, 1):
            if which == 0:
                TS("dve", t1[0:32, :], ang[0:32, :], 0.25, None, ALU.add, reads=[Ra], writes=[Rt1])
            else:
                COPY("dve", t1[0:32, :], ang[0:32, :], reads=[Ra], writes=[Rt1])
            COPY("dve", ti[0:32, :], t1[0:32, :], reads=[Rt1], writes=[Rti])
            COPY("dve", t2[0:32, :], ti[0:32, :], reads=[Rti], writes=[Rt2])
            TT("dve", t1[0:32, :], t1[0:32, :], t2[0:32, :], ALU.subtract, reads=[Rt1, Rt2], writes=[Rt1])
            TS("dve", t2[0:32, :], t1[0:32, :], 0.5, None, ALU.is_gt, reads=[Rt1], writes=[Rt2])
            TT("dve", t1[0:32, :], t1[0:32, :], t2[0:32, :], ALU.subtract, reads=[Rt1, Rt2], writes=[Rt1])
            TS("dve", t2[0:32, :], t1[0:32, :], -0.5, None, ALU.is_lt, reads=[Rt1], writes=[Rt2])
            TT("dve", t1[0:32, :], t1[0:32, :], t2[0:32, :], ALU.add, reads=[Rt1, Rt2], writes=[Rt1])
            ACT(t2[0:32, :], t1[0:32, :], AF.Sin, reads=[Rt1], writes=[Rt2], scale=6.28318)
            if which == 1:
                TS("dve", t2[0:16, :], t2[0:16, :], -1.0, None, ALU.mult, reads=[Rt2], writes=[Rt2])
            DMA("sp", cs_d[which], t2[0:32, :], reads=[Rt2], pwrites=[R["cs"]])
        sc.barrier_all()

        def load_small(l):
            DMA("sp", small, small_in[l], writes=[Rsmall])

        SM = dict(gmix=0, gffn=16, convw=32, convb=64, gmh=72, lam=80, gsub=84, ib=86, fb=87, nfb=88)

        def norm_phase(x_d, Rx, goff):
            wk.reset()
            xt = [wk.alloc(D, F32) for _ in range(2)]
            junk = wk.alloc(D, BF16)
            hb = [wk.alloc(D, BF16) for _ in range(2)]
            hs = [wk.alloc(KCD * 512, BF16).rearrange("p (c t) -> p c t", t=512) for _ in range(2)]
            st = wk.alloc(4 * TB, F32)
            Rxt = [Res(), Res()]
            Rjunk = Res()
            Rhb = [Res(), Res()]
            Rhs = [Res(), Res()]
            Rst = [Res(), Res()]
            hTv = hT_d.rearrange("c p s -> p c s")
            for tb in range(TB):
                s2 = tb % 2
                DMA("sp", xt[s2], x_d[tb * 128:(tb + 1) * 128, :], reads=[Rx], writes=[Rxt[s2]])
                ss = st[:, 4 * tb:4 * tb + 1]
                ms = st[:, 4 * tb + 1:4 * tb + 2]
                rstd = st[:, 4 * tb + 2:4 * tb + 3]
                ACT(junk, xt[s2], AF.Square, reads=[Rxt[s2]], writes=[Rst[s2]], accum_out=ss)
                TS("dve", ms, ss, 1.0 / D, EPS, ALU.mult, ALU.add, reads=[Rst[s2]], writes=[Rst[s2]])
                ACT(ms, ms, AF.Ln, reads=[Rst[s2]], writes=[Rst[s2]])
                ACT(rstd, ms, AF.Exp, reads=[Rst[s2]], writes=[Rst[s2]], scale=-0.5)
                TS("dve", hb[s2], xt[s2], rstd, None, ALU.mult, reads=[Rxt[s2], Rst[s2]], writes=[Rhb[s2]])
                stg = (tb // 4) % 2
                for half in range(2):
                    bi = (2 * tb + half) % 4
                    pt = bank(bi, BF16).rearrange("p (c t) -> p c t", t=128)
                    for j in range(8):
                        kc = half * 8 + j
                        PE(lambda o=pt[:, j, :], i=hb[s2][:, kc * 128:(kc + 1) * 128]: nc.tensor.transpose(o, i, ident_b),
                           reads=[Rhb[s2], Rcb], writes=[PSR[bi]])
                    for j in range(8):
                        kc = half * 8 + j
                        dst = hs[stg][:, kc, (tb % 4) * 128:(tb % 4 + 1) * 128]
                        gcol = small[:, goff + kc:goff + kc + 1]
                        if j % 2 == 0:
                            ACT(dst, pt[:, j, :], AF.Copy, reads=[PSR[bi], Rsmall], writes=[Rhs[stg]], scale=gcol)
                        else:
                            TS("dve", dst, pt[:, j, :], gcol, None, ALU.mult, reads=[PSR[bi], Rsmall], writes=[Rhs[stg]])
                if tb % 4 == 3:
                    tc = tb // 4
                    DMA("sp", hTv[:, :, tc * 512:(tc + 1) * 512], hs[stg], reads=[Rhs[stg]], pwrites=[R["hT"]])

        gemm_cache = {}

        def gemm_fm(groups, slabs, M, epi, out_fn, out_dt, STILE, wbufs=3):
            wk_mark = wk.off
            ST = min(STILE, S)
            NT = S // ST
            sig = ("fm", tuple((id(g[0]), g[2]) for g in groups), M, ST, wbufs)
            ck = (wk.gen, wk_mark)
            if ck in gemm_cache and gemm_cache[ck][0] == sig:
                _, inT, wsb, stage_raw = gemm_cache[ck]
            else:
                if ck in gemm_cache:
                    sc.barrier_all()
                inT = []
                shared = {}
                for (ind, Rin, KC, wfn) in groups:
                    if id(ind) in shared:
                        inT.append(shared[id(ind)])
                        continue
                    t = wk.alloc(KC * ST, BF16).rearrange("p (c t) -> p c t", t=ST)
                    inT.append((t, Res()))
                    shared[id(ind)] = inT[-1]
                wsb = []
                for (ind, Rin, KC, wfn) in groups:
                    wsb.append([(wk.alloc(KC * M, BF16).rearrange("p (c m) -> p c m", m=M), Res()) for _ in range(wbufs)])
                stage_raw = [(wk.alloc(ST, F32), Res()) for _ in range(2)]
                gemm_cache[ck] = (sig, inT, wsb, stage_raw)
            if out_dt == F32:
                stage = stage_raw
            else:
                stage = [(a.bitcast(BF16)[:, 0:ST], r) for a, r in stage_raw]
            ng = len(groups)
            nbk = 6 // ng
            cnt = 0
            u = 0
            for st in range(NT):
                loaded = set()
                for gi_, (ind, Rin, KC, wfn) in enumerate(groups):
                    if id(ind) in loaded:
                        continue
                    loaded.add(id(ind))
                    t, Rt = inT[gi_]
                    src = ind.rearrange("c p s -> p c s")
                    step = 4
                    for k0 in range(0, KC, step):
                        k1 = min(KC, k0 + step)
                        DMA("sp", t[:, k0:k1, :], src[:, k0:k1, st * ST:(st + 1) * ST], reads=[Rin], pwrites=[Rt])
                for slab in slabs:
                    wv = []
                    for gi_, (ind, Rin, KC, wfn) in enumerate(groups):
                        wt, Rw = wsb[gi_][cnt % wbufs]
                        DMA("pool", wt, wfn(slab), writes=[Rw])
                        wv.append((wt, Rw))
                    sg, Rsg = stage[cnt % 2]
                    cnt += 1
                    for tc in range(ST // 512):
                        pss = []
                        for gi_, (ind, Rin, KC, wfn) in enumerate(groups):
                            bi = (u % nbk) * ng + gi_
                            ps = bank(bi)[0:M, :]
                            t, Rt = inT[gi_]
                            wt, Rw = wv[gi_]
                            for kc in range(KC):
                                MM(ps, wt[:, kc, :], t[:, kc, tc * 512:(tc + 1) * 512], kc == 0, kc == KC - 1,
                                   reads=[Rt, Rw], writes=[PSR[bi]])
                            pss.append((ps, PSR[bi]))
                        u += 1
                        epi(slab, st * ST + tc * 512, pss, sg[0:M, tc * 512:(tc + 1) * 512], Rsg)
                    outs = out_fn(slab)
                    if not isinstance(outs, list):
                        outs = [(outs[0], outs[1], slice(0, M))]
                    for od, Rod, rows in outs:
                        DMA("sp", od[:, st * ST:(st + 1) * ST], sg[rows, :], reads=[Rsg], pwrites=[Rod])
            wk.off = wk_mark

        def gemm_tm(ind, Rin, KC, wfn, nslab, WC, epi, STILE):
            wk_mark = wk.off
            ST = min(STILE, S)
            NT = S // ST
            sig = ("tm", id(ind), KC, WC, ST)
            ck = (wk.gen, wk_mark)
            if ck in gemm_cache and gemm_cache[ck][0] == sig:
                _, t, Rt, wsb = gemm_cache[ck]
            else:
                if ck in gemm_cache:
                    sc.barrier_all()
                t = wk.alloc(KC * ST, BF16).rearrange("p (c t) -> p c t", t=ST)
                Rt = Res()
                wsb = [(wk.alloc(KC * WC, BF16).rearrange("p (c m) -> p c m", m=WC), Res()) for _ in range(2)]
                gemm_cache[ck] = (sig, t, Rt, wsb)
            src = ind.rearrange("c p s -> p c s")
            cnt = 0
            u = 0
            for st in range(NT):
                for k0 in range(0, KC, 4):
                    k1 = min(KC, k0 + 4)
                    DMA("sp", t[:, k0:k1, :], src[:, k0:k1, st * ST:(st + 1) * ST], reads=[Rin], pwrites=[Rt])
                for slab in range(nslab):
                    wt, Rw = wsb[cnt % 2]
                    cnt += 1
                    wsrc = wfn(slab)
                    kstep = max(1, 2048 // WC)
                    for k0 in range(0, KC, kstep):
                        k1 = min(KC, k0 + kstep)
                        DMA("pool", wt[:, k0:k1, :], wsrc[:, k0:k1, :], pwrites=[Rw])
                    for tb in range(ST // 128):
                        bi = u % 6
                        u += 1
                        ps = bank(bi)[:, 0:WC]
                        for kc in range(KC):
                            MM(ps, t[:, kc, tb * 128:(tb + 1) * 128], wt[:, kc, :], kc == 0, kc == KC - 1,
                               reads=[Rt, Rw], writes=[PSR[bi]])
                        epi(slab, st * (ST // 128) + tb, ps, PSR[bi])
            wk.off = wk_mark

        def layer(l, x_d, Rx, xn1_d, Rxn1, xn2_d, Rxn2):
            lam_init = 0.8 - 0.6 * math.exp(-0.3 * l)
            load_small(l)
            lo = SM["lam"]
            TT("dve", lamt[:, 0:1], small[:, lo:lo + 1], small[:, lo + 1:lo + 2], ALU.mult, reads=[Rsmall], writes=[Rlam])
            TT("dve", lamt[:, 1:2], small[:, lo + 2:lo + 3], small[:, lo + 3:lo + 4], ALU.mult, reads=[Rsmall], writes=[Rlam])
            psl = bank(7)[:, 0:2]
            MM(psl, ones_f, lamt[:, 0:2], True, True, reads=[Rlam, Rcf], writes=[PSR[7]])
            ACT(lamt[:, 2:4], psl, AF.Exp, reads=[PSR[7]], writes=[Rlam])
            TT("dve", lamt[:, 4:5], lamt[:, 3:4], lamt[:, 2:3], ALU.subtract, reads=[Rlam], writes=[Rlam])
            TS("dve", lamt[:, 5:6], lamt[:, 4:5], -lam_init, None, ALU.add, reads=[Rlam], writes=[Rlam])
            nlam = lamt[:, 5:6]
            TS("dve", small[0:4, SM["nfb"]:SM["nfb"] + 1], small[0:4, SM["fb"]:SM["fb"] + 1], -1.0, None, ALU.mult,
               reads=[Rsmall], writes=[Rsmall])

            norm_phase(x_d, Rx, SM["gmix"])
            sc.barrier_all()
            if upto == "n1":
                return

            wk.reset()
            ectr = [0]
            vst = [(wk.alloc(512, BF16), Res()) for _ in range(3)]
            vctr = [0]

            def epi_copy(slab, t0, pss, dst, Rd):
                ps, Rp_ = pss[0]
                ectr[0] += 1
                if ectr[0] % 2:
                    ACT(dst, ps, AF.Copy, reads=[Rp_], writes=[Rd])
                else:
                    COPY("dve", dst, ps, reads=[Rp_], writes=[Rd])

            def epi_sig(slab, t0, pss, dst, Rd):
                ps, Rp_ = pss[0]
                ACT(dst, ps, AF.Sigmoid, reads=[Rp_], writes=[Rd])

            def epi_rope(slab, t0, pss, dst, Rd):
                ps, Rp_ = pss[0]
                ectr[0] += 1
                k = ectr[0] % 2
                rb, Rrb = ropeb[k]
                rt, Rrt = ropet[k]
                ACT(dst, ps, AF.Copy, reads=[Rp_], writes=[Rd])
                prf = bank(7)
                pr = prf[0:32, :]
                MM(prf, rt_b, dst, True, True, reads=[Rd, Rcb], writes=[PSR[7]])
                TT("dve", rt[0:32, 0:512], ps[0:32, :], cst[0:32, t0:t0 + 512], ALU.mult, reads=[Rp_, Rcs], writes=[Rrt])
                TT("dve", rt[0:32, 512:1024], pr, snt[0:32, t0:t0 + 512], ALU.mult, reads=[PSR[7], Rcs], writes=[Rrt])
                TT("dve", dst[0:32, :], rt[0:32, 0:512], rt[0:32, 512:1024], ALU.add, reads=[Rrt], writes=[Rd])

            hgrp = lambda wbase: [(hT_d, R["hT"], KCD, lambda s: wfm_in[l, s])]
            gemm_fm(hgrp(0), range(0, 8), 128, epi_copy, lambda s: (mqk_d[s], R["mqk"]), F32, 2048)
            if upto == "g1a":
                return
            gemm_fm(hgrp(0), range(8, 16), 128, epi_sig, lambda s: (sgo_d[s - 8], R["sgo"]), F32, 2048)
            if upto == "g1a2":
                return
            gemm_fm(hgrp(0), range(48, 64), 128, epi_sig, lambda s: (sgm_d[s - 48], R["sgm"]), BF16, 2048)
            gemm_fm(hgrp(0), range(64, 80), 128, epi_sig, lambda s: (sga_d[s - 64], R["sga"]), BF16, 2048)
            if upto == "g1b":
                return
            gemm_fm(hgrp(0), range(16, 32), 128, epi_copy, lambda s: (aq_d[s - 16], R["aq"]), BF16, 2048)
            gemm_fm(hgrp(0), range(32, 48), 128, epi_copy, lambda s: (ak_d[s - 32], R["ak"]), BF16, 2048)
            if upto == "g1c":
                return
            ggrp = [(hT_d, R["hT"], KCD, lambda s: wg_in[l, s])]
            gemm_fm(ggrp, range(0, 1), 128, epi_copy, lambda s: [(gi_d, R["gi"], slice(0, 4)), (gf_d, R["gf"], slice(32, 36))], F32, 2048)
            if upto == "g1d":
                return

            def epi_v(slab, tb, ps, Rp_):
                vctr[0] += 1
                sg, Rsg = vst[vctr[0] % 3]
                if vctr[0] % 2:
                    ACT(sg, ps, AF.Copy, reads=[Rp_], writes=[Rsg])
                else:
                    COPY("dve", sg, ps, reads=[Rp_], writes=[Rsg])
                if slab < 2:
                    DMA("sp", mv_d[tb * 128:(tb + 1) * 128, slab * 512:(slab + 1) * 512], sg, reads=[Rsg], pwrites=[R["mv"]])
                else:
                    DMA("sp", av_d[tb * 128:(tb + 1) * 128, (slab - 2) * 512:(slab - 1) * 512], sg, reads=[Rsg], pwrites=[R["av"]])

            gemm_tm(hT_d, R["hT"], KCD, lambda s: wtm_in[l, s], 6, 512, epi_v, 2048)
            sc.barrier_all()
            if upto == "g1":
                return

            wk.reset()
            t_i = wk.alloc(S, F32)
            t_f = wk.alloc(S, F32)
            t_b = wk.alloc(S, F32)
            egs = wk.alloc(4 * TB, F32)
            Rti_, Rtf_, Rtb_, Regs = Res(), Res(), Res(), Res()
            sc.op("pool", lambda: nc.gpsimd.memset(t_i, 0.0), writes=[Rti_])
            sc.op("pool", lambda: nc.gpsimd.memset(t_f, 0.0), writes=[Rtf_])
            DMA("sp", t_i[0:4, :], gi_d, reads=[R["gi"]], writes=[Rti_])
            DMA("sp", t_f[0:4, :], gf_d, reads=[R["gf"]], writes=[Rtf_])
            TS("dve", t_i[0:4, :], t_i[0:4, :], small[0:4, SM["ib"]:SM["ib"] + 1], None, ALU.add, reads=[Rti_, Rsmall], writes=[Rti_])
            ACT(t_f[0:4, :], t_f[0:4, :], AF.Exp, reads=[Rtf_, Rsmall], writes=[Rtf_], scale=-1.0,
                bias=small[0:4, SM["nfb"]:SM["nfb"] + 1])
            ACT(t_f[0:4, :], t_f[0:4, :], AF.Ln, reads=[Rtf_], writes=[Rtf_], bias=1.0)
            for c in range(TB):
                sc.op("dve", lambda c=c: nc.vector.tensor_tensor_scan(out=t_b[0:4, c * 128:(c + 1) * 128], data0=ones4,
                                                                     data1=t_f[0:4, c * 128:(c + 1) * 128], initial=0.0,
                                                                     op0=ALU.mult, op1=ALU.add),
                      reads=[Rtf_, Rcf], writes=[Rtb_])
            ACT(t_f[0:4, :], t_b[0:4, :], AF.Exp, reads=[Rtb_], writes=[Rtf_], scale=-1.0)
            TT("dve", t_i[0:4, :], t_i[0:4, :], t_b[0:4, :], ALU.add, reads=[Rti_, Rtb_], writes=[Rti_])
            ACT(t_i[0:4, :], t_i[0:4, :], AF.Exp, reads=[Rti_], writes=[Rti_], bias=-0.5 * math.log(128.0))
            if upto == "m0a":
                return
            xin_ = [(wk.alloc(S, F32), Res()) for _ in range(2)]
            acc_ = [(wk.alloc(S, F32), Res()) for _ in range(2)]
            ost_ = [(wk.alloc(S, BF16), Res()) for _ in range(2)]
            cw = SM["convw"]
            for slab in range(8):
                xi, Rxi = xin_[slab % 2]
                ac, Rac = acc_[slab % 2]
                os_, Ros = ost_[slab % 2]
                h = slab % 4
                DMA("sp", xi, mqk_d[slab], reads=[R["mqk"]], writes=[Rxi])
                w = lambda j: small[:, cw + slab * 4 + j:cw + slab * 4 + j + 1]
                TS("dve", ac, xi, w(3), None, ALU.mult, reads=[Rxi, Rsmall], writes=[Rac])
                for j in range(3):
                    sh = 3 - j
                    STT(ac[:, sh:S], xi[:, 0:S - sh], w(j), ac[:, sh:S], ALU.mult, ALU.add, reads=[Rxi, Rac, Rsmall], writes=[Rac])
                ACT(ac, ac, AF.Silu, reads=[Rac, Rsmall], writes=[Rac], bias=small[:, SM["convb"] + slab:SM["convb"] + slab + 1])
                if upto == "m0c":
                    DMA("sp", hraw_d[0], ac, reads=[Rac], pwrites=[R["hraw"]])
                    return
                gsrc, Rg = (t_f, Rtf_) if slab < 4 else (t_i, Rti_)
                for tc in range(TC):
                    bi = tc % 8
                    ps = bank(bi)
                    MM(ps, sel_f[:, h, :], gsrc[:, tc * 512:(tc + 1) * 512], True, True, reads=[Rg, Rcf], writes=[PSR[bi]])
                    TT("dve", os_[:, tc * 512:(tc + 1) * 512], ac[:, tc * 512:(tc + 1) * 512], ps, ALU.mult,
                       reads=[Rac, PSR[bi]], writes=[Ros])
                    if slab < 4:
                        src = ps.rearrange("p (c t) -> p c t", t=128)[:, :, 127]
                        ACT(egs[:, h * TB + tc * 4:h * TB + tc * 4 + 4], src, AF.Copy, reads=[PSR[bi]], writes=[Regs])
                DMA("sp", qkt_d[slab], os_, reads=[Ros], pwrites=[R["qkt"]])
                if upto == "m0b":
                    return
            DMA("sp", egb_d, egs, reads=[Regs], writes=[R["egb"]])
            sc.barrier_all()
            if upto == "m0":
                return

            wk.reset()
            egs1 = wk.alloc(4 * TB, F32)
            Regs1 = Res()
            DMA("sp", egs1, egb_d, reads=[R["egb"]], writes=[Regs1])
            hd = []
            for i in range(2):
                d_ = dict(
                    q=wk.alloc(S, BF16), k=wk.alloc(S, BF16),
                    v=wk.alloc(TB * 384, BF16).rearrange("p (c v) -> p c v", v=384),
                    C=wk.alloc(384, F32), Ch=wk.alloc(384, F32), Cb=wk.alloc(384, BF16),
                    kt=[wk.alloc(128, BF16) for _ in range(2)], sm=[wk.alloc(128, BF16) for _ in range(2)],
                    dm=[wk.alloc(128, F32) for _ in range(2)],
                    hst=[wk.alloc(1024, F32).rearrange("p (j t) -> p j t", t=512) for _ in range(2)],
                    Rq=Res(), Rk=Res(), Rv=Res(), RC=Res(), RCh=Res(), RCb=Res(),
                    Rkt=[Res(), Res()], Rsm=[Res(), Res()], Rdm=[Res(), Res()], Rhst=[Res(), Res()],
                )
                hd.append(d_)
                sc.op("pool", lambda v=d_["v"]: nc.gpsimd.memset(v[:, :, 256:384], 1.0), writes=[d_["Rv"]])
            for hp in range(2):
                for i in range(2):
                    h = hp * 2 + i
                    d_ = hd[i]
                    DMA("sp", d_["q"], qkt_d[h], reads=[R["qkt"]], writes=[d_["Rq"]])
                    DMA("sp", d_["k"], qkt_d[4 + h], reads=[R["qkt"]], writes=[d_["Rk"]])
                    for c0 in range(0, TB, 8):
                        DMA("sp", d_["v"][:, c0:c0 + 8, 0:256], mv_d.rearrange("(c p) v -> p c v", p=128)[:, c0:c0 + 8, h * 256:(h + 1) * 256],
                            reads=[R["mv"]], pwrites=[d_["Rv"]])
                    sc.op("dve", lambda C=d_["C"]: nc.vector.memset(C, 0.0), writes=[d_["RC"]])
                    sc.op("dve", lambda C=d_["Ch"]: nc.vector.memset(C, 0.0), writes=[d_["RCh"]])
                    sc.op("dve", lambda C=d_["Cb"]: nc.vector.memset(C, 0.0), writes=[d_["RCb"]])
                for c in range(TB):
                    for i in range(2):
                        h = hp * 2 + i
                        d_ = hd[i]
                        b0 = i * 4
                        cs_ = slice(c * 128, (c + 1) * 128)
                        p2 = c % 2
                        psK = bank(b0 + 0, BF16)[:, 0:128]
                        psS = bank(b0 + 1)[:, 0:128]
                        psU = bank(b0 + 2)[:, 0:384]
                        psN = bank(b0 + 3)[:, 0:384].rearrange("p (j t) -> p j t", t=128)
                        PE(lambda o=psK, i_=d_["k"][:, cs_]: nc.tensor.transpose(o, i_, ident_b), reads=[d_["Rk"], Rcb], writes=[PSR[b0]])
                        ACT(d_["kt"][p2], psK, AF.Copy, reads=[PSR[b0]], writes=[d_["Rkt"][p2]])
                        MM(psS, d_["k"][:, cs_], d_["q"][:, cs_], True, True, reads=[d_["Rk"], d_["Rq"]], writes=[PSR[b0 + 1]])
                        TT("dve", d_["sm"][p2], psS, tri_b, ALU.mult, reads=[PSR[b0 + 1], Rcb], writes=[d_["Rsm"][p2]])
                        MM(psU, d_["kt"][p2], d_["v"][:, c, :], True, True, reads=[d_["Rkt"][p2], d_["Rv"]], writes=[PSR[b0 + 2]])
                        for j in range(3):
                            MM(psN[:, j, :], d_["v"][:, c, j * 128:(j + 1) * 128], d_["sm"][p2], True, False,
                               reads=[d_["Rv"], d_["Rsm"][p2]], writes=[PSR[b0 + 3]])
                            MM(psN[:, j, :], d_["Cb"][:, j * 128:(j + 1) * 128], d_["q"][:, cs_], False, True,
                               reads=[d_["RCb"], d_["Rq"]], writes=[PSR[b0 + 3]])
                        ACT(d_["dm"][p2], psN[:, 2, :], AF.Abs, reads=[PSR[b0 + 3]], writes=[d_["Rdm"][p2]])
                        TS("dve", d_["dm"][p2], d_["dm"][p2], 1.0, None, ALU.max, reads=[d_["Rdm"][p2]], writes=[d_["Rdm"][p2]])
                        sc.op("dve", lambda o=d_["dm"][p2]: nc.vector.reciprocal(out=o, in_=o), reads=[d_["Rdm"][p2]], writes=[d_["Rdm"][p2]])
                        hs_ = (c // 4) % 2
                        for j in range(2):
                            TT("dve", d_["hst"][hs_][:, j, (c % 4) * 128:(c % 4 + 1) * 128], psN[:, j, :], d_["dm"][p2], ALU.mult,
                               reads=[PSR[b0 + 3], d_["Rdm"][p2]], writes=[d_["Rhst"][hs_]])
                        if c % 4 == 3:
                            tc = c // 4
                            DMA("sp", hraw_d[2 * h:2 * h + 2].rearrange("j p s -> p j s")[:, :, tc * 512:(tc + 1) * 512], d_["hst"][hs_],
                                reads=[d_["Rhst"][hs_]], pwrites=[R["hraw"]])
                        eg = egs1[:, h * TB + c:h * TB + c + 1]
                        STT(d_["C"], psU, eg, d_["Ch"], ALU.mult, ALU.add, reads=[PSR[b0 + 2], Regs1, d_["RCh"]], writes=[d_["RC"]])
                        ACT(d_["Cb"], d_["C"], AF.Copy, reads=[d_["RC"]], writes=[d_["RCb"]])
                        if c + 1 < TB:
                            eg2 = egs1[:, h * TB + c + 1:h * TB + c + 2]
                            TS("pool", d_["Ch"], d_["C"], eg2, None, ALU.mult, reads=[d_["RC"], Regs1], writes=[d_["RCh"]])
            sc.barrier_all()
            if upto == "m1":
                return

            wk.reset()
            m2 = [dict(h=wk.alloc(1024, F32), g=wk.alloc(1024, F32), sq=wk.alloc(1024, F32), r=wk.alloc(512, F32),
                       o=wk.alloc(1024, BF16), Rh=Res(), Rg=Res(), Rsq=Res(), Rr=Res(), Ro=Res()) for _ in range(2)]
            u = 0
            for h in range(4):
                for tc in range(TC):
                    b_ = m2[u % 2]
                    bi = u % 4
                    u += 1
                    ts_ = slice(tc * 512, (tc + 1) * 512)
                    hv = b_["h"].rearrange("p (j t) -> p j t", t=512)
                    gv = b_["g"].rearrange("p (j t) -> p j t", t=512)
                    ov = b_["o"].rearrange("p (j t) -> p j t", t=512)
                    DMA("sp", hv, hraw_d[2 * h:2 * h + 2].rearrange("j p s -> p j s")[:, :, ts_], reads=[R["hraw"]], writes=[b_["Rh"]])
                    DMA("sp", gv, sgo_d[2 * h:2 * h + 2].rearrange("j p s -> p j s")[:, :, ts_], reads=[R["sgo"]], writes=[b_["Rg"]])
                    ACT(b_["sq"], b_["h"], AF.Square, reads=[b_["Rh"]], writes=[b_["Rsq"]])
                    ps = bank(bi)
                    MM(ps, ones_f, b_["sq"][:, 0:512], True, False, reads=[b_["Rsq"], Rcf], writes=[PSR[bi]])
                    MM(ps, ones_f, b_["sq"][:, 512:1024], False, True, reads=[b_["Rsq"], Rcf], writes=[PSR[bi]])
                    TS("dve", b_["r"], ps, 1.0 / 256, EPS, ALU.mult, ALU.add, reads=[PSR[bi]], writes=[b_["Rr"]])
                    ACT(b_["r"], b_["r"], AF.Ln, reads=[b_["Rr"]], writes=[b_["Rr"]])
                    ACT(b_["r"], b_["r"], AF.Exp, reads=[b_["Rr"]], writes=[b_["Rr"]], scale=-0.5)
                    for j in range(2):
                        gc = small[:, SM["gmh"] + 2 * h + j:SM["gmh"] + 2 * h + j + 1]
                        TS("pool", gv[:, j, :], gv[:, j, :], gc, None, ALU.mult, reads=[b_["Rg"], Rsmall], writes=[b_["Rg"]])
                        TT("dve", hv[:, j, :], hv[:, j, :], b_["r"], ALU.mult, reads=[b_["Rh"], b_["Rr"]], writes=[b_["Rh"]])
                        TT("dve", ov[:, j, :], hv[:, j, :], gv[:, j, :], ALU.mult, reads=[b_["Rh"], b_["Rg"]], writes=[b_["Ro"]])
                    DMA("sp", hmT_d[2 * h:2 * h + 2].rearrange("j p s -> p j s")[:, :, ts_], ov, reads=[b_["Ro"]], pwrites=[R["hmT"]])
            sc.barrier_all()
            if upto == "m2":
                return

            wk.reset()
            ab = [dict(q=[wk.alloc(S, BF16) for _ in range(2)], k=[wk.alloc(S, BF16) for _ in range(2)],
                       v=wk.alloc(TB * 256, BF16).rearrange("p (c v) -> p c v", v=256), R=Res()) for _ in range(2)]
            pT = [(wk.alloc(512, BF16), Res()) for _ in range(4)]
            on = [(wk.alloc(1024, F32), Res()) for _ in range(2)]
            rs_ = (wk.alloc(512, F32), Res())
            ot = (wk.alloc(1024, F32), Res())
            sq = (wk.alloc(1024, F32), Res())
            rr = (wk.alloc(512, F32), Res())
            ost = [(wk.alloc(1024, BF16), Res()) for _ in range(2)]
            scale = 128.0 ** -0.5
            cst = wk.alloc(S, F32)
            snt = wk.alloc(S, F32)
            Rcs = Res()
            DMA("sp", cst[0:32, :], cs_d[0], reads=[R["cs"]], pwrites=[Rcs])
            DMA("sp", snt[0:32, :], cs_d[1], reads=[R["cs"]], pwrites=[Rcs])
            xs = wk.alloc(S, BF16)
            Rxs = Res()
            rt1 = wk.alloc(1024, F32)
            rt2 = wk.alloc(1024, F32)
            Rrt12 = Res()
            RC = min(1024, S)
            uu = 0
            oc = 0
            for h in range(8):
                a_ = ab[h % 2]
                for c in range(2):
                    DMA("sp", a_["q"][c], aq_d[2 * h + c], reads=[R["aq"]], pwrites=[a_["R"]])
                    DMA("sp", a_["k"][c], ak_d[2 * h + c], reads=[R["ak"]], pwrites=[a_["R"]])
                for c0 in range(0, TB, 8):
                    DMA("sp", a_["v"][:, c0:c0 + 8, :], av_d.rearrange("(c p) v -> p c v", p=128)[:, c0:c0 + 8, h * 256:(h + 1) * 256],
                        reads=[R["av"]], pwrites=[a_["R"]])
                for c in range(2):
                    for (buf, src_d, Rsrc) in ((a_["q"][c], aq_d[2 * h + c], R["aq"]), (a_["k"][c], ak_d[2 * h + c], R["ak"])):
                        DMA("sp", xs[0:16, :], src_d[16:32, :], reads=[Rsrc], pwrites=[Rxs])
                        DMA("sp", xs[16:32, :], src_d[0:16, :], reads=[Rsrc], pwrites=[Rxs])
                        for r0_ in range(0, S, RC):
                            sl = slice(r0_, r0_ + RC)
                            TT("dve", rt1[0:32, 0:RC], buf[0:32, sl], cst[0:32, sl], ALU.mult, reads=[a_["R"], Rcs], writes=[Rrt12])
                            TT("dve", rt2[0:32, 0:RC], xs[0:32, sl], snt[0:32, sl], ALU.mult, reads=[Rxs, Rcs], writes=[Rrt12])
                            TT("dve", buf[0:32, sl], rt1[0:32, 0:RC], rt2[0:32, 0:RC], ALU.add, reads=[Rrt12], writes=[a_["R"]])
                for j in range(TC):
                    for c in range(2):
                        psO = bank(3)
                        psO1 = bank(4)
                        psSum = bank(5)
                        nk = 4 * j + 4
                        units = []
                        for m in range(nk):
                            r0 = 128 * max(0, m - 4 * j)
                            units.append((m, r0))

                        def do_s(m, r0, idx):
                            bi = idx % 3
                            MM(bank(bi)[:, r0:512], a_["k"][c][:, m * 128:(m + 1) * 128], a_["q"][c][:, j * 512 + r0:(j + 1) * 512],
                               True, True, reads=[a_["R"]], writes=[PSR[bi]])
                            p_, Rp2 = pT[idx % 4]
                            ACT(p_[:, r0:512], bank(bi)[:, r0:512], AF.Exp, reads=[PSR[bi]], writes=[Rp2], scale=scale)
                            if m >= 4 * j:
                                TT("pool", p_[:, r0:r0 + 128], p_[:, r0:r0 + 128], tri_b, ALU.mult, reads=[Rp2, Rcb], writes=[Rp2])

                        def do_pv(m, r0, idx, first, last):
                            p_, Rp2 = pT[idx % 4]
                            MM(psO[:, r0:512], a_["v"][:, m, 0:128], p_[:, r0:512], first, last, reads=[a_["R"], Rp2], writes=[PSR[3]])
                            MM(psO1[:, r0:512], a_["v"][:, m, 128:256], p_[:, r0:512], first, last, reads=[a_["R"], Rp2], writes=[PSR[4]])
                            MM(psSum[:, r0:512], ones_b, p_[:, r0:512], first, last, reads=[Rcb, Rp2], writes=[PSR[5]])

                        do_s(units[0][0], units[0][1], uu)
                        for ui in range(nk):
                            if ui + 1 < nk:
                                do_s(units[ui + 1][0], units[ui + 1][1], uu + ui + 1)
                            do_pv(units[ui][0], units[ui][1], uu + ui, ui == 0, ui == nk - 1)
                        uu += nk
                        rt_, Rrt_ = rs_
                        sc.op("dve", lambda o=rt_, i_=psSum: nc.vector.reciprocal(out=o, in_=i_), reads=[PSR[5]], writes=[Rrt_])
                        on_, Ron = on[c]
                        TT("dve", on_[:, 0:512], psO, rt_, ALU.mult, reads=[PSR[3], Rrt_], writes=[Ron])
                        TT("dve", on_[:, 512:1024], psO1, rt_, ALU.mult, reads=[PSR[4], Rrt_], writes=[Ron])
                    o_, Ro_ = ot
                    STT(o_, on[1][0], nlam, on[0][0], ALU.mult, ALU.add, reads=[on[0][1], on[1][1], Rlam], writes=[Ro_])
                    s_, Rs_ = sq
                    ACT(s_, o_, AF.Square, reads=[Ro_], writes=[Rs_])
                    psn = bank(6)
                    MM(psn, ones_f, s_[:, 0:512], True, False, reads=[Rs_, Rcf], writes=[PSR[6]])
                    MM(psn, ones_f, s_[:, 512:1024], False, True, reads=[Rs_, Rcf], writes=[PSR[6]])
                    r_, Rr_ = rr
                    TS("dve", r_, psn, 1.0 / 256, EPS, ALU.mult, ALU.add, reads=[PSR[6]], writes=[Rr_])
                    ACT(r_, r_, AF.Ln, reads=[Rr_], writes=[Rr_])
                    ACT(r_, r_, AF.Exp, reads=[Rr_], writes=[Rr_], scale=-0.5, bias=math.log(1.0 - lam_init))
                    os2, Ros2 = ost[oc % 2]
                    oc += 1
                    for jj in range(2):
                        gs = small[:, SM["gsub"] + jj:SM["gsub"] + jj + 1]
                        STT(os2[:, jj * 512:(jj + 1) * 512], o_[:, jj * 512:(jj + 1) * 512], gs, r_, ALU.mult, ALU.mult,
                            reads=[Ro_, Rr_, Rsmall], writes=[Ros2])
                    DMA("sp", haT_d[2 * h:2 * h + 2].rearrange("j p s -> p j s")[:, :, j * 512:(j + 1) * 512],
                        os2.rearrange("p (j t) -> p j t", t=512), reads=[Ros2], pwrites=[R["haT"]])
            sc.barrier_all()
            if upto == "a":
                return

            wk.reset()
            ygm = [(wk.alloc(2048, BF16), Res()) for _ in range(2)]
            yga = [(wk.alloc(2048, BF16), Res()) for _ in range(2)]
            ytm = [(wk.alloc(512, F32), Res()) for _ in range(2)]
            ytm2 = [(wk.alloc(512, F32), Res()) for _ in range(2)]
            ycur = [None, None, 0]

            def epi_y(slab, t0, pss, dst, Rd):
                ST = min(2048, S)
                if t0 % ST == 0:
                    k = ycur[2] % 2
                    ycur[2] += 1
                    ycur[0] = ygm[k]
                    ycur[1] = yga[k]
                    DMA("act", ycur[0][0][:, 0:ST], sgm_d[slab][:, t0:t0 + ST], reads=[R["sgm"]], writes=[ycur[0][1]])
                    DMA("act", ycur[1][0][:, 0:ST], sga_d[slab][:, t0:t0 + ST], reads=[R["sga"]], writes=[ycur[1][1]])
                off = t0 % ST
                (pm_, Rpm), (pa_, Rpa) = pss
                k = (t0 // 512) % 2
                a1, Ra1 = ytm[k]
                a2, Ra2 = ytm2[k]
                TT("dve", a1, pm_, ycur[0][0][:, off:off + 512], ALU.mult, reads=[Rpm, ycur[0][1]], writes=[Ra1])
                TT("dve", a2, pa_, ycur[1][0][:, off:off + 512], ALU.mult, reads=[Rpa, ycur[1][1]], writes=[Ra2])
                TT("pool", dst, a1, a2, ALU.add, reads=[Ra1, Ra2], writes=[Rd])

            gemm_fm([(hmT_d, R["hmT"], 8, lambda s: pm_in[l, s]), (haT_d, R["haT"], 16, lambda s: pa_in[l, s])],
                    range(16), 128, epi_y, lambda s: (yT_d[s], R["yT"]), BF16, 2048, wbufs=2)
            sc.barrier_all()
            if upto == "y":
                return

            wk.reset()
            xr = [(wk.alloc(512, F32), Res()) for _ in range(3)]
            octr = [0]

            def mk_epi_res(xs_d, Rxs, xd_d, Rxd, WC):
                def epi(slab, tb, ps, Rp_):
                    octr[0] += 1
                    xt_, Rxt_ = xr[octr[0] % 3]
                    rows = slice(tb * 128, (tb + 1) * 128)
                    cols = slice(slab * WC, (slab + 1) * WC)
                    DMA("act", xt_[:, 0:WC], xs_d[rows, cols], reads=[Rxs], writes=[Rxt_])
                    TT("dve", xt_[:, 0:WC], ps, xt_[:, 0:WC], ALU.add, reads=[Rp_, Rxt_], writes=[Rxt_])
                    DMA("sp", xd_d[rows, cols], xt_[:, 0:WC], reads=[Rxt_], pwrites=[Rxd])
                return epi

            gemm_tm(yT_d, R["yT"], KCD, lambda s: wo_in[l, s], 4, 512, mk_epi_res(x_d, Rx, xn1_d, Rxn1, 512), 2048)
            sc.barrier_all()
            if upto == "o":
                return

            norm_phase(xn1_d, Rxn1, SM["gffn"])
            sc.barrier_all()
            wk.reset()
            fs = [(wk.alloc(512, F32), Res()) for _ in range(2)]
            fctr = [0]

            def epi_ffn(slab, t0, pss, dst, Rd):
                (pg, Rpg), (pu, Rpu) = pss
                fctr[0] += 1
                s_, Rs_ = fs[fctr[0] % 2]
                ACT(s_, pg, AF.Silu, reads=[Rpg], writes=[Rs_])
                TT("dve", dst, pu, s_, ALU.mult, reads=[Rpu, Rs_], writes=[Rd])

            gemm_fm([(hT_d, R["hT"], KCD, lambda s: wga_in[l, s]), (hT_d, R["hT"], KCD, lambda s: wup_in[l, s])],
                    range(44), 128, epi_ffn, lambda s: (actT_d[s], R["actT"]), BF16, 2048, wbufs=2)
            sc.barrier_all()
            if upto == "f1":
                return
            wk.reset()
            xr[:] = [(wk.alloc(512, F32), Res()) for _ in range(3)]
            gemm_tm(actT_d, R["actT"], KCF, lambda s: wdn_in[l, s], 8, 256, mk_epi_res(xn1_d, Rxn1, xn2_d, Rxn2, 256), 1024)
            sc.barrier_all()

        cur, Rcur = x_in, R["xin"]
        done = False
        for l in range(depth):
            layer(l, cur, Rcur, xa_d, R["xa"], xb_d, R["xb"])
            cur, Rcur = xb_d, R["xb"]
            if upto is not None:
                done = True
                break
        if not done:
            wk.reset()
            gf_ = wk.alloc(D, F32)
            Rgf = Res()
            DMA("sp", gf_, gfin_in, writes=[Rgf])
            xt = [(wk.alloc(D, F32), Res()) for _ in range(2)]
            junk = (wk.alloc(D, BF16), Res())
            yo = [(wk.alloc(D, F32), Res()) for _ in range(2)]
            st = wk.alloc(4 * TB, F32)
            Rst = [Res(), Res()]
            for tb in range(TB):
                s2 = tb % 2
                x_, Rx_ = xt[s2]
                DMA("sp", x_, cur[tb * 128:(tb + 1) * 128, :], reads=[Rcur], writes=[Rx_])
                ss = st[:, 4 * tb:4 * tb + 1]
                ms = st[:, 4 * tb + 1:4 * tb + 2]
                rstd = st[:, 4 * tb + 2:4 * tb + 3]
                ACT(junk[0], x_, AF.Square, reads=[Rx_], writes=[Rst[s2]], accum_out=ss)
                TS("dve", ms, ss, 1.0 / D, EPS, ALU.mult, ALU.add, reads=[Rst[s2]], writes=[Rst[s2]])
                ACT(ms, ms, AF.Ln, reads=[Rst[s2]], writes=[Rst[s2]])
                ACT(rstd, ms, AF.Exp, reads=[Rst[s2]], writes=[Rst[s2]], scale=-0.5)
                y_, Ry_ = yo[s2]
                STT(y_, x_, rstd, gf_, ALU.mult, ALU.mult, reads=[Rx_, Rst[s2], Rgf], writes=[Ry_])
                DMA("sp", out_d[tb * 128:(tb + 1) * 128, :], y_, reads=[Ry_], pwrites=[R["out"]])

        block = es.enter_context(nc.Block())
        sc.emit(block)
    return nc, dbg_outs


def fm_slabs(w, ncol):
    L, K, N = w.shape
    return np.ascontiguousarray(w.reshape(L, K // 128, 128, N // ncol, ncol).transpose(0, 3, 2, 1, 4))


def host_consts():
    cf = np.zeros((128, 1024), np.float32)
    cf[:, 0:128] = 1.0
    for h in range(4):
        cf[h, 128 + h * 128:128 + (h + 1) * 128] = 1.0
    half = 16
    inv = np.power(np.float32(ROPE_THETA), -np.arange(half, dtype=np.float32) * np.float32(2.0 / 32)).astype(np.float32)
    cf[0:16, 640] = inv / np.float32(2 * math.pi)
    cf[16:32, 640] = inv / np.float32(2 * math.pi)
    cf[:, 641] = -0.5
    cf[:, 642] = 0.25
    cf[0:4, 768:896] = 1.0
    cb = np.zeros((128, 512), np.float32)
    cb[:, 0:128] = np.eye(128)
    cb[:, 128:256] = 1.0
    cb[:, 256:384] = (np.arange(128)[None, :] >= np.arange(128)[:, None])
    rt = np.zeros((32, 32), np.float32)
    for m in range(32):
        rt[(m + 16) % 32, m] = 1.0
    cb[0:32, 384:416] = rt
    return cf, cb.astype(ml_dtypes.bfloat16)


def host_layout(inp, depth):
    w_in = np.asarray(inp["w_in"])[:depth]
    o = {}
    c = lambda a, b: w_in[:, :, a:b]
    fam = [c(0, 1024), c(2048, 3072), c(3080, 5128), c(5128, 7176), c(9224, 11272), c(11272, 13320)]
    o["wfm"] = np.concatenate([fm_slabs(f, 128) for f in fam], axis=1)
    wg = np.zeros((depth, 1, 128, KCD, 128), np.float32)
    wg[:, 0, :, :, 0:4] = fm_slabs(c(3072, 3076), 4)[:, 0]
    wg[:, 0, :, :, 32:36] = fm_slabs(c(3076, 3080), 4)[:, 0]
    o["wg"] = wg
    o["wtm"] = np.concatenate([fm_slabs(c(1024, 2048), 512), fm_slabs(c(7176, 9224), 512)], axis=1)
    o["pm"] = fm_slabs(np.asarray(inp["p_m"])[:depth], 128)
    o["pa"] = fm_slabs(np.asarray(inp["p_a"])[:depth], 128)
    o["wo"] = fm_slabs(np.asarray(inp["w_out"])[:depth], 512)
    o["wga"] = fm_slabs(np.asarray(inp["w_gate"])[:depth], 128)
    o["wup"] = fm_slabs(np.asarray(inp["w_up"])[:depth], 128)
    o["wdn"] = fm_slabs(np.asarray(inp["w_down"])[:depth], 256)
    small = np.zeros((depth, 128, NSMALL), np.float32)
    g = lambda k: np.asarray(inp[k])[:depth]
    small[:, :, 0:16] = g("g_mix").reshape(depth, 16, 128).transpose(0, 2, 1)
    small[:, :, 16:32] = g("g_ffn").reshape(depth, 16, 128).transpose(0, 2, 1)
    small[:, :, 32:64] = g("conv_w").reshape(depth, 4, 8, 128).transpose(0, 3, 2, 1).reshape(depth, 128, 32)
    small[:, :, 64:72] = g("conv_b").reshape(depth, 8, 128).transpose(0, 2, 1)
    small[:, :, 72:80] = g("g_mhead").reshape(depth, 8, 128).transpose(0, 2, 1)
    small[:, :, 80] = g("lambda_q1")
    small[:, :, 81] = g("lambda_k1")
    small[:, :, 82] = g("lambda_q2")
    small[:, :, 83] = g("lambda_k2")
    small[:, :, 84:86] = g("g_sub").reshape(depth, 2, 128).transpose(0, 2, 1)
    small[:, 0:4, 86] = g("i_bias")
    small[:, 0:4, 87] = g("f_bias")
    o["small"] = small
    o["gfin"] = np.ascontiguousarray(np.broadcast_to(np.asarray(inp["g_final"])[None, :], (128, D)))
    cf, cb = host_consts()
    o["cf"] = cf
    o["cb"] = cb
    return o


_CACHE = {}


def kernel(**inputs):
    x = np.asarray(inputs["x"])
    pos = np.asarray(inputs["positions"]).astype(np.int32)
    B, S, _ = x.shape
    shared = host_layout(inputs, DEPTH)
    key = (S, DEPTH)
    if key not in _CACHE:
        _CACHE[key] = build(S, DEPTH)[0]
    nc = _CACHE[key]
    in_maps = []
    for b in range(B):
        m = dict(shared)
        m["x"] = np.ascontiguousarray(x[b])
        m["pos"] = np.ascontiguousarray(pos[b:b + 1])
        in_maps.append(m)
    res = run_bass_kernel_spmd(nc, in_maps, core_ids=list(range(B)))
    return np.stack([np.asarray(r["out"]) for r in res.results], axis=0).astype(np.float32)
```

```python
import math
from contextlib import ExitStack

import numpy as np
import ml_dtypes

import concourse.bass as bass
import concourse.mybir as mybir
from concourse.bass_utils import run_bass_kernel_spmd

F32 = mybir.dt.float32
BF16 = mybir.dt.bfloat16
I32 = mybir.dt.int32
U8 = mybir.dt.uint8
AF = mybir.ActivationFunctionType
ALU = mybir.AluOpType

D = 2048
KCD = 16
DFF = 5632
KCF = 44
DEPTH = 4
SEQ = 4096
EPS = 1e-6
NSMALL = 96
ROPE_THETA = 500000.0


class Op:
    __slots__ = ("eng", "fn", "deps", "needed", "sigval", "dma", "sem", "val", "slot")


class OpSet:
    __slots__ = ("d",)

    def __init__(self):
        self.d = {}

    def add(self, o):
        self.d[(o.eng, o.slot) if o.dma else o.eng] = o

    def ops(self):
        return self.d.values()

    def __bool__(self):
        return bool(self.d)


class Res:
    __slots__ = ("ws", "rs", "prs", "name")

    def __init__(self, name=""):
        self.ws = OpSet()
        self.rs = OpSet()
        self.prs = OpSet()
        self.name = name


class Sched:
    ENGS = ("pe", "act", "dve", "pool", "sp")
    QUEUES = ("sp", "pool", "act")

    def __init__(self, nc, es, K=4):
        self.nc = nc
        self.K = K
        self.streams = {e: [] for e in self.ENGS}
        self.engsem = {e: es.enter_context(nc.semaphore("sem_" + e)) for e in self.ENGS}
        self.ring = {q: [es.enter_context(nc.semaphore("ring_%s_%d" % (q, i))) for i in range(K)] for q in self.QUEUES}
        self.ndma = {q: 0 for q in self.QUEUES}
        self.ringlast = {q: [None] * K for q in self.QUEUES}
        self.lastc = {e: None for e in self.ENGS}
        self.barrier = {}

    def op(self, eng, fn, reads=(), writes=(), pwrites=(), dma=False):
        o = Op()
        o.eng = eng
        o.fn = fn
        o.dma = dma
        o.needed = False
        o.sigval = None
        o.slot = None
        o.sem = None
        o.val = None
        deps = set()
        if eng in self.barrier:
            deps |= self.barrier.pop(eng)
        if dma:
            n = self.ndma[eng]
            self.ndma[eng] = n + 1
            slot = n % self.K
            o.slot = slot
            o.sem = self.ring[eng][slot]
            o.val = 16 * (n // self.K + 1)
            prev = self.ringlast[eng][slot]
            if prev is not None:
                deps.add(prev)
            self.ringlast[eng][slot] = o
        for r in reads:
            deps.update(r.ws.ops())
        for w in writes:
            deps.update(w.ws.ops())
            deps.update(w.rs.ops())
            if not w.rs:
                deps.update(w.prs.ops())
        for w in pwrites:
            if w.rs:
                deps.update(w.rs.ops())
            else:
                deps.update(w.prs.ops())
        if eng == "pe":
            deps = {d for d in deps if d.dma or d.eng != "pe"}
        deps.discard(o)
        for d in deps:
            if not d.dma:
                d.needed = True
        o.deps = deps
        for r in reads:
            r.rs.add(o)
        for w in writes:
            if w.rs:
                w.prs = w.rs
                w.rs = OpSet()
            w.ws = OpSet()
            w.ws.add(o)
        for w in pwrites:
            if w.rs:
                w.prs = w.rs
                w.rs = OpSet()
                w.ws = OpSet()
            w.ws.add(o)
        self.streams[eng].append(o)
        if not dma:
            self.lastc[eng] = o
        return o

    def barrier_all(self):
        b = set()
        for e, o in self.lastc.items():
            if o is not None:
                b.add(o)
        for q in self.QUEUES:
            for o in self.ringlast[q]:
                if o is not None:
                    b.add(o)
        for e in self.ENGS:
            self.barrier[e] = set(b) | self.barrier.get(e, set())

    def emit(self, block):
        nc = self.nc
        for e, st in self.streams.items():
            c = 0
            for o in st:
                if (not o.dma) and o.needed:
                    c += 1
                    o.sigval = c
        engmap = {"pe": block.tensor, "act": block.scalar, "dve": block.vector, "pool": block.gpsimd, "sp": block.sync}
        nceng = {"pe": nc.tensor, "act": nc.scalar, "dve": nc.vector, "pool": nc.gpsimd, "sp": nc.sync}
        finals = []
        for q in self.QUEUES:
            for o in self.ringlast[q]:
                if o is not None:
                    finals.append((o.sem, o.val))
        engsem = self.engsem

        def mk(e, st):
            def body(_e):
                eng = nceng[e]
                known = {}
                for o in st:
                    waits = {}
                    for d in o.deps:
                        if d.dma:
                            sem, v = d.sem, d.val
                        else:
                            sem, v = engsem[d.eng], d.sigval
                        k = id(sem)
                        if k not in waits or waits[k][1] < v:
                            waits[k] = (sem, v)
                    for k, (sem, v) in waits.items():
                        if known.get(k, 0) < v:
                            eng.wait_ge(sem, v)
                            known[k] = v
                    ins = o.fn()
                    if o.dma:
                        ins.then_inc(o.sem, 16)
                    elif o.needed:
                        ins.then_inc(engsem[e], 1)
                if e == "sp":
                    for sem, v in finals:
                        eng.wait_ge(sem, v)
            return body

        for e, st in self.streams.items():
            engmap[e](mk(e, st))


class Carver:
    def __init__(self, base, size, start=0):
        self.base = base
        self.size = size
        self.start = start
        self.off = start
        self.gen = 0

    def reset(self):
        self.off = self.start
        self.gen += 1

    def alloc(self, n, dt):
        nb = {F32: 4, BF16: 2, I32: 4, U8: 1}[dt]
        off = (self.off + 63) // 64 * 64
        assert off + n * nb <= self.size, ("SBUF overflow", off, n * nb, self.size)
        self.off = off + n * nb
        return self.base[:, off:off + n * nb].bitcast(dt)


def build(S, depth, debug=False, upto=None):
    nc = bass.Bass("TRN2", target_bir_lowering=False)
    TB = S // 128
    TC = S // 512
    dbg_outs = []

    def din(name, shape, dt):
        return nc.dram_tensor(name, list(shape), dt, kind="ExternalInput").ap()

    def dscr(name, shape, dt):
        if debug:
            dbg_outs.append(name)
            return nc.dram_tensor(name, list(shape), dt, kind="ExternalOutput").ap()
        return nc.dram_tensor(name, list(shape), dt, kind="Internal").ap()

    x_in = din("x", [S, D], F32)
    pos_in = din("pos", [1, S], I32)
    small_in = din("small", [depth, 128, NSMALL], F32)
    gfin_in = din("gfin", [128, D], F32)
    cf_in = din("cf", [128, 1024], F32)
    cb_in = din("cb", [128, 512], BF16)
    wfm_in = din("wfm", [depth, 80, 128, KCD, 128], F32)
    wg_in = din("wg", [depth, 1, 128, KCD, 128], F32)
    wtm_in = din("wtm", [depth, 6, 128, KCD, 512], F32)
    pm_in = din("pm", [depth, 16, 128, 8, 128], F32)
    pa_in = din("pa", [depth, 16, 128, 16, 128], F32)
    wo_in = din("wo", [depth, 4, 128, KCD, 512], F32)
    wga_in = din("wga", [depth, 44, 128, KCD, 128], F32)
    wup_in = din("wup", [depth, 44, 128, KCD, 128], F32)
    wdn_in = din("wdn", [depth, 8, 128, KCF, 256], F32)
    out_d = nc.dram_tensor("out", [S, D], F32, kind="ExternalOutput").ap()

    hT_d = dscr("hT", [KCD, 128, S], BF16)
    mqk_d = dscr("mqk", [8, 128, S], F32)
    sgo_d = dscr("sgo", [8, 128, S], F32)
    gi_d = dscr("gi", [4, S], F32)
    gf_d = dscr("gf", [4, S], F32)
    aq_d = dscr("aq", [16, 128, S], BF16)
    ak_d = dscr("ak", [16, 128, S], BF16)
    sgm_d = dscr("sgm", [16, 128, S], BF16)
    sga_d = dscr("sga", [16, 128, S], BF16)
    mv_d = dscr("mv", [S, 1024], BF16)
    av_d = dscr("av", [S, 2048], BF16)
    qkt_d = dscr("qkt", [8, 128, S], BF16)
    egb_d = dscr("egb", [128, 4 * TB], F32)
    hraw_d = dscr("hraw", [8, 128, S], F32)
    hmT_d = dscr("hmT", [8, 128, S], BF16)
    haT_d = dscr("haT", [16, 128, S], BF16)
    yT_d = dscr("yT", [16, 128, S], BF16)
    actT_d = dscr("actT", [KCF, 128, S], BF16)
    xa_d = dscr("xa", [S, D], F32)
    xb_d = dscr("xb", [S, D], F32)
    cs_d = dscr("cs", [2, 32, S], F32)

    R = {}
    for n in ("hT", "mqk", "sgo", "gi", "gf", "aq", "ak", "sgm", "sga", "mv", "av", "qkt", "egb", "hraw", "hmT", "haT",
              "yT", "actT", "xa", "xb", "cs", "out", "xin"):
        R[n] = Res(n)

    es = ExitStack()
    with es:
        sb = es.enter_context(nc.sbuf_tensor("sb", [128, 196608], U8))
        pst = es.enter_context(nc.psum_tensor("ps", [128, 16384], U8))
        sc = Sched(nc, es, K=4)

        def bank(i, dt=F32):
            return pst[:, i * 2048:(i + 1) * 2048].bitcast(dt)

        PSR = [Res("ps%d" % i) for i in range(8)]

        PERS = 12288
        pc = Carver(sb, PERS, 0)
        wk = Carver(sb, 196608, PERS)

        def DMA(q, out, in_, reads=(), writes=(), pwrites=()):
            eng = {"sp": nc.sync, "pool": nc.gpsimd, "act": nc.scalar}[q]
            n = out.shape[-1]
            if n > 2048 and tuple(out.shape) == tuple(in_.shape):
                r = None
                for c0 in range(0, n, 2048):
                    c1 = min(n, c0 + 2048)
                    idx = tuple([slice(None)] * (len(out.shape) - 1) + [slice(c0, c1)])
                    o_, i_ = out[idx], in_[idx]
                    r = sc.op(q, lambda o_=o_, i_=i_: eng.dma_start(out=o_, in_=i_), reads, writes, pwrites, dma=True)
                return r
            return sc.op(q, lambda: eng.dma_start(out=out, in_=in_), reads, writes, pwrites, dma=True)

        def PE(fn, reads=(), writes=()):
            return sc.op("pe", fn, reads, writes)

        def MM(out, lhsT, rhs, start, stop, reads=(), writes=()):
            return sc.op("pe", lambda: nc.tensor.matmul(out, lhsT, rhs, start=start, stop=stop), reads, writes)

        def ACT(out, in_, func, reads=(), writes=(), bias=None, scale=None, accum_out=None):
            kw = {}
            if bias is not None:
                kw["bias"] = bias
            if scale is not None:
                kw["scale"] = scale
            if accum_out is not None:
                kw["accum_out"] = accum_out
            return sc.op("act", lambda: nc.scalar.activation(out=out, in_=in_, func=func, **kw), reads, writes)

        def TS(eng, out, in0, s1, s2, op0, op1=None, reads=(), writes=()):
            e = {"dve": nc.vector, "pool": nc.gpsimd}[eng]
            if op1 is None:
                return sc.op(eng, lambda: e.tensor_scalar(out=out, in0=in0, scalar1=s1, scalar2=None, op0=op0), reads, writes)
            return sc.op(eng, lambda: e.tensor_scalar(out=out, in0=in0, scalar1=s1, scalar2=s2, op0=op0, op1=op1), reads, writes)

        def TT(eng, out, in0, in1, op, reads=(), writes=()):
            e = {"dve": nc.vector, "pool": nc.gpsimd}[eng]
            return sc.op(eng, lambda: e.tensor_tensor(out=out, in0=in0, in1=in1, op=op), reads, writes)

        def STT(out, in0, scalar, in1, op0, op1, reads=(), writes=()):
            return sc.op("dve", lambda: nc.vector.scalar_tensor_tensor(out=out, in0=in0, scalar=scalar, in1=in1, op0=op0, op1=op1), reads, writes)

        def COPY(eng, out, in_, reads=(), writes=()):
            if eng == "act":
                return sc.op("act", lambda: nc.scalar.copy(out=out, in_=in_), reads, writes)
            e = {"dve": nc.vector, "pool": nc.gpsimd}[eng]
            return sc.op(eng, lambda: e.tensor_copy(out=out, in_=in_), reads, writes)

        cf = pc.alloc(1024, F32)
        cbt = pc.alloc(512, BF16)
        small = pc.alloc(NSMALL, F32)
        lamt = pc.alloc(8, F32)
        Rcf, Rcb, Rsmall, Rlam = Res("cf"), Res("cb"), Res("small"), Res("lam")
        DMA("sp", cf, cf_in, writes=[Rcf])
        DMA("sp", cbt, cb_in, writes=[Rcb])
        ones_f = cf[:, 0:128]
        sel_f = cf[:, 128:640].rearrange("p (h c) -> p h c", c=128)
        invf = cf[0:32, 640:641]
        mhalf = cf[:, 641:642]
        c025 = cf[0:32, 642:643]
        ones4 = cf[0:4, 768:896]
        ident_b = cbt[:, 0:128]
        ones_b = cbt[:, 128:256]
        tri_b = cbt[:, 256:384]
        rt_b = cbt[:, 384:512]

        wk.reset()
        posi = wk.alloc(S, I32)
        ang = wk.alloc(S, F32)
        t1 = wk.alloc(S, F32)
        t2 = wk.alloc(S, F32)
        ti = wk.alloc(S, I32)
        Rp, Ra, Rt1, Rt2, Rti = Res(), Res(), Res(), Res(), Res()
        DMA("sp", posi[0:32, :], pos_in.partition_broadcast(32), writes=[Rp])
        COPY("dve", ang[0:32, :], posi[0:32, :], reads=[Rp], writes=[Ra])
        TS("dve", ang[0:32, :], ang[0:32, :], invf, None, ALU.mult, reads=[Ra, Rcf], writes=[Ra])
        for which in (0, 1):
            if which == 0:
                TS("dve", t1[0:32, :], ang[0:32, :], 0.25, None, ALU.add, reads=[Ra], writes=[Rt1])
            else:
                COPY("dve", t1[0:32, :], ang[0:32, :], reads=[Ra], writes=[Rt1])
            COPY("dve", ti[0:32, :], t1[0:32, :], reads=[Rt1], writes=[Rti])
            COPY("dve", t2[0:32, :], ti[0:32, :], reads=[Rti], writes=[Rt2])
            TT("dve", t1[0:32, :], t1[0:32, :], t2[0:32, :], ALU.subtract, reads=[Rt1, Rt2], writes=[Rt1])
            TS("dve", t2[0:32, :], t1[0:32, :], 0.5, None, ALU.is_gt, reads=[Rt1], writes=[Rt2])
            TT("dve", t1[0:32, :], t1[0:32, :], t2[0:32, :], ALU.subtract, reads=[Rt1, Rt2], writes=[Rt1])
            TS("dve", t2[0:32, :], t1[0:32, :], -0.5, None, ALU.is_lt, reads=[Rt1], writes=[Rt2])
            TT("dve", t1[0:32, :], t1[0:32, :], t2[0:32, :], ALU.add, reads=[Rt1, Rt2], writes=[Rt1])
            ACT(t2[0:32, :], t1[0:32, :], AF.Sin, reads=[Rt1], writes=[Rt2], scale=6.28318)
            if which == 1:
                TS("dve", t2[0:16, :], t2[0:16, :], -1.0, None, ALU.mult, reads=[Rt2], writes=[Rt2])
            DMA("sp", cs_d[which], t2[0:32, :], reads=[Rt2], pwrites=[R["cs"]])
        sc.barrier_all()

        def load_small(l):
            DMA("sp", small, small_in[l], writes=[Rsmall])

        SM = dict(gmix=0, gffn=16, convw=32, convb=64, gmh=72, lam=80, gsub=84, ib=86, fb=87, nfb=88)

        def norm_phase(x_d, Rx, goff):
            wk.reset()
            xt = [wk.alloc(D, F32) for _ in range(2)]
            junk = wk.alloc(D, BF16)
            hb = [wk.alloc(D, BF16) for _ in range(2)]
            hs = [wk.alloc(KCD * 512, BF16).rearrange("p (c t) -> p c t", t=512) for _ in range(2)]
            st = wk.alloc(4 * TB, F32)
            Rxt = [Res(), Res()]
            Rjunk = Res()
            Rhb = [Res(), Res()]
            Rhs = [Res(), Res()]
            Rst = [Res(), Res()]
            hTv = hT_d.rearrange("c p s -> p c s")
            for tb in range(TB):
                s2 = tb % 2
                DMA("sp", xt[s2], x_d[tb * 128:(tb + 1) * 128, :], reads=[Rx], writes=[Rxt[s2]])
                ss = st[:, 4 * tb:4 * tb + 1]
                ms = st[:, 4 * tb + 1:4 * tb + 2]
                rstd = st[:, 4 * tb + 2:4 * tb + 3]
                ACT(junk, xt[s2], AF.Square, reads=[Rxt[s2]], writes=[Rst[s2]], accum_out=ss)
                TS("dve", ms, ss, 1.0 / D, EPS, ALU.mult, ALU.add, reads=[Rst[s2]], writes=[Rst[s2]])
                ACT(ms, ms, AF.Ln, reads=[Rst[s2]], writes=[Rst[s2]])
                ACT(rstd, ms, AF.Exp, reads=[Rst[s2]], writes=[Rst[s2]], scale=-0.5)
                TS("dve", hb[s2], xt[s2], rstd, None, ALU.mult, reads=[Rxt[s2], Rst[s2]], writes=[Rhb[s2]])
                stg = (tb // 4) % 2
                for half in range(2):
                    bi = (2 * tb + half) % 4
                    pt = bank(bi, BF16).rearrange("p (c t) -> p c t", t=128)
                    for j in range(8):
                        kc = half * 8 + j
                        PE(lambda o=pt[:, j, :], i=hb[s2][:, kc * 128:(kc + 1) * 128]: nc.tensor.transpose(o, i, ident_b),
                           reads=[Rhb[s2], Rcb], writes=[PSR[bi]])
                    for j in range(8):
                        kc = half * 8 + j
                        dst = hs[stg][:, kc, (tb % 4) * 128:(tb % 4 + 1) * 128]
                        gcol = small[:, goff + kc:goff + kc + 1]
                        if j % 2 == 0:
                            ACT(dst, pt[:, j, :], AF.Copy, reads=[PSR[bi], Rsmall], writes=[Rhs[stg]], scale=gcol)
                        else:
                            TS("dve", dst, pt[:, j, :], gcol, None, ALU.mult, reads=[PSR[bi], Rsmall], writes=[Rhs[stg]])
                if tb % 4 == 3:
                    tc = tb // 4
                    DMA("sp", hTv[:, :, tc * 512:(tc + 1) * 512], hs[stg], reads=[Rhs[stg]], pwrites=[R["hT"]])

        gemm_cache = {}

        def gemm_fm(groups, slabs, M, epi, out_fn, out_dt, STILE, wbufs=3):
            wk_mark = wk.off
            ST = min(STILE, S)
            NT = S // ST
            sig = ("fm", tuple((id(g[0]), g[2]) for g in groups), M, ST, wbufs)
            ck = (wk.gen, wk_mark)
            if ck in gemm_cache and gemm_cache[ck][0] == sig:
                _, inT, wsb, stage_raw = gemm_cache[ck]
            else:
                if ck in gemm_cache:
                    sc.barrier_all()
                inT = []
                shared = {}
                for (ind, Rin, KC, wfn) in groups:
                    if id(ind) in shared:
                        inT.append(shared[id(ind)])
                        continue
                    t = wk.alloc(KC * ST, BF16).rearrange("p (c t) -> p c t", t=ST)
                    inT.append((t, Res()))
                    shared[id(ind)] = inT[-1]
                wsb = []
                for (ind, Rin, KC, wfn) in groups:
                    wsb.append([(wk.alloc(KC * M, BF16).rearrange("p (c m) -> p c m", m=M), Res()) for _ in range(wbufs)])
                stage_raw = [(wk.alloc(ST, F32), Res()) for _ in range(2)]
                gemm_cache[ck] = (sig, inT, wsb, stage_raw)
            stage_f = stage_raw
            stage_b = [(a.bitcast(BF16)[:, 0:ST], r) for a, r in stage_raw]
            ng = len(groups)
            nbk = 6 // ng
            cnt = 0
            u = 0
            for st in range(NT):
                loaded = set()
                for gi_, (ind, Rin, KC, wfn) in enumerate(groups):
                    if id(ind) in loaded:
                        continue
                    loaded.add(id(ind))
                    t, Rt = inT[gi_]
                    src = ind.rearrange("c p s -> p c s")
                    step = 4
                    for k0 in range(0, KC, step):
                        k1 = min(KC, k0 + step)
                        DMA("sp", t[:, k0:k1, :], src[:, k0:k1, st * ST:(st + 1) * ST], reads=[Rin], pwrites=[Rt])
                for slab in slabs:
                    wv = []
                    for gi_, (ind, Rin, KC, wfn) in enumerate(groups):
                        wt, Rw = wsb[gi_][cnt % wbufs]
                        DMA("pool", wt, wfn(slab), writes=[Rw])
                        wv.append((wt, Rw))
                    odt = out_dt(slab) if callable(out_dt) else out_dt
                    sg, Rsg = (stage_f if odt == F32 else stage_b)[cnt % 2]
                    cnt += 1
                    for tc in range(ST // 512):
                        pss = []
                        for gi_, (ind, Rin, KC, wfn) in enumerate(groups):
                            bi = (u % nbk) * ng + gi_
                            ps = bank(bi)[0:M, :]
                            t, Rt = inT[gi_]
                            wt, Rw = wv[gi_]
                            for kc in range(KC):
                                MM(ps, wt[:, kc, :], t[:, kc, tc * 512:(tc + 1) * 512], kc == 0, kc == KC - 1,
                                   reads=[Rt, Rw], writes=[PSR[bi]])
                            pss.append((ps, PSR[bi]))
                        u += 1
                        epi(slab, st * ST + tc * 512, pss, sg[0:M, tc * 512:(tc + 1) * 512], Rsg)
                    outs = out_fn(slab)
                    if not isinstance(outs, list):
                        outs = [(outs[0], outs[1], slice(0, M))]
                    for od, Rod, rows in outs:
                        DMA("sp", od[:, st * ST:(st + 1) * ST], sg[rows, :], reads=[Rsg], pwrites=[Rod])
            wk.off = wk_mark

        def gemm_tm(ind, Rin, KC, wfn, nslab, WC, epi, STILE):
            wk_mark = wk.off
            ST = min(STILE, S)
            NT = S // ST
            sig = ("tm", id(ind), KC, WC, ST)
            ck = (wk.gen, wk_mark)
            if ck in gemm_cache and gemm_cache[ck][0] == sig:
                _, t, Rt, wsb = gemm_cache[ck]
            else:
                if ck in gemm_cache:
                    sc.barrier_all()
                t = wk.alloc(KC * ST, BF16).rearrange("p (c t) -> p c t", t=ST)
                Rt = Res()
                wsb = [(wk.alloc(KC * WC, BF16).rearrange("p (c m) -> p c m", m=WC), Res()) for _ in range(2)]
                gemm_cache[ck] = (sig, t, Rt, wsb)
            src = ind.rearrange("c p s -> p c s")
            cnt = 0
            u = 0
            for st in range(NT):
                for k0 in range(0, KC, 4):
                    k1 = min(KC, k0 + 4)
                    DMA("sp", t[:, k0:k1, :], src[:, k0:k1, st * ST:(st + 1) * ST], reads=[Rin], pwrites=[Rt])
                for slab in range(nslab):
                    wt, Rw = wsb[cnt % 2]
                    cnt += 1
                    wsrc = wfn(slab)
                    kstep = max(1, 2048 // WC)
                    for k0 in range(0, KC, kstep):
                        k1 = min(KC, k0 + kstep)
                        DMA("pool", wt[:, k0:k1, :], wsrc[:, k0:k1, :], pwrites=[Rw])
                    for tb in range(ST // 128):
                        bi = u % 6
                        u += 1
                        ps = bank(bi)[:, 0:WC]
                        for kc in range(KC):
                            MM(ps, t[:, kc, tb * 128:(tb + 1) * 128], wt[:, kc, :], kc == 0, kc == KC - 1,
                               reads=[Rt, Rw], writes=[PSR[bi]])
                        epi(slab, st * (ST // 128) + tb, ps, PSR[bi])
            wk.off = wk_mark

        def layer(l, x_d, Rx, xn1_d, Rxn1, xn2_d, Rxn2):
            lam_init = 0.8 - 0.6 * math.exp(-0.3 * l)
            load_small(l)
            lo = SM["lam"]
            TT("dve", lamt[:, 0:1], small[:, lo:lo + 1], small[:, lo + 1:lo + 2], ALU.mult, reads=[Rsmall], writes=[Rlam])
            TT("dve", lamt[:, 1:2], small[:, lo + 2:lo + 3], small[:, lo + 3:lo + 4], ALU.mult, reads=[Rsmall], writes=[Rlam])
            psl = bank(7)[:, 0:2]
            MM(psl, ones_f, lamt[:, 0:2], True, True, reads=[Rlam, Rcf], writes=[PSR[7]])
            ACT(lamt[:, 2:4], psl, AF.Exp, reads=[PSR[7]], writes=[Rlam])
            TT("dve", lamt[:, 4:5], lamt[:, 3:4], lamt[:, 2:3], ALU.subtract, reads=[Rlam], writes=[Rlam])
            TS("dve", lamt[:, 5:6], lamt[:, 4:5], -lam_init, None, ALU.add, reads=[Rlam], writes=[Rlam])
            nlam = lamt[:, 5:6]
            TS("dve", small[0:4, SM["nfb"]:SM["nfb"] + 1], small[0:4, SM["fb"]:SM["fb"] + 1], -1.0, None, ALU.mult,
               reads=[Rsmall], writes=[Rsmall])

            norm_phase(x_d, Rx, SM["gmix"])
            sc.barrier_all()
            if upto == "n1":
                return

            wk.reset()
            ectr = [0]
            vst = [(wk.alloc(512, BF16), Res()) for _ in range(3)]
            vctr = [0]

            def epi_copy(slab, t0, pss, dst, Rd):
                ps, Rp_ = pss[0]
                ectr[0] += 1
                if ectr[0] % 2:
                    ACT(dst, ps, AF.Copy, reads=[Rp_], writes=[Rd])
                else:
                    COPY("dve", dst, ps, reads=[Rp_], writes=[Rd])

            def epi_sig(slab, t0, pss, dst, Rd):
                ps, Rp_ = pss[0]
                ACT(dst, ps, AF.Sigmoid, reads=[Rp_], writes=[Rd])

            def epi_rope(slab, t0, pss, dst, Rd):
                ps, Rp_ = pss[0]
                ectr[0] += 1
                k = ectr[0] % 2
                rb, Rrb = ropeb[k]
                rt, Rrt = ropet[k]
                ACT(dst, ps, AF.Copy, reads=[Rp_], writes=[Rd])
                prf = bank(7)
                pr = prf[0:32, :]
                MM(prf, rt_b, dst, True, True, reads=[Rd, Rcb], writes=[PSR[7]])
                TT("dve", rt[0:32, 0:512], ps[0:32, :], cst[0:32, t0:t0 + 512], ALU.mult, reads=[Rp_, Rcs], writes=[Rrt])
                TT("dve", rt[0:32, 512:1024], pr, snt[0:32, t0:t0 + 512], ALU.mult, reads=[PSR[7], Rcs], writes=[Rrt])
                TT("dve", dst[0:32, :], rt[0:32, 0:512], rt[0:32, 512:1024], ALU.add, reads=[Rrt], writes=[Rd])

            def g1_w(sl):
                return wg_in[l, 0] if sl == 80 else wfm_in[l, sl]

            def g1_epi(sl, t0, pss, dst, Rd):
                if 8 <= sl < 16 or 48 <= sl < 80:
                    epi_sig(sl, t0, pss, dst, Rd)
                else:
                    epi_copy(sl, t0, pss, dst, Rd)

            def g1_out(sl):
                if sl < 8:
                    return (mqk_d[sl], R["mqk"])
                if sl < 16:
                    return (sgo_d[sl - 8], R["sgo"])
                if sl < 32:
                    return (aq_d[sl - 16], R["aq"])
                if sl < 48:
                    return (ak_d[sl - 32], R["ak"])
                if sl < 64:
                    return (sgm_d[sl - 48], R["sgm"])
                if sl < 80:
                    return (sga_d[sl - 64], R["sga"])
                return [(gi_d, R["gi"], slice(0, 4)), (gf_d, R["gf"], slice(32, 36))]

            def g1_dt(sl):
                return F32 if (sl < 16 or sl == 80) else BF16

            gemm_fm([(hT_d, R["hT"], KCD, g1_w)], list(range(81)), 128, g1_epi, g1_out, g1_dt, 2048)
            if upto == "g1d":
                return

            def epi_v(slab, tb, ps, Rp_):
                vctr[0] += 1
                sg, Rsg = vst[vctr[0] % 3]
                if vctr[0] % 2:
                    ACT(sg, ps, AF.Copy, reads=[Rp_], writes=[Rsg])
                else:
                    COPY("dve", sg, ps, reads=[Rp_], writes=[Rsg])
                if slab < 2:
                    DMA("sp", mv_d[tb * 128:(tb + 1) * 128, slab * 512:(slab + 1) * 512], sg, reads=[Rsg], pwrites=[R["mv"]])
                else:
                    DMA("sp", av_d[tb * 128:(tb + 1) * 128, (slab - 2) * 512:(slab - 1) * 512], sg, reads=[Rsg], pwrites=[R["av"]])

            gemm_tm(hT_d, R["hT"], KCD, lambda s: wtm_in[l, s], 6, 512, epi_v, 2048)
            sc.barrier_all()
            if upto == "g1":
                return

            wk.reset()
            t_i = wk.alloc(S, F32)
            t_f = wk.alloc(S, F32)
            t_b = wk.alloc(S, F32)
            egs = wk.alloc(4 * TB, F32)
            Rti_, Rtf_, Rtb_, Regs = Res(), Res(), Res(), Res()
            sc.op("pool", lambda: nc.gpsimd.memset(t_i, 0.0), writes=[Rti_])
            sc.op("pool", lambda: nc.gpsimd.memset(t_f, 0.0), writes=[Rtf_])
            DMA("sp", t_i[0:4, :], gi_d, reads=[R["gi"]], writes=[Rti_])
            DMA("sp", t_f[0:4, :], gf_d, reads=[R["gf"]], writes=[Rtf_])
            TS("dve", t_i[0:4, :], t_i[0:4, :], small[0:4, SM["ib"]:SM["ib"] + 1], None, ALU.add, reads=[Rti_, Rsmall], writes=[Rti_])
            ACT(t_f[0:4, :], t_f[0:4, :], AF.Exp, reads=[Rtf_, Rsmall], writes=[Rtf_], scale=-1.0,
                bias=small[0:4, SM["nfb"]:SM["nfb"] + 1])
            ACT(t_f[0:4, :], t_f[0:4, :], AF.Ln, reads=[Rtf_], writes=[Rtf_], bias=1.0)
            for c in range(TB):
                sc.op("dve", lambda c=c: nc.vector.tensor_tensor_scan(out=t_b[0:4, c * 128:(c + 1) * 128], data0=ones4,
                                                                     data1=t_f[0:4, c * 128:(c + 1) * 128], initial=0.0,
                                                                     op0=ALU.mult, op1=ALU.add),
                      reads=[Rtf_, Rcf], writes=[Rtb_])
            ACT(t_f[0:4, :], t_b[0:4, :], AF.Exp, reads=[Rtb_], writes=[Rtf_], scale=-1.0)
            TT("dve", t_i[0:4, :], t_i[0:4, :], t_b[0:4, :], ALU.add, reads=[Rti_, Rtb_], writes=[Rti_])
            ACT(t_i[0:4, :], t_i[0:4, :], AF.Exp, reads=[Rti_], writes=[Rti_], bias=-0.5 * math.log(128.0))
            if upto == "m0a":
                return
            xin_ = [(wk.alloc(S, F32), Res()) for _ in range(2)]
            acc_ = [(wk.alloc(S, F32), Res()) for _ in range(2)]
            ost_ = [(wk.alloc(S, BF16), Res()) for _ in range(2)]
            cw = SM["convw"]
            for slab in range(8):
                xi, Rxi = xin_[slab % 2]
                ac, Rac = acc_[slab % 2]
                os_, Ros = ost_[slab % 2]
                h = slab % 4
                DMA("sp", xi, mqk_d[slab], reads=[R["mqk"]], writes=[Rxi])
                w = lambda j: small[:, cw + slab * 4 + j:cw + slab * 4 + j + 1]
                TS("dve", ac, xi, w(3), None, ALU.mult, reads=[Rxi, Rsmall], writes=[Rac])
                for j in range(3):
                    sh = 3 - j
                    STT(ac[:, sh:S], xi[:, 0:S - sh], w(j), ac[:, sh:S], ALU.mult, ALU.add, reads=[Rxi, Rac, Rsmall], writes=[Rac])
                ACT(ac, ac, AF.Silu, reads=[Rac, Rsmall], writes=[Rac], bias=small[:, SM["convb"] + slab:SM["convb"] + slab + 1])
                if upto == "m0c":
                    DMA("sp", hraw_d[0], ac, reads=[Rac], pwrites=[R["hraw"]])
                    return
                gsrc, Rg = (t_f, Rtf_) if slab < 4 else (t_i, Rti_)
                for tc in range(TC):
                    bi = tc % 8
                    ps = bank(bi)
                    MM(ps, sel_f[:, h, :], gsrc[:, tc * 512:(tc + 1) * 512], True, True, reads=[Rg, Rcf], writes=[PSR[bi]])
                    TT("dve", os_[:, tc * 512:(tc + 1) * 512], ac[:, tc * 512:(tc + 1) * 512], ps, ALU.mult,
                       reads=[Rac, PSR[bi]], writes=[Ros])
                    if slab < 4:
                        src = ps.rearrange("p (c t) -> p c t", t=128)[:, :, 127]
                        ACT(egs[:, h * TB + tc * 4:h * TB + tc * 4 + 4], src, AF.Copy, reads=[PSR[bi]], writes=[Regs])
                DMA("sp", qkt_d[slab], os_, reads=[Ros], pwrites=[R["qkt"]])
                if upto == "m0b":
                    return
            DMA("sp", egb_d, egs, reads=[Regs], writes=[R["egb"]])
            sc.barrier_all()
            if upto == "m0":
                return

            wk.reset()
            egs1 = wk.alloc(4 * TB, F32)
            Regs1 = Res()
            DMA("sp", egs1, egb_d, reads=[R["egb"]], writes=[Regs1])
            hd = []
            for i in range(2):
                d_ = dict(
                    q=wk.alloc(S, BF16), k=wk.alloc(S, BF16),
                    v=wk.alloc(TB * 384, BF16).rearrange("p (c v) -> p c v", v=384),
                    C=wk.alloc(384, F32), Ch=wk.alloc(384, F32), Cb=wk.alloc(384, BF16),
                    kt=[wk.alloc(128, BF16) for _ in range(2)], sm=[wk.alloc(128, BF16) for _ in range(2)],
                    dm=[wk.alloc(128, F32) for _ in range(2)],
                    hst=[wk.alloc(1024, F32).rearrange("p (j t) -> p j t", t=512) for _ in range(2)],
                    Rq=Res(), Rk=Res(), Rv=Res(), RC=Res(), RCh=Res(), RCb=Res(),
                    Rkt=[Res(), Res()], Rsm=[Res(), Res()], Rdm=[Res(), Res()], Rhst=[Res(), Res()],
                )
                hd.append(d_)
                sc.op("pool", lambda v=d_["v"]: nc.gpsimd.memset(v[:, :, 256:384], 1.0), writes=[d_["Rv"]])
            for hp in range(2):
                for i in range(2):
                    h = hp * 2 + i
                    d_ = hd[i]
                    DMA("sp", d_["q"], qkt_d[h], reads=[R["qkt"]], writes=[d_["Rq"]])
                    DMA("sp", d_["k"], qkt_d[4 + h], reads=[R["qkt"]], writes=[d_["Rk"]])
                    for c0 in range(0, TB, 8):
                        DMA("sp", d_["v"][:, c0:c0 + 8, 0:256], mv_d.rearrange("(c p) v -> p c v", p=128)[:, c0:c0 + 8, h * 256:(h + 1) * 256],
                            reads=[R["mv"]], pwrites=[d_["Rv"]])
                    sc.op("dve", lambda C=d_["C"]: nc.vector.memset(C, 0.0), writes=[d_["RC"]])
                    sc.op("dve", lambda C=d_["Ch"]: nc.vector.memset(C, 0.0), writes=[d_["RCh"]])
                    sc.op("dve", lambda C=d_["Cb"]: nc.vector.memset(C, 0.0), writes=[d_["RCb"]])
                for c in range(TB):
                    for i in range(2):
                        h = hp * 2 + i
                        d_ = hd[i]
                        b0 = i * 4
                        cs_ = slice(c * 128, (c + 1) * 128)
                        p2 = c % 2
                        psK = bank(b0 + 0, BF16)[:, 0:128]
                        psS = bank(b0 + 1)[:, 0:128]
                        psU = bank(b0 + 2)[:, 0:384]
                        psN = bank(b0 + 3)[:, 0:384].rearrange("p (j t) -> p j t", t=128)
                        PE(lambda o=psK, i_=d_["k"][:, cs_]: nc.tensor.transpose(o, i_, ident_b), reads=[d_["Rk"], Rcb], writes=[PSR[b0]])
                        ACT(d_["kt"][p2], psK, AF.Copy, reads=[PSR[b0]], writes=[d_["Rkt"][p2]])
                        MM(psS, d_["k"][:, cs_], d_["q"][:, cs_], True, True, reads=[d_["Rk"], d_["Rq"]], writes=[PSR[b0 + 1]])
                        TT("dve", d_["sm"][p2], psS, tri_b, ALU.mult, reads=[PSR[b0 + 1], Rcb], writes=[d_["Rsm"][p2]])
                        MM(psU, d_["kt"][p2], d_["v"][:, c, :], True, True, reads=[d_["Rkt"][p2], d_["Rv"]], writes=[PSR[b0 + 2]])
                        for j in range(3):
                            MM(psN[:, j, :], d_["v"][:, c, j * 128:(j + 1) * 128], d_["sm"][p2], True, False,
                               reads=[d_["Rv"], d_["Rsm"][p2]], writes=[PSR[b0 + 3]])
                            MM(psN[:, j, :], d_["Cb"][:, j * 128:(j + 1) * 128], d_["q"][:, cs_], False, True,
                               reads=[d_["RCb"], d_["Rq"]], writes=[PSR[b0 + 3]])
                        ACT(d_["dm"][p2], psN[:, 2, :], AF.Abs, reads=[PSR[b0 + 3]], writes=[d_["Rdm"][p2]])
                        TS("dve", d_["dm"][p2], d_["dm"][p2], 1.0, None, ALU.max, reads=[d_["Rdm"][p2]], writes=[d_["Rdm"][p2]])
                        sc.op("dve", lambda o=d_["dm"][p2]: nc.vector.reciprocal(out=o, in_=o), reads=[d_["Rdm"][p2]], writes=[d_["Rdm"][p2]])
                        hs_ = (c // 4) % 2
                        for j in range(2):
                            TT("dve", d_["hst"][hs_][:, j, (c % 4) * 128:(c % 4 + 1) * 128], psN[:, j, :], d_["dm"][p2], ALU.mult,
                               reads=[PSR[b0 + 3], d_["Rdm"][p2]], writes=[d_["Rhst"][hs_]])
                        if c % 4 == 3:
                            tc = c // 4
                            DMA("sp", hraw_d[2 * h:2 * h + 2].rearrange("j p s -> p j s")[:, :, tc * 512:(tc + 1) * 512], d_["hst"][hs_],
                                reads=[d_["Rhst"][hs_]], pwrites=[R["hraw"]])
                        eg = egs1[:, h * TB + c:h * TB + c + 1]
                        STT(d_["C"], psU, eg, d_["Ch"], ALU.mult, ALU.add, reads=[PSR[b0 + 2], Regs1, d_["RCh"]], writes=[d_["RC"]])
                        ACT(d_["Cb"], d_["C"], AF.Copy, reads=[d_["RC"]], writes=[d_["RCb"]])
                        if c + 1 < TB:
                            eg2 = egs1[:, h * TB + c + 1:h * TB + c + 2]
                            TS("pool", d_["Ch"], d_["C"], eg2, None, ALU.mult, reads=[d_["RC"], Regs1], writes=[d_["RCh"]])
            sc.barrier_all()
            if upto == "m1":
                return

            wk.reset()
            m2 = [dict(h=wk.alloc(1024, F32), g=wk.alloc(1024, F32), sq=wk.alloc(1024, F32), r=wk.alloc(512, F32),
                       o=wk.alloc(1024, BF16), Rh=Res(), Rg=Res(), Rsq=Res(), Rr=Res(), Ro=Res()) for _ in range(2)]
            u = 0
            for h in range(4):
                for tc in range(TC):
                    b_ = m2[u % 2]
                    bi = u % 4
                    u += 1
                    ts_ = slice(tc * 512, (tc + 1) * 512)
                    hv = b_["h"].rearrange("p (j t) -> p j t", t=512)
                    gv = b_["g"].rearrange("p (j t) -> p j t", t=512)
                    ov = b_["o"].rearrange("p (j t) -> p j t", t=512)
                    DMA("sp", hv, hraw_d[2 * h:2 * h + 2].rearrange("j p s -> p j s")[:, :, ts_], reads=[R["hraw"]], writes=[b_["Rh"]])
                    DMA("sp", gv, sgo_d[2 * h:2 * h + 2].rearrange("j p s -> p j s")[:, :, ts_], reads=[R["sgo"]], writes=[b_["Rg"]])
                    ACT(b_["sq"], b_["h"], AF.Square, reads=[b_["Rh"]], writes=[b_["Rsq"]])
                    ps = bank(bi)
                    MM(ps, ones_f, b_["sq"][:, 0:512], True, False, reads=[b_["Rsq"], Rcf], writes=[PSR[bi]])
                    MM(ps, ones_f, b_["sq"][:, 512:1024], False, True, reads=[b_["Rsq"], Rcf], writes=[PSR[bi]])
                    TS("dve", b_["r"], ps, 1.0 / 256, EPS, ALU.mult, ALU.add, reads=[PSR[bi]], writes=[b_["Rr"]])
                    ACT(b_["r"], b_["r"], AF.Ln, reads=[b_["Rr"]], writes=[b_["Rr"]])
                    ACT(b_["r"], b_["r"], AF.Exp, reads=[b_["Rr"]], writes=[b_["Rr"]], scale=-0.5)
                    for j in range(2):
                        gc = small[:, SM["gmh"] + 2 * h + j:SM["gmh"] + 2 * h + j + 1]
                        ACT(gv[:, j, :], gv[:, j, :], AF.Copy, reads=[b_["Rg"], Rsmall], writes=[b_["Rg"]], scale=gc)
                        TT("dve", hv[:, j, :], hv[:, j, :], b_["r"], ALU.mult, reads=[b_["Rh"], b_["Rr"]], writes=[b_["Rh"]])
                        TT("dve", ov[:, j, :], hv[:, j, :], gv[:, j, :], ALU.mult, reads=[b_["Rh"], b_["Rg"]], writes=[b_["Ro"]])
                    DMA("sp", hmT_d[2 * h:2 * h + 2].rearrange("j p s -> p j s")[:, :, ts_], ov, reads=[b_["Ro"]], pwrites=[R["hmT"]])
            sc.barrier_all()
            if upto == "m2":
                return

            wk.reset()
            ab = [dict(q=[wk.alloc(S, BF16) for _ in range(2)], k=[wk.alloc(S, BF16) for _ in range(2)],
                       v=wk.alloc(TB * 256, BF16).rearrange("p (c v) -> p c v", v=256), R=Res()) for _ in range(2)]
            pT = [(wk.alloc(512, BF16), Res()) for _ in range(4)]
            on = [(wk.alloc(1024, F32), Res()) for _ in range(2)]
            rs_ = (wk.alloc(512, F32), Res())
            ot = (wk.alloc(1024, F32), Res())
            sq = (wk.alloc(1024, F32), Res())
            rr = (wk.alloc(512, F32), Res())
            ost = [(wk.alloc(1024, BF16), Res()) for _ in range(2)]
            scale = 128.0 ** -0.5
            cst = wk.alloc(S, F32)
            snt = wk.alloc(S, F32)
            Rcs = Res()
            DMA("sp", cst[0:32, :], cs_d[0], reads=[R["cs"]], pwrites=[Rcs])
            DMA("sp", snt[0:32, :], cs_d[1], reads=[R["cs"]], pwrites=[Rcs])
            xs = wk.alloc(S, BF16)
            Rxs = Res()
            rt1 = wk.alloc(1024, F32)
            rt2 = wk.alloc(1024, F32)
            Rrt12 = Res()
            RC = min(1024, S)
            uu = 0
            oc = 0
            def head_thunks(hh):
                aa = ab[hh % 2]
                th = []
                for c in range(2):
                    th.append(lambda c=c: DMA("sp", aa["q"][c], aq_d[2 * hh + c], reads=[R["aq"]], pwrites=[aa["R"]]))
                    th.append(lambda c=c: DMA("sp", aa["k"][c], ak_d[2 * hh + c], reads=[R["ak"]], pwrites=[aa["R"]]))
                for c0 in range(0, TB, 8):
                    th.append(lambda c0=c0: DMA("sp", aa["v"][:, c0:c0 + 8, :],
                                                av_d.rearrange("(c p) v -> p c v", p=128)[:, c0:c0 + 8, hh * 256:(hh + 1) * 256],
                                                reads=[R["av"]], pwrites=[aa["R"]]))
                for c in range(2):
                    for (buf, src_d, Rsrc) in ((aa["q"][c], aq_d[2 * hh + c], R["aq"]), (aa["k"][c], ak_d[2 * hh + c], R["ak"])):
                        def ld(src_d=src_d, Rsrc=Rsrc):
                            DMA("sp", xs[0:16, :], src_d[16:32, :], reads=[Rsrc], pwrites=[Rxs])
                            DMA("sp", xs[16:32, :], src_d[0:16, :], reads=[Rsrc], pwrites=[Rxs])
                        th.append(ld)
                        for r0_ in range(0, S, RC):
                            def rp(buf=buf, sl=slice(r0_, r0_ + RC)):
                                TT("dve", rt1[0:32, 0:RC], buf[0:32, sl], cst[0:32, sl], ALU.mult, reads=[aa["R"], Rcs], writes=[Rrt12])
                                TT("dve", rt2[0:32, 0:RC], xs[0:32, sl], snt[0:32, sl], ALU.mult, reads=[Rxs, Rcs], writes=[Rrt12])
                                TT("dve", buf[0:32, sl], rt1[0:32, 0:RC], rt2[0:32, 0:RC], ALU.add, reads=[Rrt12], writes=[aa["R"]])
                            th.append(rp)
                return th

            for t_ in head_thunks(0):
                t_()
            for h in range(8):
                a_ = ab[h % 2]
                pend = head_thunks(h + 1) if h + 1 < 8 else []
                per_j = -(-len(pend) // TC) if pend else 0
                for j in range(TC):
                    for c in range(2):
                        psO = bank(3)
                        psO1 = bank(4)
                        psSum = bank(5)
                        nk = 4 * j + 4
                        units = []
                        for m in range(nk):
                            r0 = 128 * max(0, m - 4 * j)
                            units.append((m, r0))

                        def do_s(m, r0, idx):
                            bi = idx % 3
                            MM(bank(bi)[:, r0:512], a_["k"][c][:, m * 128:(m + 1) * 128], a_["q"][c][:, j * 512 + r0:(j + 1) * 512],
                               True, True, reads=[a_["R"]], writes=[PSR[bi]])
                            p_, Rp2 = pT[idx % 4]
                            ACT(p_[:, r0:512], bank(bi)[:, r0:512], AF.Exp, reads=[PSR[bi]], writes=[Rp2], scale=scale)
                            if m >= 4 * j:
                                TT("dve", p_[:, r0:r0 + 128], p_[:, r0:r0 + 128], tri_b, ALU.mult, reads=[Rp2, Rcb], writes=[Rp2])

                        def do_pv(m, r0, idx, first, last):
                            p_, Rp2 = pT[idx % 4]
                            MM(psO[:, r0:512], a_["v"][:, m, 0:128], p_[:, r0:512], first, last, reads=[a_["R"], Rp2], writes=[PSR[3]])
                            MM(psO1[:, r0:512], a_["v"][:, m, 128:256], p_[:, r0:512], first, last, reads=[a_["R"], Rp2], writes=[PSR[4]])
                            MM(psSum[:, r0:512], ones_b, p_[:, r0:512], first, last, reads=[Rcb, Rp2], writes=[PSR[5]])

                        LA = 2
                        for ui in range(min(LA, nk)):
                            do_s(units[ui][0], units[ui][1], uu + ui)
                        for ui in range(nk):
                            if ui + LA < nk:
                                do_s(units[ui + LA][0], units[ui + LA][1], uu + ui + LA)
                            do_pv(units[ui][0], units[ui][1], uu + ui, ui == 0, ui == nk - 1)
                        uu += nk
                        rt_, Rrt_ = rs_
                        sc.op("dve", lambda o=rt_, i_=psSum: nc.vector.reciprocal(out=o, in_=i_), reads=[PSR[5]], writes=[Rrt_])
                        on_, Ron = on[c]
                        TT("dve", on_[:, 0:512], psO, rt_, ALU.mult, reads=[PSR[3], Rrt_], writes=[Ron])
                        TT("dve", on_[:, 512:1024], psO1, rt_, ALU.mult, reads=[PSR[4], Rrt_], writes=[Ron])
                    o_, Ro_ = ot
                    STT(o_, on[1][0], nlam, on[0][0], ALU.mult, ALU.add, reads=[on[0][1], on[1][1], Rlam], writes=[Ro_])
                    s_, Rs_ = sq
                    ACT(s_, o_, AF.Square, reads=[Ro_], writes=[Rs_])
                    psn = bank(6)
                    MM(psn, ones_f, s_[:, 0:512], True, False, reads=[Rs_, Rcf], writes=[PSR[6]])
                    MM(psn, ones_f, s_[:, 512:1024], False, True, reads=[Rs_, Rcf], writes=[PSR[6]])
                    r_, Rr_ = rr
                    TS("dve", r_, psn, 1.0 / 256, EPS, ALU.mult, ALU.add, reads=[PSR[6]], writes=[Rr_])
                    ACT(r_, r_, AF.Ln, reads=[Rr_], writes=[Rr_])
                    ACT(r_, r_, AF.Exp, reads=[Rr_], writes=[Rr_], scale=-0.5, bias=math.log(1.0 - lam_init))
                    os2, Ros2 = ost[oc % 2]
                    oc += 1
                    for jj in range(2):
                        gs = small[:, SM["gsub"] + jj:SM["gsub"] + jj + 1]
                        STT(os2[:, jj * 512:(jj + 1) * 512], o_[:, jj * 512:(jj + 1) * 512], gs, r_, ALU.mult, ALU.mult,
                            reads=[Ro_, Rr_, Rsmall], writes=[Ros2])
                    DMA("sp", haT_d[2 * h:2 * h + 2].rearrange("j p s -> p j s")[:, :, j * 512:(j + 1) * 512],
                        os2.rearrange("p (j t) -> p j t", t=512), reads=[Ros2], pwrites=[R["haT"]])
                    for t_ in pend[j * per_j:(j + 1) * per_j]:
                        t_()
            sc.barrier_all()
            if upto == "a":
                return

            wk.reset()
            ygm = [(wk.alloc(2048, BF16), Res()) for _ in range(2)]
            yga = [(wk.alloc(2048, BF16), Res()) for _ in range(2)]
            ytm = [(wk.alloc(512, F32), Res()) for _ in range(2)]
            ytm2 = [(wk.alloc(512, F32), Res()) for _ in range(2)]
            ycur = [None, None, 0]

            def epi_y(slab, t0, pss, dst, Rd):
                ST = min(2048, S)
                if t0 % ST == 0:
                    k = ycur[2] % 2
                    ycur[2] += 1
                    ycur[0] = ygm[k]
                    ycur[1] = yga[k]
                    DMA("act", ycur[0][0][:, 0:ST], sgm_d[slab][:, t0:t0 + ST], reads=[R["sgm"]], writes=[ycur[0][1]])
                    DMA("act", ycur[1][0][:, 0:ST], sga_d[slab][:, t0:t0 + ST], reads=[R["sga"]], writes=[ycur[1][1]])
                off = t0 % ST
                (pm_, Rpm), (pa_, Rpa) = pss
                k = (t0 // 512) % 2
                a1, Ra1 = ytm[k]
                a2, Ra2 = ytm2[k]
                TT("dve", a1, pm_, ycur[0][0][:, off:off + 512], ALU.mult, reads=[Rpm, ycur[0][1]], writes=[Ra1])
                TT("dve", a2, pa_, ycur[1][0][:, off:off + 512], ALU.mult, reads=[Rpa, ycur[1][1]], writes=[Ra2])
                TT("dve", dst, a1, a2, ALU.add, reads=[Ra1, Ra2], writes=[Rd])

            gemm_fm([(hmT_d, R["hmT"], 8, lambda s: pm_in[l, s]), (haT_d, R["haT"], 16, lambda s: pa_in[l, s])],
                    range(16), 128, epi_y, lambda s: (yT_d[s], R["yT"]), BF16, 2048, wbufs=2)
            sc.barrier_all()
            if upto == "y":
                return

            wk.reset()
            xr = [(wk.alloc(512, F32), Res()) for _ in range(3)]
            octr = [0]

            def mk_epi_res(xs_d, Rxs, xd_d, Rxd, WC):
                def epi(slab, tb, ps, Rp_):
                    octr[0] += 1
                    xt_, Rxt_ = xr[octr[0] % 3]
                    rows = slice(tb * 128, (tb + 1) * 128)
                    cols = slice(slab * WC, (slab + 1) * WC)
                    DMA("act", xt_[:, 0:WC], xs_d[rows, cols], reads=[Rxs], writes=[Rxt_])
                    TT("dve", xt_[:, 0:WC], ps, xt_[:, 0:WC], ALU.add, reads=[Rp_, Rxt_], writes=[Rxt_])
                    DMA("sp", xd_d[rows, cols], xt_[:, 0:WC], reads=[Rxt_], pwrites=[Rxd])
                return epi

            gemm_tm(yT_d, R["yT"], KCD, lambda s: wo_in[l, s], 4, 512, mk_epi_res(x_d, Rx, xn1_d, Rxn1, 512), 2048)
            sc.barrier_all()
            if upto == "o":
                return

            norm_phase(xn1_d, Rxn1, SM["gffn"])
            sc.barrier_all()
            wk.reset()
            fs = [(wk.alloc(512, F32), Res()) for _ in range(2)]
            fctr = [0]

            def epi_ffn(slab, t0, pss, dst, Rd):
                (pg, Rpg), (pu, Rpu) = pss
                fctr[0] += 1
                s_, Rs_ = fs[fctr[0] % 2]
                ACT(s_, pg, AF.Silu, reads=[Rpg], writes=[Rs_])
                TT("dve", dst, pu, s_, ALU.mult, reads=[Rpu, Rs_], writes=[Rd])

            gemm_fm([(hT_d, R["hT"], KCD, lambda s: wga_in[l, s]), (hT_d, R["hT"], KCD, lambda s: wup_in[l, s])],
                    range(44), 128, epi_ffn, lambda s: (actT_d[s], R["actT"]), BF16, 2048, wbufs=2)
            sc.barrier_all()
            if upto == "f1":
                return
            wk.reset()
            xr[:] = [(wk.alloc(512, F32), Res()) for _ in range(3)]
            gemm_tm(actT_d, R["actT"], KCF, lambda s: wdn_in[l, s], 8, 256, mk_epi_res(xn1_d, Rxn1, xn2_d, Rxn2, 256), 1024)
            sc.barrier_all()

        cur, Rcur = x_in, R["xin"]
        done = False
        for l in range(depth):
            layer(l, cur, Rcur, xa_d, R["xa"], xb_d, R["xb"])
            cur, Rcur = xb_d, R["xb"]
            if upto is not None:
                done = True
                break
        if not done:
            wk.reset()
            gf_ = wk.alloc(D, F32)
            Rgf = Res()
            DMA("sp", gf_, gfin_in, writes=[Rgf])
            xt = [(wk.alloc(D, F32), Res()) for _ in range(2)]
            junk = (wk.alloc(D, BF16), Res())
            yo = [(wk.alloc(D, F32), Res()) for _ in range(2)]
            st = wk.alloc(4 * TB, F32)
            Rst = [Res(), Res()]
            for tb in range(TB):
                s2 = tb % 2
                x_, Rx_ = xt[s2]
                DMA("sp", x_, cur[tb * 128:(tb + 1) * 128, :], reads=[Rcur], writes=[Rx_])
                ss = st[:, 4 * tb:4 * tb + 1]
                ms = st[:, 4 * tb + 1:4 * tb + 2]
                rstd = st[:, 4 * tb + 2:4 * tb + 3]
                ACT(junk[0], x_, AF.Square, reads=[Rx_], writes=[Rst[s2]], accum_out=ss)
                TS("dve", ms, ss, 1.0 / D, EPS, ALU.mult, ALU.add, reads=[Rst[s2]], writes=[Rst[s2]])
                ACT(ms, ms, AF.Ln, reads=[Rst[s2]], writes=[Rst[s2]])
                ACT(rstd, ms, AF.Exp, reads=[Rst[s2]], writes=[Rst[s2]], scale=-0.5)
                y_, Ry_ = yo[s2]
                STT(y_, x_, rstd, gf_, ALU.mult, ALU.mult, reads=[Rx_, Rst[s2], Rgf], writes=[Ry_])
                DMA("sp", out_d[tb * 128:(tb + 1) * 128, :], y_, reads=[Ry_], pwrites=[R["out"]])

        block = es.enter_context(nc.Block())
        sc.emit(block)
    return nc, dbg_outs


def fm_slabs(w, ncol):
    L, K, N = w.shape
    return np.ascontiguousarray(w.reshape(L, K // 128, 128, N // ncol, ncol).transpose(0, 3, 2, 1, 4))


def host_consts():
    cf = np.zeros((128, 1024), np.float32)
    cf[:, 0:128] = 1.0
    for h in range(4):
        cf[h, 128 + h * 128:128 + (h + 1) * 128] = 1.0
    half = 16
    inv = np.power(np.float32(ROPE_THETA), -np.arange(half, dtype=np.float32) * np.float32(2.0 / 32)).astype(np.float32)
    cf[0:16, 640] = inv / np.float32(2 * math.pi)
    cf[16:32, 640] = inv / np.float32(2 * math.pi)
    cf[:, 641] = -0.5
    cf[:, 642] = 0.25
    cf[0:4, 768:896] = 1.0
    cb = np.zeros((128, 512), np.float32)
    cb[:, 0:128] = np.eye(128)
    cb[:, 128:256] = 1.0
    cb[:, 256:384] = (np.arange(128)[None, :] >= np.arange(128)[:, None])
    rt = np.zeros((32, 32), np.float32)
    for m in range(32):
        rt[(m + 16) % 32, m] = 1.0
    cb[0:32, 384:416] = rt
    return cf, cb.astype(ml_dtypes.bfloat16)


def host_layout(inp, depth):
    w_in = np.asarray(inp["w_in"])[:depth]
    o = {}
    c = lambda a, b: w_in[:, :, a:b]
    fam = [c(0, 1024), c(2048, 3072), c(3080, 5128), c(5128, 7176), c(9224, 11272), c(11272, 13320)]
    o["wfm"] = np.concatenate([fm_slabs(f, 128) for f in fam], axis=1)
    wg = np.zeros((depth, 1, 128, KCD, 128), np.float32)
    wg[:, 0, :, :, 0:4] = fm_slabs(c(3072, 3076), 4)[:, 0]
    wg[:, 0, :, :, 32:36] = fm_slabs(c(3076, 3080), 4)[:, 0]
    o["wg"] = wg
    o["wtm"] = np.concatenate([fm_slabs(c(1024, 2048), 512), fm_slabs(c(7176, 9224), 512)], axis=1)
    o["pm"] = fm_slabs(np.asarray(inp["p_m"])[:depth], 128)
    o["pa"] = fm_slabs(np.asarray(inp["p_a"])[:depth], 128)
    o["wo"] = fm_slabs(np.asarray(inp["w_out"])[:depth], 512)
    o["wga"] = fm_slabs(np.asarray(inp["w_gate"])[:depth], 128)
    o["wup"] = fm_slabs(np.asarray(inp["w_up"])[:depth], 128)
    o["wdn"] = fm_slabs(np.asarray(inp["w_down"])[:depth], 256)
    small = np.zeros((depth, 128, NSMALL), np.float32)
    g = lambda k: np.asarray(inp[k])[:depth]
    small[:, :, 0:16] = g("g_mix").reshape(depth, 16, 128).transpose(0, 2, 1)
    small[:, :, 16:32] = g("g_ffn").reshape(depth, 16, 128).transpose(0, 2, 1)
    small[:, :, 32:64] = g("conv_w").reshape(depth, 4, 8, 128).transpose(0, 3, 2, 1).reshape(depth, 128, 32)
    small[:, :, 64:72] = g("conv_b").reshape(depth, 8, 128).transpose(0, 2, 1)
    small[:, :, 72:80] = g("g_mhead").reshape(depth, 8, 128).transpose(0, 2, 1)
    small[:, :, 80] = g("lambda_q1")
    small[:, :, 81] = g("lambda_k1")
    small[:, :, 82] = g("lambda_q2")
    small[:, :, 83] = g("lambda_k2")
    small[:, :, 84:86] = g("g_sub").reshape(depth, 2, 128).transpose(0, 2, 1)
    small[:, 0:4, 86] = g("i_bias")
    small[:, 0:4, 87] = g("f_bias")
    o["small"] = small
    o["gfin"] = np.ascontiguousarray(np.broadcast_to(np.asarray(inp["g_final"])[None, :], (128, D)))
    cf, cb = host_consts()
    o["cf"] = cf
    o["cb"] = cb
    return o


_CACHE = {}


def kernel(**inputs):
    x = np.asarray(inputs["x"])
    pos = np.asarray(inputs["positions"]).astype(np.int32)
    B, S, _ = x.shape
    shared = host_layout(inputs, DEPTH)
    key = (S, DEPTH)
    if key not in _CACHE:
        _CACHE[key] = build(S, DEPTH)[0]
    nc = _CACHE[key]
    in_maps = []
    for b in range(B):
        m = dict(shared)
        m["x"] = np.ascontiguousarray(x[b])
        m["pos"] = np.ascontiguousarray(pos[b:b + 1])
        in_maps.append(m)
    res = run_bass_kernel_spmd(nc, in_maps, core_ids=list(range(B)))
    return np.stack([np.asarray(r["out"]) for r in res.results], axis=0).astype(np.float32)
```

```python
import math
from contextlib import ExitStack

import numpy as np
import ml_dtypes

import concourse.bass as bass
import concourse.mybir as mybir
from concourse.bass_utils import run_bass_kernel_spmd

F32 = mybir.dt.float32
BF16 = mybir.dt.bfloat16
I32 = mybir.dt.int32
U8 = mybir.dt.uint8
AF = mybir.ActivationFunctionType
ALU = mybir.AluOpType

D = 2048
KCD = 16
DFF = 5632
KCF = 44
DEPTH = 4
SEQ = 4096
EPS = 1e-6
NSMALL = 96
ROPE_THETA = 500000.0


class Op:
    __slots__ = ("eng", "fn", "deps", "needed", "sigval", "dma", "sem", "val", "slot")


class OpSet:
    __slots__ = ("d",)

    def __init__(self):
        self.d = {}

    def add(self, o):
        self.d[(o.eng, o.slot) if o.dma else o.eng] = o

    def ops(self):
        return self.d.values()

    def __bool__(self):
        return bool(self.d)


class Res:
    __slots__ = ("ws", "rs", "prs", "name")

    def __init__(self, name=""):
        self.ws = OpSet()
        self.rs = OpSet()
        self.prs = OpSet()
        self.name = name


class Sched:
    ENGS = ("pe", "act", "dve", "pool", "sp")
    QUEUES = ("sp", "pool", "act")

    def __init__(self, nc, es, K=4):
        self.nc = nc
        self.K = K
        self.streams = {e: [] for e in self.ENGS}
        self.engsem = {e: es.enter_context(nc.semaphore("sem_" + e)) for e in self.ENGS}
        self.ring = {q: [es.enter_context(nc.semaphore("ring_%s_%d" % (q, i))) for i in range(K)] for q in self.QUEUES}
        self.ndma = {q: 0 for q in self.QUEUES}
        self.ringlast = {q: [None] * K for q in self.QUEUES}
        self.lastc = {e: None for e in self.ENGS}
        self.barrier = {}

    def op(self, eng, fn, reads=(), writes=(), pwrites=(), dma=False):
        o = Op()
        o.eng = eng
        o.fn = fn
        o.dma = dma
        o.needed = False
        o.sigval = None
        o.slot = None
        o.sem = None
        o.val = None
        deps = set()
        if eng in self.barrier:
            deps |= self.barrier.pop(eng)
        if dma:
            n = self.ndma[eng]
            self.ndma[eng] = n + 1
            slot = n % self.K
            o.slot = slot
            o.sem = self.ring[eng][slot]
            o.val = 16 * (n // self.K + 1)
            prev = self.ringlast[eng][slot]
            if prev is not None:
                deps.add(prev)
            self.ringlast[eng][slot] = o
        for r in reads:
            deps.update(r.ws.ops())
        for w in writes:
            deps.update(w.ws.ops())
            deps.update(w.rs.ops())
            if not w.rs:
                deps.update(w.prs.ops())
        for w in pwrites:
            if w.rs:
                deps.update(w.rs.ops())
            else:
                deps.update(w.prs.ops())
        if eng == "pe":
            deps = {d for d in deps if d.dma or d.eng != "pe"}
        deps.discard(o)
        for d in deps:
            if not d.dma:
                d.needed = True
        o.deps = deps
        for r in reads:
            r.rs.add(o)
        for w in writes:
            if w.rs:
                w.prs = w.rs
                w.rs = OpSet()
            w.ws = OpSet()
            w.ws.add(o)
        for w in pwrites:
            if w.rs:
                w.prs = w.rs
                w.rs = OpSet()
                w.ws = OpSet()
            w.ws.add(o)
        self.streams[eng].append(o)
        if not dma:
            self.lastc[eng] = o
        return o

    def barrier_all(self):
        b = set()
        for e, o in self.lastc.items():
            if o is not None:
                b.add(o)
        for q in self.QUEUES:
            for o in self.ringlast[q]:
                if o is not None:
                    b.add(o)
        for e in self.ENGS:
            self.barrier[e] = set(b) | self.barrier.get(e, set())

    def emit(self, block):
        nc = self.nc
        for e, st in self.streams.items():
            c = 0
            for o in st:
                if (not o.dma) and o.needed:
                    c += 1
                    o.sigval = c
        engmap = {"pe": block.tensor, "act": block.scalar, "dve": block.vector, "pool": block.gpsimd, "sp": block.sync}
        nceng = {"pe": nc.tensor, "act": nc.scalar, "dve": nc.vector, "pool": nc.gpsimd, "sp": nc.sync}
        finals = []
        for q in self.QUEUES:
            for o in self.ringlast[q]:
                if o is not None:
                    finals.append((o.sem, o.val))
        engsem = self.engsem

        def mk(e, st):
            def body(_e):
                eng = nceng[e]
                known = {}
                for o in st:
                    waits = {}
                    for d in o.deps:
                        if d.dma:
                            sem, v = d.sem, d.val
                        else:
                            sem, v = engsem[d.eng], d.sigval
                        k = id(sem)
                        if k not in waits or waits[k][1] < v:
                            waits[k] = (sem, v)
                    for k, (sem, v) in waits.items():
                        if known.get(k, 0) < v:
                            eng.wait_ge(sem, v)
                            known[k] = v
                    ins = o.fn()
                    if o.dma:
                        ins.then_inc(o.sem, 16)
                    elif o.needed:
                        ins.then_inc(engsem[e], 1)
                if e == "sp":
                    for sem, v in finals:
                        eng.wait_ge(sem, v)
            return body

        for e, st in self.streams.items():
            engmap[e](mk(e, st))


class Carver:
    def __init__(self, base, size, start=0):
        self.base = base
        self.size = size
        self.start = start
        self.off = start
        self.gen = 0

    def reset(self):
        self.off = self.start
        self.gen += 1

    def alloc(self, n, dt):
        nb = {F32: 4, BF16: 2, I32: 4, U8: 1}[dt]
        off = (self.off + 63) // 64 * 64
        assert off + n * nb <= self.size, ("SBUF overflow", off, n * nb, self.size)
        self.off = off + n * nb
        return self.base[:, off:off + n * nb].bitcast(dt)


def build(S, depth, debug=False, upto=None):
    nc = bass.Bass("TRN2", target_bir_lowering=False)
    TB = S // 128
    TC = S // 512
    dbg_outs = []

    def din(name, shape, dt):
        return nc.dram_tensor(name, list(shape), dt, kind="ExternalInput").ap()

    def dscr(name, shape, dt):
        if debug:
            dbg_outs.append(name)
            return nc.dram_tensor(name, list(shape), dt, kind="ExternalOutput").ap()
        return nc.dram_tensor(name, list(shape), dt, kind="Internal").ap()

    x_in = din("x", [S, D], F32)
    pos_in = din("pos", [1, S], I32)
    small_in = din("small", [depth, 128, NSMALL], F32)
    gfin_in = din("gfin", [128, D], F32)
    cf_in = din("cf", [128, 1024], F32)
    cb_in = din("cb", [128, 512], BF16)
    wfm_in = din("wfm", [depth, 80, 128, KCD, 128], F32)
    wg_in = din("wg", [depth, 1, 128, KCD, 128], F32)
    wtm_in = din("wtm", [depth, 6, 128, KCD, 512], F32)
    pm_in = din("pm", [depth, 16, 128, 8, 128], F32)
    pa_in = din("pa", [depth, 16, 128, 16, 128], F32)
    wo_in = din("wo", [depth, 4, 128, KCD, 512], F32)
    wga_in = din("wga", [depth, 44, 128, KCD, 128], F32)
    wup_in = din("wup", [depth, 44, 128, KCD, 128], F32)
    wdn_in = din("wdn", [depth, 8, 128, KCF, 256], F32)
    out_d = nc.dram_tensor("out", [S, D], F32, kind="ExternalOutput").ap()

    hT_d = dscr("hT", [KCD, 128, S], BF16)
    mqk_d = dscr("mqk", [8, 128, S], F32)
    sgo_d = dscr("sgo", [8, 128, S], F32)
    gi_d = dscr("gi", [4, S], F32)
    gf_d = dscr("gf", [4, S], F32)
    aq_d = dscr("aq", [16, 128, S], BF16)
    ak_d = dscr("ak", [16, 128, S], BF16)
    sgm_d = dscr("sgm", [16, 128, S], BF16)
    sga_d = dscr("sga", [16, 128, S], BF16)
    mv_d = dscr("mv", [S, 1024], BF16)
    av_d = dscr("av", [S, 2048], BF16)
    qkt_d = dscr("qkt", [8, 128, S], BF16)
    egb_d = dscr("egb", [128, 4 * TB], F32)
    hraw_d = dscr("hraw", [8, 128, S], F32)
    hmT_d = dscr("hmT", [8, 128, S], BF16)
    haT_d = dscr("haT", [16, 128, S], BF16)
    yT_d = dscr("yT", [16, 128, S], BF16)
    actT_d = dscr("actT", [KCF, 128, S], BF16)
    xa_d = dscr("xa", [S, D], F32)
    xb_d = dscr("xb", [S, D], F32)
    cs_d = dscr("cs", [2, 32, S], F32)

    R = {}
    for n in ("hT", "mqk", "sgo", "gi", "gf", "aq", "ak", "sgm", "sga", "mv", "av", "qkt", "egb", "hraw", "hmT", "haT",
              "yT", "actT", "xa", "xb", "cs", "out", "xin"):
        R[n] = Res(n)

    es = ExitStack()
    with es:
        sb = es.enter_context(nc.sbuf_tensor("sb", [128, 196608], U8))
        pst = es.enter_context(nc.psum_tensor("ps", [128, 16384], U8))
        sc = Sched(nc, es, K=4)

        def bank(i, dt=F32):
            return pst[:, i * 2048:(i + 1) * 2048].bitcast(dt)

        PSR = [Res("ps%d" % i) for i in range(8)]

        PERS = 12288
        pc = Carver(sb, PERS, 0)
        wk = Carver(sb, 196608, PERS)

        def DMA(q, out, in_, reads=(), writes=(), pwrites=()):
            eng = {"sp": nc.sync, "pool": nc.gpsimd, "act": nc.scalar}[q]
            n = out.shape[-1]
            if n > 2048 and tuple(out.shape) == tuple(in_.shape):
                r = None
                for c0 in range(0, n, 2048):
                    c1 = min(n, c0 + 2048)
                    idx = tuple([slice(None)] * (len(out.shape) - 1) + [slice(c0, c1)])
                    o_, i_ = out[idx], in_[idx]
                    r = sc.op(q, lambda o_=o_, i_=i_: eng.dma_start(out=o_, in_=i_), reads, writes, pwrites, dma=True)
                return r
            return sc.op(q, lambda: eng.dma_start(out=out, in_=in_), reads, writes, pwrites, dma=True)

        def PE(fn, reads=(), writes=()):
            return sc.op("pe", fn, reads, writes)

        def MM(out, lhsT, rhs, start, stop, reads=(), writes=()):
            return sc.op("pe", lambda: nc.tensor.matmul(out, lhsT, rhs, start=start, stop=stop), reads, writes)

        def ACT(out, in_, func, reads=(), writes=(), bias=None, scale=None, accum_out=None):
            kw = {}
            if bias is not None:
                kw["bias"] = bias
            if scale is not None:
                kw["scale"] = scale
            if accum_out is not None:
                kw["accum_out"] = accum_out
            return sc.op("act", lambda: nc.scalar.activation(out=out, in_=in_, func=func, **kw), reads, writes)

        def TS(eng, out, in0, s1, s2, op0, op1=None, reads=(), writes=()):
            e = {"dve": nc.vector, "pool": nc.gpsimd}[eng]
            if op1 is None:
                return sc.op(eng, lambda: e.tensor_scalar(out=out, in0=in0, scalar1=s1, scalar2=None, op0=op0), reads, writes)
            return sc.op(eng, lambda: e.tensor_scalar(out=out, in0=in0, scalar1=s1, scalar2=s2, op0=op0, op1=op1), reads, writes)

        def TT(eng, out, in0, in1, op, reads=(), writes=()):
            e = {"dve": nc.vector, "pool": nc.gpsimd}[eng]
            return sc.op(eng, lambda: e.tensor_tensor(out=out, in0=in0, in1=in1, op=op), reads, writes)

        def STT(out, in0, scalar, in1, op0, op1, reads=(), writes=()):
            return sc.op("dve", lambda: nc.vector.scalar_tensor_tensor(out=out, in0=in0, scalar=scalar, in1=in1, op0=op0, op1=op1), reads, writes)

        def COPY(eng, out, in_, reads=(), writes=()):
            if eng == "act":
                return sc.op("act", lambda: nc.scalar.copy(out=out, in_=in_), reads, writes)
            e = {"dve": nc.vector, "pool": nc.gpsimd}[eng]
            return sc.op(eng, lambda: e.tensor_copy(out=out, in_=in_), reads, writes)

        cf = pc.alloc(1024, F32)
        cbt = pc.alloc(512, BF16)
        small = pc.alloc(NSMALL, F32)
        lamt = pc.alloc(8, F32)
        Rcf, Rcb, Rsmall, Rlam = Res("cf"), Res("cb"), Res("small"), Res("lam")
        DMA("sp", cf, cf_in, writes=[Rcf])
        DMA("sp", cbt, cb_in, writes=[Rcb])
        ones_f = cf[:, 0:128]
        sel_f = cf[:, 128:640].rearrange("p (h c) -> p h c", c=128)
        invf = cf[0:32, 640:641]
        mhalf = cf[:, 641:642]
        c025 = cf[0:32, 642:643]
        ones4 = cf[0:4, 768:896]
        ident_b = cbt[:, 0:128]
        ones_b = cbt[:, 128:256]
        tri_b = cbt[:, 256:384]
        rt_b = cbt[:, 384:512]

        wk.reset()
        posi = wk.alloc(S, I32)
        ang = wk.alloc(S, F32)
        t1 = wk.alloc(S, F32)
        t2 = wk.alloc(S, F32)
        ti = wk.alloc(S, I32)
        Rp, Ra, Rt1, Rt2, Rti = Res(), Res(), Res(), Res(), Res()
        DMA("sp", posi[0:32, :], pos_in.partition_broadcast(32), writes=[Rp])
        COPY("dve", ang[0:32, :], posi[0:32, :], reads=[Rp], writes=[Ra])
        TS("dve", ang[0:32, :], ang[0:32, :], invf, None, ALU.mult, reads=[Ra, Rcf], writes=[Ra])
        for which in (0, 1):
            if which == 0:
                TS("dve", t1[0:32, :], ang[0:32, :], 0.25, None, ALU.add, reads=[Ra], writes=[Rt1])
            else:
                COPY("dve", t1[0:32, :], ang[0:32, :], reads=[Ra], writes=[Rt1])
            COPY("dve", ti[0:32, :], t1[0:32, :], reads=[Rt1], writes=[Rti])
            COPY("dve", t2[0:32, :], ti[0:32, :], reads=[Rti], writes=[Rt2])
            TT("dve", t1[0:32, :], t1[0:32, :], t2[0:32, :], ALU.subtract, reads=[Rt1, Rt2], writes=[Rt1])
            TS("dve", t2[0:32, :], t1[0:32, :], 0.5, None, ALU.is_gt, reads=[Rt1], writes=[Rt2])
            TT("dve", t1[0:32, :], t1[0:32, :], t2[0:32, :], ALU.subtract, reads=[Rt1, Rt2], writes=[Rt1])
            TS("dve", t2[0:32, :], t1[0:32, :], -0.5, None, ALU.is_lt, reads=[Rt1], writes=[Rt2])
            TT("dve", t1[0:32, :], t1[0:32, :], t2[0:32, :], ALU.add, reads=[Rt1, Rt2], writes=[Rt1])
            ACT(t2[0:32, :], t1[0:32, :], AF.Sin, reads=[Rt1], writes=[Rt2], scale=6.28318)
            if which == 1:
                TS("dve", t2[0:16, :], t2[0:16, :], -1.0, None, ALU.mult, reads=[Rt2], writes=[Rt2])
            DMA("sp", cs_d[which], t2[0:32, :], reads=[Rt2], pwrites=[R["cs"]])
        sc.barrier_all()

        def load_small(l):
            DMA("sp", small, small_in[l], writes=[Rsmall])

        SM = dict(gmix=0, gffn=16, convw=32, convb=64, gmh=72, lam=80, gsub=84, ib=86, fb=87, nfb=88)

        def norm_phase(x_d, Rx, goff):
            wk.reset()
            NB = 3
            xt = [wk.alloc(D, F32) for _ in range(NB)]
            junk = wk.alloc(D, BF16)
            hb = [wk.alloc(D, BF16) for _ in range(NB)]
            hs = [wk.alloc(KCD * 512, BF16).rearrange("p (c t) -> p c t", t=512) for _ in range(2)]
            st = wk.alloc(4 * TB, F32)
            Rxt = [Res() for _ in range(NB)]
            Rhb = [Res() for _ in range(NB)]
            Rhs = [Res(), Res()]
            Rst = [Res() for _ in range(NB)]
            hTv = hT_d.rearrange("c p s -> p c s")

            def stage_a(tb):
                s2 = tb % NB
                DMA("sp", xt[s2], x_d[tb * 128:(tb + 1) * 128, :], reads=[Rx], writes=[Rxt[s2]])
                ss = st[:, 4 * tb:4 * tb + 1]
                ms = st[:, 4 * tb + 1:4 * tb + 2]
                rstd = st[:, 4 * tb + 2:4 * tb + 3]
                ACT(junk, xt[s2], AF.Square, reads=[Rxt[s2]], writes=[Rst[s2]], accum_out=ss)
                TS("dve", ms, ss, 1.0 / D, EPS, ALU.mult, ALU.add, reads=[Rst[s2]], writes=[Rst[s2]])
                ACT(ms, ms, AF.Ln, reads=[Rst[s2]], writes=[Rst[s2]])
                ACT(rstd, ms, AF.Exp, reads=[Rst[s2]], writes=[Rst[s2]], scale=-0.5)
                TS("dve", hb[s2], xt[s2], rstd, None, ALU.mult, reads=[Rxt[s2], Rst[s2]], writes=[Rhb[s2]])

            def stage_b(tb):
                s2 = tb % NB
                stg = (tb // 4) % 2
                for half in range(2):
                    bi = (2 * tb + half) % 4
                    pt = bank(bi, BF16).rearrange("p (c t) -> p c t", t=128)
                    for j in range(8):
                        kc = half * 8 + j
                        PE(lambda o=pt[:, j, :], i=hb[s2][:, kc * 128:(kc + 1) * 128]: nc.tensor.transpose(o, i, ident_b),
                           reads=[Rhb[s2], Rcb], writes=[PSR[bi]])
                    dst = hs[stg][:, half * 8:half * 8 + 8, (tb % 4) * 128:(tb % 4 + 1) * 128]
                    gb = small[:, goff + half * 8:goff + half * 8 + 8].unsqueeze(2).to_broadcast([128, 8, 128])
                    TT("dve", dst, pt, gb, ALU.mult, reads=[PSR[bi], Rsmall], writes=[Rhs[stg]])
                if tb % 4 == 3:
                    tc = tb // 4
                    DMA("sp", hTv[:, :, tc * 512:(tc + 1) * 512], hs[stg], reads=[Rhs[stg]], pwrites=[R["hT"]])

            stage_a(0)
            for tb in range(TB):
                if tb + 1 < TB:
                    stage_a(tb + 1)
                stage_b(tb)

        gemm_cache = {}

        def gemm_fm(groups, slabs, M, epi, out_fn, out_dt, STILE, wbufs=3):
            wk_mark = wk.off
            ST = min(STILE, S)
            NT = S // ST
            sig = ("fm", tuple((id(g[0]), g[2]) for g in groups), M, ST, wbufs)
            ck = (wk.gen, wk_mark)
            if ck in gemm_cache and gemm_cache[ck][0] == sig:
                _, inT, wsb, stage_raw = gemm_cache[ck]
            else:
                if ck in gemm_cache:
                    sc.barrier_all()
                inT = []
                shared = {}
                for (ind, Rin, KC, wfn) in groups:
                    if id(ind) in shared:
                        inT.append(shared[id(ind)])
                        continue
                    t = wk.alloc(KC * ST, BF16).rearrange("p (c t) -> p c t", t=ST)
                    inT.append((t, Res()))
                    shared[id(ind)] = inT[-1]
                wsb = []
                for (ind, Rin, KC, wfn) in groups:
                    wsb.append([(wk.alloc(KC * M, BF16).rearrange("p (c m) -> p c m", m=M), Res()) for _ in range(wbufs)])
                stage_raw = [(wk.alloc(ST, F32), Res()) for _ in range(2)]
                gemm_cache[ck] = (sig, inT, wsb, stage_raw)
            stage_f = stage_raw
            stage_b = [(a.bitcast(BF16)[:, 0:ST], r) for a, r in stage_raw]
            ng = len(groups)
            nbk = 6 // ng
            cnt = 0
            u = 0
            for st in range(NT):
                loaded = set()
                for gi_, (ind, Rin, KC, wfn) in enumerate(groups):
                    if id(ind) in loaded:
                        continue
                    loaded.add(id(ind))
                    t, Rt = inT[gi_]
                    src = ind.rearrange("c p s -> p c s")
                    step = 4
                    for k0 in range(0, KC, step):
                        k1 = min(KC, k0 + step)
                        DMA("sp", t[:, k0:k1, :], src[:, k0:k1, st * ST:(st + 1) * ST], reads=[Rin], pwrites=[Rt])
                for slab in slabs:
                    wv = []
                    for gi_, (ind, Rin, KC, wfn) in enumerate(groups):
                        wt, Rw = wsb[gi_][cnt % wbufs]
                        DMA("pool", wt, wfn(slab), writes=[Rw])
                        wv.append((wt, Rw))
                    odt = out_dt(slab) if callable(out_dt) else out_dt
                    sg, Rsg = (stage_f if odt == F32 else stage_b)[cnt % 2]
                    cnt += 1
                    for tc in range(ST // 512):
                        pss = []
                        for gi_, (ind, Rin, KC, wfn) in enumerate(groups):
                            bi = (u % nbk) * ng + gi_
                            ps = bank(bi)[0:M, :]
                            t, Rt = inT[gi_]
                            wt, Rw = wv[gi_]
                            for kc in range(KC):
                                MM(ps, wt[:, kc, :], t[:, kc, tc * 512:(tc + 1) * 512], kc == 0, kc == KC - 1,
                                   reads=[Rt, Rw], writes=[PSR[bi]])
                            pss.append((ps, PSR[bi]))
                        u += 1
                        epi(slab, st * ST + tc * 512, pss, sg[0:M, tc * 512:(tc + 1) * 512], Rsg)
                    outs = out_fn(slab)
                    if not isinstance(outs, list):
                        outs = [(outs[0], outs[1], slice(0, M))]
                    for od, Rod, rows in outs:
                        DMA("sp", od[:, st * ST:(st + 1) * ST], sg[rows, :], reads=[Rsg], pwrites=[Rod])
            wk.off = wk_mark

        def gemm_tm(ind, Rin, KC, wfn, nslab, WC, epi, STILE):
            wk_mark = wk.off
            ST = min(STILE, S)
            NT = S // ST
            sig = ("tm", id(ind), KC, WC, ST)
            ck = (wk.gen, wk_mark)
            if ck in gemm_cache and gemm_cache[ck][0] == sig:
                _, t, Rt, wsb = gemm_cache[ck]
            else:
                if ck in gemm_cache:
                    sc.barrier_all()
                t = wk.alloc(KC * ST, BF16).rearrange("p (c t) -> p c t", t=ST)
                Rt = Res()
                wsb = [(wk.alloc(KC * WC, BF16).rearrange("p (c m) -> p c m", m=WC), Res()) for _ in range(2)]
                gemm_cache[ck] = (sig, t, Rt, wsb)
            src = ind.rearrange("c p s -> p c s")
            cnt = 0
            u = 0
            for st in range(NT):
                for k0 in range(0, KC, 4):
                    k1 = min(KC, k0 + 4)
                    DMA("sp", t[:, k0:k1, :], src[:, k0:k1, st * ST:(st + 1) * ST], reads=[Rin], pwrites=[Rt])
                for slab in range(nslab):
                    wt, Rw = wsb[cnt % 2]
                    cnt += 1
                    wsrc = wfn(slab)
                    kstep = max(1, 2048 // WC)
                    for k0 in range(0, KC, kstep):
                        k1 = min(KC, k0 + kstep)
                        DMA("pool", wt[:, k0:k1, :], wsrc[:, k0:k1, :], pwrites=[Rw])
                    for tb in range(ST // 128):
                        bi = u % 6
                        u += 1
                        ps = bank(bi)[:, 0:WC]
                        for kc in range(KC):
                            MM(ps, t[:, kc, tb * 128:(tb + 1) * 128], wt[:, kc, :], kc == 0, kc == KC - 1,
                               reads=[Rt, Rw], writes=[PSR[bi]])
                        epi(slab, st * (ST // 128) + tb, ps, PSR[bi])
            wk.off = wk_mark

        def layer(l, x_d, Rx, xn1_d, Rxn1, xn2_d, Rxn2):
            lam_init = 0.8 - 0.6 * math.exp(-0.3 * l)
            load_small(l)
            lo = SM["lam"]
            TT("dve", lamt[:, 0:1], small[:, lo:lo + 1], small[:, lo + 1:lo + 2], ALU.mult, reads=[Rsmall], writes=[Rlam])
            TT("dve", lamt[:, 1:2], small[:, lo + 2:lo + 3], small[:, lo + 3:lo + 4], ALU.mult, reads=[Rsmall], writes=[Rlam])
            psl = bank(7)[:, 0:2]
            MM(psl, ones_f, lamt[:, 0:2], True, True, reads=[Rlam, Rcf], writes=[PSR[7]])
            ACT(lamt[:, 2:4], psl, AF.Exp, reads=[PSR[7]], writes=[Rlam])
            TT("dve", lamt[:, 4:5], lamt[:, 3:4], lamt[:, 2:3], ALU.subtract, reads=[Rlam], writes=[Rlam])
            TS("dve", lamt[:, 5:6], lamt[:, 4:5], -lam_init, None, ALU.add, reads=[Rlam], writes=[Rlam])
            nlam = lamt[:, 5:6]
            TS("dve", small[0:4, SM["nfb"]:SM["nfb"] + 1], small[0:4, SM["fb"]:SM["fb"] + 1], -1.0, None, ALU.mult,
               reads=[Rsmall], writes=[Rsmall])

            norm_phase(x_d, Rx, SM["gmix"])
            sc.barrier_all()
            if upto == "n1":
                return

            wk.reset()
            ectr = [0]
            vst = [(wk.alloc(512, BF16), Res()) for _ in range(3)]
            vctr = [0]

            def epi_copy(slab, t0, pss, dst, Rd):
                ps, Rp_ = pss[0]
                ectr[0] += 1
                if ectr[0] % 2:
                    ACT(dst, ps, AF.Copy, reads=[Rp_], writes=[Rd])
                else:
                    COPY("dve", dst, ps, reads=[Rp_], writes=[Rd])

            def epi_sig(slab, t0, pss, dst, Rd):
                ps, Rp_ = pss[0]
                ACT(dst, ps, AF.Sigmoid, reads=[Rp_], writes=[Rd])

            def epi_rope(slab, t0, pss, dst, Rd):
                ps, Rp_ = pss[0]
                ectr[0] += 1
                k = ectr[0] % 2
                rb, Rrb = ropeb[k]
                rt, Rrt = ropet[k]
                ACT(dst, ps, AF.Copy, reads=[Rp_], writes=[Rd])
                prf = bank(7)
                pr = prf[0:32, :]
                MM(prf, rt_b, dst, True, True, reads=[Rd, Rcb], writes=[PSR[7]])
                TT("dve", rt[0:32, 0:512], ps[0:32, :], cst[0:32, t0:t0 + 512], ALU.mult, reads=[Rp_, Rcs], writes=[Rrt])
                TT("dve", rt[0:32, 512:1024], pr, snt[0:32, t0:t0 + 512], ALU.mult, reads=[PSR[7], Rcs], writes=[Rrt])
                TT("dve", dst[0:32, :], rt[0:32, 0:512], rt[0:32, 512:1024], ALU.add, reads=[Rrt], writes=[Rd])

            def g1_w(sl):
                return wg_in[l, 0] if sl == 80 else wfm_in[l, sl]

            def g1_epi(sl, t0, pss, dst, Rd):
                if 8 <= sl < 16 or 48 <= sl < 80:
                    epi_sig(sl, t0, pss, dst, Rd)
                else:
                    epi_copy(sl, t0, pss, dst, Rd)

            def g1_out(sl):
                if sl < 8:
                    return (mqk_d[sl], R["mqk"])
                if sl < 16:
                    return (sgo_d[sl - 8], R["sgo"])
                if sl < 32:
                    return (aq_d[sl - 16], R["aq"])
                if sl < 48:
                    return (ak_d[sl - 32], R["ak"])
                if sl < 64:
                    return (sgm_d[sl - 48], R["sgm"])
                if sl < 80:
                    return (sga_d[sl - 64], R["sga"])
                return [(gi_d, R["gi"], slice(0, 4)), (gf_d, R["gf"], slice(32, 36))]

            def g1_dt(sl):
                return F32 if (sl < 16 or sl == 80) else BF16

            gemm_fm([(hT_d, R["hT"], KCD, g1_w)], list(range(81)), 128, g1_epi, g1_out, g1_dt, 2048)
            if upto == "g1d":
                return

            def epi_v(slab, tb, ps, Rp_):
                vctr[0] += 1
                sg, Rsg = vst[vctr[0] % 3]
                if vctr[0] % 2:
                    ACT(sg, ps, AF.Copy, reads=[Rp_], writes=[Rsg])
                else:
                    COPY("dve", sg, ps, reads=[Rp_], writes=[Rsg])
                if slab < 2:
                    DMA("sp", mv_d[tb * 128:(tb + 1) * 128, slab * 512:(slab + 1) * 512], sg, reads=[Rsg], pwrites=[R["mv"]])
                else:
                    DMA("sp", av_d[tb * 128:(tb + 1) * 128, (slab - 2) * 512:(slab - 1) * 512], sg, reads=[Rsg], pwrites=[R["av"]])

            gemm_tm(hT_d, R["hT"], KCD, lambda s: wtm_in[l, s], 6, 512, epi_v, 2048)
            sc.barrier_all()
            if upto == "g1":
                return

            wk.reset()
            t_i = wk.alloc(S, F32)
            t_f = wk.alloc(S, F32)
            t_b = wk.alloc(S, F32)
            egs = wk.alloc(4 * TB, F32)
            Rti_, Rtf_, Rtb_, Regs = Res(), Res(), Res(), Res()
            sc.op("pool", lambda: nc.gpsimd.memset(t_i, 0.0), writes=[Rti_])
            sc.op("pool", lambda: nc.gpsimd.memset(t_f, 0.0), writes=[Rtf_])
            DMA("sp", t_i[0:4, :], gi_d, reads=[R["gi"]], writes=[Rti_])
            DMA("sp", t_f[0:4, :], gf_d, reads=[R["gf"]], writes=[Rtf_])
            TS("dve", t_i[0:4, :], t_i[0:4, :], small[0:4, SM["ib"]:SM["ib"] + 1], None, ALU.add, reads=[Rti_, Rsmall], writes=[Rti_])
            ACT(t_f[0:4, :], t_f[0:4, :], AF.Exp, reads=[Rtf_, Rsmall], writes=[Rtf_], scale=-1.0,
                bias=small[0:4, SM["nfb"]:SM["nfb"] + 1])
            ACT(t_f[0:4, :], t_f[0:4, :], AF.Ln, reads=[Rtf_], writes=[Rtf_], bias=1.0)
            for c in range(TB):
                sc.op("dve", lambda c=c: nc.vector.tensor_tensor_scan(out=t_b[0:4, c * 128:(c + 1) * 128], data0=ones4,
                                                                     data1=t_f[0:4, c * 128:(c + 1) * 128], initial=0.0,
                                                                     op0=ALU.mult, op1=ALU.add),
                      reads=[Rtf_, Rcf], writes=[Rtb_])
            ACT(t_f[0:4, :], t_b[0:4, :], AF.Exp, reads=[Rtb_], writes=[Rtf_], scale=-1.0)
            TT("dve", t_i[0:4, :], t_i[0:4, :], t_b[0:4, :], ALU.add, reads=[Rti_, Rtb_], writes=[Rti_])
            ACT(t_i[0:4, :], t_i[0:4, :], AF.Exp, reads=[Rti_], writes=[Rti_], bias=-0.5 * math.log(128.0))
            if upto == "m0a":
                return
            xin_ = [(wk.alloc(S, F32), Res()) for _ in range(2)]
            acc_ = [(wk.alloc(S, F32), Res()) for _ in range(2)]
            ost_ = [(wk.alloc(S, BF16), Res()) for _ in range(2)]
            cw = SM["convw"]
            for slab in range(8):
                xi, Rxi = xin_[slab % 2]
                ac, Rac = acc_[slab % 2]
                os_, Ros = ost_[slab % 2]
                h = slab % 4
                DMA("sp", xi, mqk_d[slab], reads=[R["mqk"]], writes=[Rxi])
                w = lambda j: small[:, cw + slab * 4 + j:cw + slab * 4 + j + 1]
                TS("dve", ac, xi, w(3), None, ALU.mult, reads=[Rxi, Rsmall], writes=[Rac])
                for j in range(3):
                    sh = 3 - j
                    STT(ac[:, sh:S], xi[:, 0:S - sh], w(j), ac[:, sh:S], ALU.mult, ALU.add, reads=[Rxi, Rac, Rsmall], writes=[Rac])
                ACT(ac, ac, AF.Silu, reads=[Rac, Rsmall], writes=[Rac], bias=small[:, SM["convb"] + slab:SM["convb"] + slab + 1])
                if upto == "m0c":
                    DMA("sp", hraw_d[0], ac, reads=[Rac], pwrites=[R["hraw"]])
                    return
                gsrc, Rg = (t_f, Rtf_) if slab < 4 else (t_i, Rti_)
                for tc in range(TC):
                    bi = tc % 8
                    ps = bank(bi)
                    MM(ps, sel_f[:, h, :], gsrc[:, tc * 512:(tc + 1) * 512], True, True, reads=[Rg, Rcf], writes=[PSR[bi]])
                    TT("dve", os_[:, tc * 512:(tc + 1) * 512], ac[:, tc * 512:(tc + 1) * 512], ps, ALU.mult,
                       reads=[Rac, PSR[bi]], writes=[Ros])
                    if slab < 4:
                        src = ps.rearrange("p (c t) -> p c t", t=128)[:, :, 127]
                        ACT(egs[:, h * TB + tc * 4:h * TB + tc * 4 + 4], src, AF.Copy, reads=[PSR[bi]], writes=[Regs])
                DMA("sp", qkt_d[slab], os_, reads=[Ros], pwrites=[R["qkt"]])
                if upto == "m0b":
                    return
            DMA("sp", egb_d, egs, reads=[Regs], writes=[R["egb"]])
            sc.barrier_all()
            if upto == "m0":
                return

            wk.reset()
            egs1 = wk.alloc(4 * TB, F32)
            Regs1 = Res()
            DMA("sp", egs1, egb_d, reads=[R["egb"]], writes=[Regs1])
            hd = []
            for i in range(2):
                d_ = dict(
                    q=wk.alloc(S, BF16), k=wk.alloc(S, BF16),
                    v=wk.alloc(TB * 384, BF16).rearrange("p (c v) -> p c v", v=384),
                    C=wk.alloc(384, F32), Ch=wk.alloc(384, F32), Cb=wk.alloc(384, BF16),
                    kt=[wk.alloc(128, BF16) for _ in range(2)], sm=[wk.alloc(128, BF16) for _ in range(2)],
                    dm=[wk.alloc(128, F32) for _ in range(2)],
                    hst=[wk.alloc(1024, F32).rearrange("p (j t) -> p j t", t=512) for _ in range(2)],
                    Rq=Res(), Rk=Res(), Rv=Res(), RC=Res(), RCh=Res(), RCb=Res(),
                    Rkt=[Res(), Res()], Rsm=[Res(), Res()], Rdm=[Res(), Res()], Rhst=[Res(), Res()],
                )
                hd.append(d_)
                sc.op("pool", lambda v=d_["v"]: nc.gpsimd.memset(v[:, :, 256:384], 1.0), writes=[d_["Rv"]])
            for hp in range(2):
                for i in range(2):
                    h = hp * 2 + i
                    d_ = hd[i]
                    DMA("sp", d_["q"], qkt_d[h], reads=[R["qkt"]], writes=[d_["Rq"]])
                    DMA("sp", d_["k"], qkt_d[4 + h], reads=[R["qkt"]], writes=[d_["Rk"]])
                    for c0 in range(0, TB, 8):
                        DMA("sp", d_["v"][:, c0:c0 + 8, 0:256], mv_d.rearrange("(c p) v -> p c v", p=128)[:, c0:c0 + 8, h * 256:(h + 1) * 256],
                            reads=[R["mv"]], pwrites=[d_["Rv"]])
                    sc.op("dve", lambda C=d_["C"]: nc.vector.memset(C, 0.0), writes=[d_["RC"]])
                    sc.op("dve", lambda C=d_["Ch"]: nc.vector.memset(C, 0.0), writes=[d_["RCh"]])
                    sc.op("dve", lambda C=d_["Cb"]: nc.vector.memset(C, 0.0), writes=[d_["RCb"]])
                for c in range(TB):
                    for i in range(2):
                        h = hp * 2 + i
                        d_ = hd[i]
                        b0 = i * 4
                        cs_ = slice(c * 128, (c + 1) * 128)
                        p2 = c % 2
                        psK = bank(b0 + 0, BF16)[:, 0:128]
                        psS = bank(b0 + 1)[:, 0:128]
                        psU = bank(b0 + 2)[:, 0:384]
                        psN = bank(b0 + 3)[:, 0:384].rearrange("p (j t) -> p j t", t=128)
                        PE(lambda o=psK, i_=d_["k"][:, cs_]: nc.tensor.transpose(o, i_, ident_b), reads=[d_["Rk"], Rcb], writes=[PSR[b0]])
                        ACT(d_["kt"][p2], psK, AF.Copy, reads=[PSR[b0]], writes=[d_["Rkt"][p2]])
                        MM(psS, d_["k"][:, cs_], d_["q"][:, cs_], True, True, reads=[d_["Rk"], d_["Rq"]], writes=[PSR[b0 + 1]])
                        TT("dve", d_["sm"][p2], psS, tri_b, ALU.mult, reads=[PSR[b0 + 1], Rcb], writes=[d_["Rsm"][p2]])
                        MM(psU, d_["kt"][p2], d_["v"][:, c, :], True, True, reads=[d_["Rkt"][p2], d_["Rv"]], writes=[PSR[b0 + 2]])
                        for j in range(3):
                            MM(psN[:, j, :], d_["v"][:, c, j * 128:(j + 1) * 128], d_["sm"][p2], True, False,
                               reads=[d_["Rv"], d_["Rsm"][p2]], writes=[PSR[b0 + 3]])
                            MM(psN[:, j, :], d_["Cb"][:, j * 128:(j + 1) * 128], d_["q"][:, cs_], False, True,
                               reads=[d_["RCb"], d_["Rq"]], writes=[PSR[b0 + 3]])
                        ACT(d_["dm"][p2], psN[:, 2, :], AF.Abs, reads=[PSR[b0 + 3]], writes=[d_["Rdm"][p2]])
                        TS("dve", d_["dm"][p2], d_["dm"][p2], 1.0, None, ALU.max, reads=[d_["Rdm"][p2]], writes=[d_["Rdm"][p2]])
                        sc.op("dve", lambda o=d_["dm"][p2]: nc.vector.reciprocal(out=o, in_=o), reads=[d_["Rdm"][p2]], writes=[d_["Rdm"][p2]])
                        hs_ = (c // 4) % 2
                        for j in range(2):
                            TT("dve", d_["hst"][hs_][:, j, (c % 4) * 128:(c % 4 + 1) * 128], psN[:, j, :], d_["dm"][p2], ALU.mult,
                               reads=[PSR[b0 + 3], d_["Rdm"][p2]], writes=[d_["Rhst"][hs_]])
                        if c % 4 == 3:
                            tc = c // 4
                            DMA("sp", hraw_d[2 * h:2 * h + 2].rearrange("j p s -> p j s")[:, :, tc * 512:(tc + 1) * 512], d_["hst"][hs_],
                                reads=[d_["Rhst"][hs_]], pwrites=[R["hraw"]])
                        eg = egs1[:, h * TB + c:h * TB + c + 1]
                        STT(d_["C"], psU, eg, d_["Ch"], ALU.mult, ALU.add, reads=[PSR[b0 + 2], Regs1, d_["RCh"]], writes=[d_["RC"]])
                        ACT(d_["Cb"], d_["C"], AF.Copy, reads=[d_["RC"]], writes=[d_["RCb"]])
                        if c + 1 < TB:
                            eg2 = egs1[:, h * TB + c + 1:h * TB + c + 2]
                            TS("pool", d_["Ch"], d_["C"], eg2, None, ALU.mult, reads=[d_["RC"], Regs1], writes=[d_["RCh"]])
            sc.barrier_all()
            if upto == "m1":
                return

            wk.reset()
            m2 = [dict(h=wk.alloc(1024, F32), g=wk.alloc(1024, F32), sq=wk.alloc(1024, F32), r=wk.alloc(512, F32),
                       o=wk.alloc(1024, BF16), Rh=Res(), Rg=Res(), Rsq=Res(), Rr=Res(), Ro=Res()) for _ in range(2)]
            u = 0
            for h in range(4):
                for tc in range(TC):
                    b_ = m2[u % 2]
                    bi = u % 4
                    u += 1
                    ts_ = slice(tc * 512, (tc + 1) * 512)
                    hv = b_["h"].rearrange("p (j t) -> p j t", t=512)
                    gv = b_["g"].rearrange("p (j t) -> p j t", t=512)
                    ov = b_["o"].rearrange("p (j t) -> p j t", t=512)
                    DMA("sp", hv, hraw_d[2 * h:2 * h + 2].rearrange("j p s -> p j s")[:, :, ts_], reads=[R["hraw"]], writes=[b_["Rh"]])
                    DMA("sp", gv, sgo_d[2 * h:2 * h + 2].rearrange("j p s -> p j s")[:, :, ts_], reads=[R["sgo"]], writes=[b_["Rg"]])
                    ACT(b_["sq"], b_["h"], AF.Square, reads=[b_["Rh"]], writes=[b_["Rsq"]])
                    ps = bank(bi)
                    MM(ps, ones_f, b_["sq"][:, 0:512], True, False, reads=[b_["Rsq"], Rcf], writes=[PSR[bi]])
                    MM(ps, ones_f, b_["sq"][:, 512:1024], False, True, reads=[b_["Rsq"], Rcf], writes=[PSR[bi]])
                    TS("dve", b_["r"], ps, 1.0 / 256, EPS, ALU.mult, ALU.add, reads=[PSR[bi]], writes=[b_["Rr"]])
                    ACT(b_["r"], b_["r"], AF.Ln, reads=[b_["Rr"]], writes=[b_["Rr"]])
                    ACT(b_["r"], b_["r"], AF.Exp, reads=[b_["Rr"]], writes=[b_["Rr"]], scale=-0.5)
                    for j in range(2):
                        gc = small[:, SM["gmh"] + 2 * h + j:SM["gmh"] + 2 * h + j + 1]
                        ACT(gv[:, j, :], gv[:, j, :], AF.Copy, reads=[b_["Rg"], Rsmall], writes=[b_["Rg"]], scale=gc)
                        TT("dve", hv[:, j, :], hv[:, j, :], b_["r"], ALU.mult, reads=[b_["Rh"], b_["Rr"]], writes=[b_["Rh"]])
                        TT("dve", ov[:, j, :], hv[:, j, :], gv[:, j, :], ALU.mult, reads=[b_["Rh"], b_["Rg"]], writes=[b_["Ro"]])
                    DMA("sp", hmT_d[2 * h:2 * h + 2].rearrange("j p s -> p j s")[:, :, ts_], ov, reads=[b_["Ro"]], pwrites=[R["hmT"]])
            sc.barrier_all()
            if upto == "m2":
                return

            wk.reset()
            ab = [dict(q=[wk.alloc(S, BF16) for _ in range(2)], k=[wk.alloc(S, BF16) for _ in range(2)],
                       v=wk.alloc(TB * 256, BF16).rearrange("p (c v) -> p c v", v=256), R=Res()) for _ in range(2)]
            pT = [(wk.alloc(512, BF16), Res()) for _ in range(4)]
            on = [(wk.alloc(1024, F32), Res()) for _ in range(2)]
            rs_ = (wk.alloc(512, F32), Res())
            ot = (wk.alloc(1024, F32), Res())
            sq = (wk.alloc(1024, F32), Res())
            rr = (wk.alloc(512, F32), Res())
            ost = [(wk.alloc(1024, BF16), Res()) for _ in range(2)]
            scale = 128.0 ** -0.5
            cst = wk.alloc(S, F32)
            snt = wk.alloc(S, F32)
            Rcs = Res()
            DMA("sp", cst[0:32, :], cs_d[0], reads=[R["cs"]], pwrites=[Rcs])
            DMA("sp", snt[0:32, :], cs_d[1], reads=[R["cs"]], pwrites=[Rcs])
            xs = wk.alloc(S, BF16)
            Rxs = Res()
            rt1 = wk.alloc(1024, F32)
            rt2 = wk.alloc(1024, F32)
            Rrt12 = Res()
            RC = min(1024, S)
            uu = 0
            oc = 0
            def head_thunks(hh):
                aa = ab[hh % 2]
                th = []
                for c in range(2):
                    th.append(lambda c=c: DMA("sp", aa["q"][c], aq_d[2 * hh + c], reads=[R["aq"]], pwrites=[aa["R"]]))
                    th.append(lambda c=c: DMA("sp", aa["k"][c], ak_d[2 * hh + c], reads=[R["ak"]], pwrites=[aa["R"]]))
                for c0 in range(0, TB, 8):
                    th.append(lambda c0=c0: DMA("sp", aa["v"][:, c0:c0 + 8, :],
                                                av_d.rearrange("(c p) v -> p c v", p=128)[:, c0:c0 + 8, hh * 256:(hh + 1) * 256],
                                                reads=[R["av"]], pwrites=[aa["R"]]))
                for c in range(2):
                    for (buf, src_d, Rsrc) in ((aa["q"][c], aq_d[2 * hh + c], R["aq"]), (aa["k"][c], ak_d[2 * hh + c], R["ak"])):
                        def ld(src_d=src_d, Rsrc=Rsrc):
                            DMA("sp", xs[0:16, :], src_d[16:32, :], reads=[Rsrc], pwrites=[Rxs])
                            DMA("sp", xs[16:32, :], src_d[0:16, :], reads=[Rsrc], pwrites=[Rxs])
                        th.append(ld)
                        for r0_ in range(0, S, RC):
                            def rp(buf=buf, sl=slice(r0_, r0_ + RC)):
                                TT("dve", rt1[0:32, 0:RC], buf[0:32, sl], cst[0:32, sl], ALU.mult, reads=[aa["R"], Rcs], writes=[Rrt12])
                                TT("dve", rt2[0:32, 0:RC], xs[0:32, sl], snt[0:32, sl], ALU.mult, reads=[Rxs, Rcs], writes=[Rrt12])
                                TT("dve", buf[0:32, sl], rt1[0:32, 0:RC], rt2[0:32, 0:RC], ALU.add, reads=[Rrt12], writes=[aa["R"]])
                            th.append(rp)
                return th

            deferred = [None]
            for t_ in head_thunks(0):
                t_()
            for h in range(8):
                a_ = ab[h % 2]
                pend = head_thunks(h + 1) if h + 1 < 8 else []
                per_j = -(-len(pend) // TC) if pend else 0
                for j in range(TC):
                    for c in range(2):
                        bO, bO1, bS = (2, 3, 4) if c == 0 else (5, 6, 7)
                        psO = bank(bO)
                        psO1 = bank(bO1)
                        psSum = bank(bS)
                        nk = 4 * j + 4
                        units = []
                        for m in range(nk):
                            r0 = 128 * max(0, m - 4 * j)
                            units.append((m, r0))

                        def do_s(m, r0, idx):
                            bi = idx % 2
                            MM(bank(bi)[:, r0:512], a_["k"][c][:, m * 128:(m + 1) * 128], a_["q"][c][:, j * 512 + r0:(j + 1) * 512],
                               True, True, reads=[a_["R"]], writes=[PSR[bi]])
                            p_, Rp2 = pT[idx % 4]
                            ACT(p_[:, r0:512], bank(bi)[:, r0:512], AF.Exp, reads=[PSR[bi]], writes=[Rp2], scale=scale)
                            if m >= 4 * j:
                                TT("dve", p_[:, r0:r0 + 128], p_[:, r0:r0 + 128], tri_b, ALU.mult, reads=[Rp2, Rcb], writes=[Rp2])

                        def do_pv(m, r0, idx, first, last):
                            p_, Rp2 = pT[idx % 4]
                            MM(psO[:, r0:512], a_["v"][:, m, 0:128], p_[:, r0:512], first, last, reads=[a_["R"], Rp2], writes=[PSR[bO]])
                            MM(psO1[:, r0:512], a_["v"][:, m, 128:256], p_[:, r0:512], first, last, reads=[a_["R"], Rp2], writes=[PSR[bO1]])
                            MM(psSum[:, r0:512], ones_b, p_[:, r0:512], first, last, reads=[Rcb, Rp2], writes=[PSR[bS]])

                        LA = 2
                        for ui in range(min(LA, nk)):
                            do_s(units[ui][0], units[ui][1], uu + ui)
                        for ui in range(nk):
                            if ui + LA < nk:
                                do_s(units[ui + LA][0], units[ui + LA][1], uu + ui + LA)
                            do_pv(units[ui][0], units[ui][1], uu + ui, ui == 0, ui == nk - 1)
                            if ui == 0 and c == 0 and deferred[0] is not None:
                                deferred[0]()
                                deferred[0] = None
                        uu += nk
                        rt_, Rrt_ = rs_
                        sc.op("dve", lambda o=rt_, i_=psSum: nc.vector.reciprocal(out=o, in_=i_), reads=[PSR[bS]], writes=[Rrt_])
                        on_, Ron = on[c]
                        TT("dve", on_[:, 0:512], psO, rt_, ALU.mult, reads=[PSR[bO], Rrt_], writes=[Ron])
                        TT("dve", on_[:, 512:1024], psO1, rt_, ALU.mult, reads=[PSR[bO1], Rrt_], writes=[Ron])
                    o_, Ro_ = ot
                    STT(o_, on[1][0], nlam, on[0][0], ALU.mult, ALU.add, reads=[on[0][1], on[1][1], Rlam], writes=[Ro_])
                    s_, Rs_ = sq
                    ACT(s_, o_, AF.Square, reads=[Ro_], writes=[Rs_])
                    os2, Ros2 = ost[oc % 2]
                    oc += 1

                    def norm_tail(h=h, j=j, o_=o_, Ro_=Ro_, s_=s_, Rs_=Rs_, os2=os2, Ros2=Ros2):
                        psn = bank(7)
                        MM(psn, ones_f, s_[:, 0:512], True, False, reads=[Rs_, Rcf], writes=[PSR[7]])
                        MM(psn, ones_f, s_[:, 512:1024], False, True, reads=[Rs_, Rcf], writes=[PSR[7]])
                        r_, Rr_ = rr
                        TS("dve", r_, psn, 1.0 / 256, EPS, ALU.mult, ALU.add, reads=[PSR[7]], writes=[Rr_])
                        ACT(r_, r_, AF.Ln, reads=[Rr_], writes=[Rr_])
                        ACT(r_, r_, AF.Exp, reads=[Rr_], writes=[Rr_], scale=-0.5, bias=math.log(1.0 - lam_init))
                        for jj in range(2):
                            gs = small[:, SM["gsub"] + jj:SM["gsub"] + jj + 1]
                            STT(os2[:, jj * 512:(jj + 1) * 512], o_[:, jj * 512:(jj + 1) * 512], gs, r_, ALU.mult, ALU.mult,
                                reads=[Ro_, Rr_, Rsmall], writes=[Ros2])
                        DMA("sp", haT_d[2 * h:2 * h + 2].rearrange("j p s -> p j s")[:, :, j * 512:(j + 1) * 512],
                            os2.rearrange("p (j t) -> p j t", t=512), reads=[Ros2], pwrites=[R["haT"]])

                    deferred[0] = norm_tail
                    for t_ in pend[j * per_j:(j + 1) * per_j]:
                        t_()
            if deferred[0] is not None:
                deferred[0]()
                deferred[0] = None
            sc.barrier_all()
            if upto == "a":
                return

            wk.reset()
            ygm = [(wk.alloc(2048, BF16), Res()) for _ in range(2)]
            yga = [(wk.alloc(2048, BF16), Res()) for _ in range(2)]
            ytm = [(wk.alloc(512, F32), Res()) for _ in range(2)]
            ytm2 = [(wk.alloc(512, F32), Res()) for _ in range(2)]
            ycur = [None, None, 0]

            def epi_y(slab, t0, pss, dst, Rd):
                ST = min(2048, S)
                if t0 % ST == 0:
                    k = ycur[2] % 2
                    ycur[2] += 1
                    ycur[0] = ygm[k]
                    ycur[1] = yga[k]
                    DMA("act", ycur[0][0][:, 0:ST], sgm_d[slab][:, t0:t0 + ST], reads=[R["sgm"]], writes=[ycur[0][1]])
                    DMA("act", ycur[1][0][:, 0:ST], sga_d[slab][:, t0:t0 + ST], reads=[R["sga"]], writes=[ycur[1][1]])
                off = t0 % ST
                (pm_, Rpm), (pa_, Rpa) = pss
                k = (t0 // 512) % 2
                a1, Ra1 = ytm[k]
                a2, Ra2 = ytm2[k]
                TT("dve", a1, pm_, ycur[0][0][:, off:off + 512], ALU.mult, reads=[Rpm, ycur[0][1]], writes=[Ra1])
                TT("dve", a2, pa_, ycur[1][0][:, off:off + 512], ALU.mult, reads=[Rpa, ycur[1][1]], writes=[Ra2])
                TT("dve", dst, a1, a2, ALU.add, reads=[Ra1, Ra2], writes=[Rd])

            gemm_fm([(hmT_d, R["hmT"], 8, lambda s: pm_in[l, s]), (haT_d, R["haT"], 16, lambda s: pa_in[l, s])],
                    range(16), 128, epi_y, lambda s: (yT_d[s], R["yT"]), BF16, 2048, wbufs=2)
            sc.barrier_all()
            if upto == "y":
                return

            wk.reset()
            xr = [(wk.alloc(512, F32), Res()) for _ in range(3)]
            octr = [0]

            def mk_epi_res(xs_d, Rxs, xd_d, Rxd, WC):
                def epi(slab, tb, ps, Rp_):
                    octr[0] += 1
                    xt_, Rxt_ = xr[octr[0] % 3]
                    rows = slice(tb * 128, (tb + 1) * 128)
                    cols = slice(slab * WC, (slab + 1) * WC)
                    DMA("act", xt_[:, 0:WC], xs_d[rows, cols], reads=[Rxs], writes=[Rxt_])
                    TT("dve", xt_[:, 0:WC], ps, xt_[:, 0:WC], ALU.add, reads=[Rp_, Rxt_], writes=[Rxt_])
                    DMA("sp", xd_d[rows, cols], xt_[:, 0:WC], reads=[Rxt_], pwrites=[Rxd])
                return epi

            gemm_tm(yT_d, R["yT"], KCD, lambda s: wo_in[l, s], 4, 512, mk_epi_res(x_d, Rx, xn1_d, Rxn1, 512), 2048)
            sc.barrier_all()
            if upto == "o":
                return

            norm_phase(xn1_d, Rxn1, SM["gffn"])
            sc.barrier_all()
            wk.reset()
            fs = [(wk.alloc(512, F32), Res()) for _ in range(2)]
            fctr = [0]

            def epi_ffn(slab, t0, pss, dst, Rd):
                (pg, Rpg), (pu, Rpu) = pss
                fctr[0] += 1
                s_, Rs_ = fs[fctr[0] % 2]
                ACT(s_, pg, AF.Silu, reads=[Rpg], writes=[Rs_])
                TT("dve", dst, pu, s_, ALU.mult, reads=[Rpu, Rs_], writes=[Rd])

            gemm_fm([(hT_d, R["hT"], KCD, lambda s: wga_in[l, s]), (hT_d, R["hT"], KCD, lambda s: wup_in[l, s])],
                    range(44), 128, epi_ffn, lambda s: (actT_d[s], R["actT"]), BF16, 2048, wbufs=2)
            sc.barrier_all()
            if upto == "f1":
                return
            wk.reset()
            xr[:] = [(wk.alloc(512, F32), Res()) for _ in range(3)]
            gemm_tm(actT_d, R["actT"], KCF, lambda s: wdn_in[l, s], 8, 256, mk_epi_res(xn1_d, Rxn1, xn2_d, Rxn2, 256), 1024)
            sc.barrier_all()

        cur, Rcur = x_in, R["xin"]
        done = False
        for l in range(depth):
            layer(l, cur, Rcur, xa_d, R["xa"], xb_d, R["xb"])
            cur, Rcur = xb_d, R["xb"]
            if upto is not None:
                done = True
                break
        if not done:
            wk.reset()
            gf_ = wk.alloc(D, F32)
            Rgf = Res()
            DMA("sp", gf_, gfin_in, writes=[Rgf])
            xt = [(wk.alloc(D, F32), Res()) for _ in range(2)]
            junk = (wk.alloc(D, BF16), Res())
            yo = [(wk.alloc(D, F32), Res()) for _ in range(2)]
            st = wk.alloc(4 * TB, F32)
            Rst = [Res(), Res()]
            for tb in range(TB):
                s2 = tb % 2
                x_, Rx_ = xt[s2]
                DMA("sp", x_, cur[tb * 128:(tb + 1) * 128, :], reads=[Rcur], writes=[Rx_])
                ss = st[:, 4 * tb:4 * tb + 1]
                ms = st[:, 4 * tb + 1:4 * tb + 2]
                rstd = st[:, 4 * tb + 2:4 * tb + 3]
                ACT(junk[0], x_, AF.Square, reads=[Rx_], writes=[Rst[s2]], accum_out=ss)
                TS("dve", ms, ss, 1.0 / D, EPS, ALU.mult, ALU.add, reads=[Rst[s2]], writes=[Rst[s2]])
                ACT(ms, ms, AF.Ln, reads=[Rst[s2]], writes=[Rst[s2]])
                ACT(rstd, ms, AF.Exp, reads=[Rst[s2]], writes=[Rst[s2]], scale=-0.5)
                y_, Ry_ = yo[s2]
                STT(y_, x_, rstd, gf_, ALU.mult, ALU.mult, reads=[Rx_, Rst[s2], Rgf], writes=[Ry_])
                DMA("sp", out_d[tb * 128:(tb + 1) * 128, :], y_, reads=[Ry_], pwrites=[R["out"]])

        block = es.enter_context(nc.Block())
        sc.emit(block)
    return nc, dbg_outs


def fm_slabs(w, ncol):
    L, K, N = w.shape
    return np.ascontiguousarray(w.reshape(L, K // 128, 128, N // ncol, ncol).transpose(0, 3, 2, 1, 4))


def host_consts():
    cf = np.zeros((128, 1024), np.float32)
    cf[:, 0:128] = 1.0
    for h in range(4):
        cf[h, 128 + h * 128:128 + (h + 1) * 128] = 1.0
    half = 16
    inv = np.power(np.float32(ROPE_THETA), -np.arange(half, dtype=np.float32) * np.float32(2.0 / 32)).astype(np.float32)
    cf[0:16, 640] = inv / np.float32(2 * math.pi)
    cf[16:32, 640] = inv / np.float32(2 * math.pi)
    cf[:, 641] = -0.5
    cf[:, 642] = 0.25
    cf[0:4, 768:896] = 1.0
    cb = np.zeros((128, 512), np.float32)
    cb[:, 0:128] = np.eye(128)
    cb[:, 128:256] = 1.0
    cb[:, 256:384] = (np.arange(128)[None, :] >= np.arange(128)[:, None])
    rt = np.zeros((32, 32), np.float32)
    for m in range(32):
        rt[(m + 16) % 32, m] = 1.0
    cb[0:32, 384:416] = rt
    return cf, cb.astype(ml_dtypes.bfloat16)


def host_layout(inp, depth):
    w_in = np.asarray(inp["w_in"])[:depth]
    o = {}
    c = lambda a, b: w_in[:, :, a:b]
    fam = [c(0, 1024), c(2048, 3072), c(3080, 5128), c(5128, 7176), c(9224, 11272), c(11272, 13320)]
    o["wfm"] = np.concatenate([fm_slabs(f, 128) for f in fam], axis=1)
    wg = np.zeros((depth, 1, 128, KCD, 128), np.float32)
    wg[:, 0, :, :, 0:4] = fm_slabs(c(3072, 3076), 4)[:, 0]
    wg[:, 0, :, :, 32:36] = fm_slabs(c(3076, 3080), 4)[:, 0]
    o["wg"] = wg
    o["wtm"] = np.concatenate([fm_slabs(c(1024, 2048), 512), fm_slabs(c(7176, 9224), 512)], axis=1)
    o["pm"] = fm_slabs(np.asarray(inp["p_m"])[:depth], 128)
    o["pa"] = fm_slabs(np.asarray(inp["p_a"])[:depth], 128)
    o["wo"] = fm_slabs(np.asarray(inp["w_out"])[:depth], 512)
    o["wga"] = fm_slabs(np.asarray(inp["w_gate"])[:depth], 128)
    o["wup"] = fm_slabs(np.asarray(inp["w_up"])[:depth], 128)
    o["wdn"] = fm_slabs(np.asarray(inp["w_down"])[:depth], 256)
    small = np.zeros((depth, 128, NSMALL), np.float32)
    g = lambda k: np.asarray(inp[k])[:depth]
    small[:, :, 0:16] = g("g_mix").reshape(depth, 16, 128).transpose(0, 2, 1)
    small[:, :, 16:32] = g("g_ffn").reshape(depth, 16, 128).transpose(0, 2, 1)
    small[:, :, 32:64] = g("conv_w").reshape(depth, 4, 8, 128).transpose(0, 3, 2, 1).reshape(depth, 128, 32)
    small[:, :, 64:72] = g("conv_b").reshape(depth, 8, 128).transpose(0, 2, 1)
    small[:, :, 72:80] = g("g_mhead").reshape(depth, 8, 128).transpose(0, 2, 1)
    small[:, :, 80] = g("lambda_q1")
    small[:, :, 81] = g("lambda_k1")
    small[:, :, 82] = g("lambda_q2")
    small[:, :, 83] = g("lambda_k2")
    small[:, :, 84:86] = g("g_sub").reshape(depth, 2, 128).transpose(0, 2, 1)
    small[:, 0:4, 86] = g("i_bias")
    small[:, 0:4, 87] = g("f_bias")
    o["small"] = small
    o["gfin"] = np.ascontiguousarray(np.broadcast_to(np.asarray(inp["g_final"])[None, :], (128, D)))
    cf, cb = host_consts()
    o["cf"] = cf
    o["cb"] = cb
    return o


_CACHE = {}


def kernel(**inputs):
    x = np.asarray(inputs["x"])
    pos = np.asarray(inputs["positions"]).astype(np.int32)
    B, S, _ = x.shape
    shared = host_layout(inputs, DEPTH)
    key = (S, DEPTH)
    if key not in _CACHE:
        _CACHE[key] = build(S, DEPTH)[0]
    nc = _CACHE[key]
    in_maps = []
    for b in range(B):
        m = dict(shared)
        m["x"] = np.ascontiguousarray(x[b])
        m["pos"] = np.ascontiguousarray(pos[b:b + 1])
        in_maps.append(m)
    res = run_bass_kernel_spmd(nc, in_maps, core_ids=list(range(B)))
    return np.stack([np.asarray(r["out"]) for r in res.results], axis=0).astype(np.float32)
```

```python
import math
from contextlib import ExitStack

import numpy as np
import ml_dtypes

import concourse.bass as bass
import concourse.mybir as mybir
from concourse.bass_utils import run_bass_kernel_spmd

F32 = mybir.dt.float32
BF16 = mybir.dt.bfloat16
I32 = mybir.dt.int32
U8 = mybir.dt.uint8
AF = mybir.ActivationFunctionType
ALU = mybir.AluOpType

D = 2048
KCD = 16
DFF = 5632
KCF = 44
DEPTH = 4
SEQ = 4096
EPS = 1e-6
NSMALL = 96
ROPE_THETA = 500000.0


class Op:
    __slots__ = ("eng", "fn", "deps", "needed", "sigval", "dma", "sem", "val", "slot")


class OpSet:
    __slots__ = ("d",)

    def __init__(self):
        self.d = {}

    def add(self, o):
        self.d[(o.eng, o.slot) if o.dma else o.eng] = o

    def ops(self):
        return self.d.values()

    def __bool__(self):
        return bool(self.d)


class Res:
    __slots__ = ("ws", "rs", "prs", "name")

    def __init__(self, name=""):
        self.ws = OpSet()
        self.rs = OpSet()
        self.prs = OpSet()
        self.name = name


class Sched:
    ENGS = ("pe", "act", "dve", "pool", "sp")
    QUEUES = ("sp", "pool", "act")

    def __init__(self, nc, es, K=4):
        self.nc = nc
        self.K = K
        self.streams = {e: [] for e in self.ENGS}
        self.engsem = {e: es.enter_context(nc.semaphore("sem_" + e)) for e in self.ENGS}
        self.ring = {q: [es.enter_context(nc.semaphore("ring_%s_%d" % (q, i))) for i in range(K)] for q in self.QUEUES}
        self.ndma = {q: 0 for q in self.QUEUES}
        self.ringlast = {q: [None] * K for q in self.QUEUES}
        self.lastc = {e: None for e in self.ENGS}
        self.barrier = {}

    def op(self, eng, fn, reads=(), writes=(), pwrites=(), dma=False):
        o = Op()
        o.eng = eng
        o.fn = fn
        o.dma = dma
        o.needed = False
        o.sigval = None
        o.slot = None
        o.sem = None
        o.val = None
        deps = set()
        if eng in self.barrier:
            deps |= self.barrier.pop(eng)
        if dma:
            n = self.ndma[eng]
            self.ndma[eng] = n + 1
            slot = n % self.K
            o.slot = slot
            o.sem = self.ring[eng][slot]
            o.val = 16 * (n // self.K + 1)
            prev = self.ringlast[eng][slot]
            if prev is not None:
                deps.add(prev)
            self.ringlast[eng][slot] = o
        for r in reads:
            deps.update(r.ws.ops())
        for w in writes:
            deps.update(w.ws.ops())
            deps.update(w.rs.ops())
            if not w.rs:
                deps.update(w.prs.ops())
        for w in pwrites:
            if w.rs:
                deps.update(w.rs.ops())
            else:
                deps.update(w.prs.ops())
        if eng == "pe":
            deps = {d for d in deps if d.dma or d.eng != "pe"}
        deps.discard(o)
        for d in deps:
            if not d.dma:
                d.needed = True
        o.deps = deps
        for r in reads:
            r.rs.add(o)
        for w in writes:
            if w.rs:
                w.prs = w.rs
                w.rs = OpSet()
            w.ws = OpSet()
            w.ws.add(o)
        for w in pwrites:
            if w.rs:
                w.prs = w.rs
                w.rs = OpSet()
                w.ws = OpSet()
            w.ws.add(o)
        self.streams[eng].append(o)
        if not dma:
            self.lastc[eng] = o
        return o

    def barrier_all(self):
        b = set()
        for e, o in self.lastc.items():
            if o is not None:
                b.add(o)
        for q in self.QUEUES:
            for o in self.ringlast[q]:
                if o is not None:
                    b.add(o)
        for e in self.ENGS:
            self.barrier[e] = set(b) | self.barrier.get(e, set())

    def emit(self, block):
        nc = self.nc
        for e, st in self.streams.items():
            c = 0
            for o in st:
                if (not o.dma) and o.needed:
                    c += 1
                    o.sigval = c
        engmap = {"pe": block.tensor, "act": block.scalar, "dve": block.vector, "pool": block.gpsimd, "sp": block.sync}
        nceng = {"pe": nc.tensor, "act": nc.scalar, "dve": nc.vector, "pool": nc.gpsimd, "sp": nc.sync}
        finals = []
        for q in self.QUEUES:
            for o in self.ringlast[q]:
                if o is not None:
                    finals.append((o.sem, o.val))
        engsem = self.engsem

        def mk(e, st):
            def body(_e):
                eng = nceng[e]
                known = {}
                for o in st:
                    waits = {}
                    for d in o.deps:
                        if d.dma:
                            sem, v = d.sem, d.val
                        else:
                            sem, v = engsem[d.eng], d.sigval
                        k = id(sem)
                        if k not in waits or waits[k][1] < v:
                            waits[k] = (sem, v)
                    for k, (sem, v) in waits.items():
                        if known.get(k, 0) < v:
                            eng.wait_ge(sem, v)
                            known[k] = v
                    ins = o.fn()
                    if o.dma:
                        ins.then_inc(o.sem, 16)
                    elif o.needed:
                        ins.then_inc(engsem[e], 1)
                if e == "sp":
                    for sem, v in finals:
                        eng.wait_ge(sem, v)
            return body

        for e, st in self.streams.items():
            engmap[e](mk(e, st))


class Carver:
    def __init__(self, base, size, start=0):
        self.base = base
        self.size = size
        self.start = start
        self.off = start
        self.gen = 0

    def reset(self):
        self.off = self.start
        self.gen += 1

    def alloc(self, n, dt):
        nb = {F32: 4, BF16: 2, I32: 4, U8: 1}[dt]
        off = (self.off + 63) // 64 * 64
        assert off + n * nb <= self.size, ("SBUF overflow", off, n * nb, self.size)
        self.off = off + n * nb
        return self.base[:, off:off + n * nb].bitcast(dt)


def build(S, depth, debug=False, upto=None):
    nc = bass.Bass("TRN2", target_bir_lowering=False)
    TB = S // 128
    TC = S // 512
    dbg_outs = []

    def din(name, shape, dt):
        return nc.dram_tensor(name, list(shape), dt, kind="ExternalInput").ap()

    def dscr(name, shape, dt):
        if debug:
            dbg_outs.append(name)
            return nc.dram_tensor(name, list(shape), dt, kind="ExternalOutput").ap()
        return nc.dram_tensor(name, list(shape), dt, kind="Internal").ap()

    x_in = din("x", [S, D], F32)
    pos_in = din("pos", [1, S], I32)
    small_in = din("small", [depth, 128, NSMALL], F32)
    gfin_in = din("gfin", [128, D], F32)
    cf_in = din("cf", [128, 1024], F32)
    cb_in = din("cb", [128, 512], BF16)
    wfm_in = din("wfm", [depth, 80, 128, KCD, 128], F32)
    wg_in = din("wg", [depth, 1, 128, KCD, 128], F32)
    wtm_in = din("wtm", [depth, 6, 128, KCD, 512], F32)
    pm_in = din("pm", [depth, 16, 128, 8, 128], F32)
    pa_in = din("pa", [depth, 16, 128, 16, 128], F32)
    wo_in = din("wo", [depth, 4, 128, KCD, 512], F32)
    wga_in = din("wga", [depth, 44, 128, KCD, 128], F32)
    wup_in = din("wup", [depth, 44, 128, KCD, 128], F32)
    wdn_in = din("wdn", [depth, 8, 128, KCF, 256], F32)
    out_d = nc.dram_tensor("out", [S, D], F32, kind="ExternalOutput").ap()

    hT_d = dscr("hT", [KCD, 128, S], BF16)
    mqk_d = dscr("mqk", [8, 128, S], F32)
    sgo_d = dscr("sgo", [8, 128, S], F32)
    gi_d = dscr("gi", [4, S], F32)
    gf_d = dscr("gf", [4, S], F32)
    aq_d = dscr("aq", [16, 128, S], BF16)
    ak_d = dscr("ak", [16, 128, S], BF16)
    sgm_d = dscr("sgm", [16, 128, S], BF16)
    sga_d = dscr("sga", [16, 128, S], BF16)
    mv_d = dscr("mv", [S, 1024], BF16)
    av_d = dscr("av", [S, 2048], BF16)
    qkt_d = dscr("qkt", [8, 128, S], BF16)
    egb_d = dscr("egb", [128, 4 * TB], F32)
    hraw_d = dscr("hraw", [8, 128, S], F32)
    hmT_d = dscr("hmT", [8, 128, S], BF16)
    haT_d = dscr("haT", [16, 128, S], BF16)
    yT_d = dscr("yT", [16, 128, S], BF16)
    actT_d = dscr("actT", [KCF, 128, S], BF16)
    xa_d = dscr("xa", [S, D], F32)
    xb_d = dscr("xb", [S, D], F32)
    cs_d = dscr("cs", [2, 32, S], F32)

    R = {}
    for n in ("hT", "mqk", "sgo", "gi", "gf", "aq", "ak", "sgm", "sga", "mv", "av", "qkt", "egb", "hraw", "hmT", "haT",
              "yT", "actT", "xa", "xb", "cs", "out", "xin"):
        R[n] = Res(n)

    es = ExitStack()
    with es:
        sb = es.enter_context(nc.sbuf_tensor("sb", [128, 196608], U8))
        pst = es.enter_context(nc.psum_tensor("ps", [128, 16384], U8))
        sc = Sched(nc, es, K=4)

        def bank(i, dt=F32):
            return pst[:, i * 2048:(i + 1) * 2048].bitcast(dt)

        PSR = [Res("ps%d" % i) for i in range(8)]

        PERS = 12288
        pc = Carver(sb, PERS, 0)
        wk = Carver(sb, 196608, PERS)

        def DMA(q, out, in_, reads=(), writes=(), pwrites=()):
            eng = {"sp": nc.sync, "pool": nc.gpsimd, "act": nc.scalar}[q]
            n = out.shape[-1]
            if n > 2048 and tuple(out.shape) == tuple(in_.shape):
                r = None
                for c0 in range(0, n, 2048):
                    c1 = min(n, c0 + 2048)
                    idx = tuple([slice(None)] * (len(out.shape) - 1) + [slice(c0, c1)])
                    o_, i_ = out[idx], in_[idx]
                    r = sc.op(q, lambda o_=o_, i_=i_: eng.dma_start(out=o_, in_=i_), reads, writes, pwrites, dma=True)
                return r
            return sc.op(q, lambda: eng.dma_start(out=out, in_=in_), reads, writes, pwrites, dma=True)

        def PE(fn, reads=(), writes=()):
            return sc.op("pe", fn, reads, writes)

        def MM(out, lhsT, rhs, start, stop, reads=(), writes=()):
            return sc.op("pe", lambda: nc.tensor.matmul(out, lhsT, rhs, start=start, stop=stop), reads, writes)

        def ACT(out, in_, func, reads=(), writes=(), bias=None, scale=None, accum_out=None):
            kw = {}
            if bias is not None:
                kw["bias"] = bias
            if scale is not None:
                kw["scale"] = scale
            if accum_out is not None:
                kw["accum_out"] = accum_out
            return sc.op("act", lambda: nc.scalar.activation(out=out, in_=in_, func=func, **kw), reads, writes)

        def TS(eng, out, in0, s1, s2, op0, op1=None, reads=(), writes=()):
            e = {"dve": nc.vector, "pool": nc.gpsimd}[eng]
            if op1 is None:
                return sc.op(eng, lambda: e.tensor_scalar(out=out, in0=in0, scalar1=s1, scalar2=None, op0=op0), reads, writes)
            return sc.op(eng, lambda: e.tensor_scalar(out=out, in0=in0, scalar1=s1, scalar2=s2, op0=op0, op1=op1), reads, writes)

        def TT(eng, out, in0, in1, op, reads=(), writes=()):
            e = {"dve": nc.vector, "pool": nc.gpsimd}[eng]
            return sc.op(eng, lambda: e.tensor_tensor(out=out, in0=in0, in1=in1, op=op), reads, writes)

        def STT(out, in0, scalar, in1, op0, op1, reads=(), writes=()):
            return sc.op("dve", lambda: nc.vector.scalar_tensor_tensor(out=out, in0=in0, scalar=scalar, in1=in1, op0=op0, op1=op1), reads, writes)

        def COPY(eng, out, in_, reads=(), writes=()):
            if eng == "act":
                return sc.op("act", lambda: nc.scalar.copy(out=out, in_=in_), reads, writes)
            e = {"dve": nc.vector, "pool": nc.gpsimd}[eng]
            return sc.op(eng, lambda: e.tensor_copy(out=out, in_=in_), reads, writes)

        cf = pc.alloc(1024, F32)
        cbt = pc.alloc(512, BF16)
        small = pc.alloc(NSMALL, F32)
        lamt = pc.alloc(8, F32)
        Rcf, Rcb, Rsmall, Rlam = Res("cf"), Res("cb"), Res("small"), Res("lam")
        DMA("sp", cf, cf_in, writes=[Rcf])
        DMA("sp", cbt, cb_in, writes=[Rcb])
        ones_f = cf[:, 0:128]
        sel_f = cf[:, 128:640].rearrange("p (h c) -> p h c", c=128)
        invf = cf[0:32, 640:641]
        mhalf = cf[:, 641:642]
        c025 = cf[0:32, 642:643]
        ones4 = cf[0:4, 768:896]
        ident_b = cbt[:, 0:128]
        ones_b = cbt[:, 128:256]
        tri_b = cbt[:, 256:384]
        rt_b = cbt[:, 384:512]

        wk.reset()
        posi = wk.alloc(S, I32)
        ang = wk.alloc(S, F32)
        t1 = wk.alloc(S, F32)
        t2 = wk.alloc(S, F32)
        ti = wk.alloc(S, I32)
        Rp, Ra, Rt1, Rt2, Rti = Res(), Res(), Res(), Res(), Res()
        DMA("sp", posi[0:32, :], pos_in.partition_broadcast(32), writes=[Rp])
        COPY("dve", ang[0:32, :], posi[0:32, :], reads=[Rp], writes=[Ra])
        TS("dve", ang[0:32, :], ang[0:32, :], invf, None, ALU.mult, reads=[Ra, Rcf], writes=[Ra])
        for which in (0, 1):
            if which == 0:
                TS("dve", t1[0:32, :], ang[0:32, :], 0.25, None, ALU.add, reads=[Ra], writes=[Rt1])
            else:
                COPY("dve", t1[0:32, :], ang[0:32, :], reads=[Ra], writes=[Rt1])
            COPY("dve", ti[0:32, :], t1[0:32, :], reads=[Rt1], writes=[Rti])
            COPY("dve", t2[0:32, :], ti[0:32, :], reads=[Rti], writes=[Rt2])
            TT("dve", t1[0:32, :], t1[0:32, :], t2[0:32, :], ALU.subtract, reads=[Rt1, Rt2], writes=[Rt1])
            TS("dve", t2[0:32, :], t1[0:32, :], 0.5, None, ALU.is_gt, reads=[Rt1], writes=[Rt2])
            TT("dve", t1[0:32, :], t1[0:32, :], t2[0:32, :], ALU.subtract, reads=[Rt1, Rt2], writes=[Rt1])
            TS("dve", t2[0:32, :], t1[0:32, :], -0.5, None, ALU.is_lt, reads=[Rt1], writes=[Rt2])
            TT("dve", t1[0:32, :], t1[0:32, :], t2[0:32, :], ALU.add, reads=[Rt1, Rt2], writes=[Rt1])
            ACT(t2[0:32, :], t1[0:32, :], AF.Sin, reads=[Rt1], writes=[Rt2], scale=6.28318)
            if which == 1:
                TS("dve", t2[0:16, :], t2[0:16, :], -1.0, None, ALU.mult, reads=[Rt2], writes=[Rt2])
            DMA("sp", cs_d[which], t2[0:32, :], reads=[Rt2], pwrites=[R["cs"]])
        sc.barrier_all()

        def load_small(l):
            DMA("sp", small, small_in[l], writes=[Rsmall])

        SM = dict(gmix=0, gffn=16, convw=32, convb=64, gmh=72, lam=80, gsub=84, ib=86, fb=87, nfb=88)

        def norm_phase(x_d, Rx, goff):
            wk.reset()
            NB = 3
            xt = [wk.alloc(D, F32) for _ in range(NB)]
            junk = wk.alloc(D, BF16)
            hb = [wk.alloc(D, BF16) for _ in range(NB)]
            hs = [wk.alloc(KCD * 512, BF16).rearrange("p (c t) -> p c t", t=512) for _ in range(2)]
            st = wk.alloc(4 * TB, F32)
            Rxt = [Res() for _ in range(NB)]
            Rhb = [Res() for _ in range(NB)]
            Rhs = [Res(), Res()]
            Rst = [Res() for _ in range(NB)]
            hTv = hT_d.rearrange("c p s -> p c s")

            def stage_a(tb):
                s2 = tb % NB
                DMA("sp", xt[s2], x_d[tb * 128:(tb + 1) * 128, :], reads=[Rx], writes=[Rxt[s2]])
                ss = st[:, 4 * tb:4 * tb + 1]
                ms = st[:, 4 * tb + 1:4 * tb + 2]
                rstd = st[:, 4 * tb + 2:4 * tb + 3]
                ACT(junk, xt[s2], AF.Square, reads=[Rxt[s2]], writes=[Rst[s2]], accum_out=ss)
                TS("dve", ms, ss, 1.0 / D, EPS, ALU.mult, ALU.add, reads=[Rst[s2]], writes=[Rst[s2]])
                ACT(ms, ms, AF.Ln, reads=[Rst[s2]], writes=[Rst[s2]])
                ACT(rstd, ms, AF.Exp, reads=[Rst[s2]], writes=[Rst[s2]], scale=-0.5)
                TS("dve", hb[s2], xt[s2], rstd, None, ALU.mult, reads=[Rxt[s2], Rst[s2]], writes=[Rhb[s2]])

            def stage_b(tb):
                s2 = tb % NB
                stg = (tb // 4) % 2
                for half in range(2):
                    bi = (2 * tb + half) % 4
                    pt = bank(bi, BF16).rearrange("p (c t) -> p c t", t=128)
                    for j in range(8):
                        kc = half * 8 + j
                        PE(lambda o=pt[:, j, :], i=hb[s2][:, kc * 128:(kc + 1) * 128]: nc.tensor.transpose(o, i, ident_b),
                           reads=[Rhb[s2], Rcb], writes=[PSR[bi]])
                    dst = hs[stg][:, half * 8:half * 8 + 8, (tb % 4) * 128:(tb % 4 + 1) * 128]
                    gb = small[:, goff + half * 8:goff + half * 8 + 8].unsqueeze(2).to_broadcast([128, 8, 128])
                    TT("dve", dst, pt, gb, ALU.mult, reads=[PSR[bi], Rsmall], writes=[Rhs[stg]])
                if tb % 4 == 3:
                    tc = tb // 4
                    DMA("sp", hTv[:, :, tc * 512:(tc + 1) * 512], hs[stg], reads=[Rhs[stg]], pwrites=[R["hT"]])

            stage_a(0)
            for tb in range(TB):
                if tb + 1 < TB:
                    stage_a(tb + 1)
                stage_b(tb)

        gemm_cache = {}

        def gemm_fm(groups, slabs, M, epi, out_fn, out_dt, STILE, wbufs=3):
            wk_mark = wk.off
            ST = min(STILE, S)
            NT = S // ST
            sig = ("fm", tuple((id(g[0]), g[2]) for g in groups), M, ST, wbufs)
            ck = (wk.gen, wk_mark)
            if ck in gemm_cache and gemm_cache[ck][0] == sig:
                _, inT, wsb, stage_raw = gemm_cache[ck]
            else:
                if ck in gemm_cache:
                    sc.barrier_all()
                inT = []
                shared = {}
                for (ind, Rin, KC, wfn) in groups:
                    if id(ind) in shared:
                        inT.append(shared[id(ind)])
                        continue
                    t = wk.alloc(KC * ST, BF16).rearrange("p (c t) -> p c t", t=ST)
                    inT.append((t, Res()))
                    shared[id(ind)] = inT[-1]
                wsb = []
                for (ind, Rin, KC, wfn) in groups:
                    wsb.append([(wk.alloc(KC * M, BF16).rearrange("p (c m) -> p c m", m=M), Res()) for _ in range(wbufs)])
                stage_raw = [(wk.alloc(ST, F32), Res()) for _ in range(2)]
                gemm_cache[ck] = (sig, inT, wsb, stage_raw)
            stage_f = stage_raw
            stage_b = [(a.bitcast(BF16)[:, 0:ST], r) for a, r in stage_raw]
            ng = len(groups)
            nbk = 6 // ng
            cnt = 0
            u = 0
            for st in range(NT):
                loaded = set()
                for gi_, (ind, Rin, KC, wfn) in enumerate(groups):
                    if id(ind) in loaded:
                        continue
                    loaded.add(id(ind))
                    t, Rt = inT[gi_]
                    src = ind.rearrange("c p s -> p c s")
                    step = 4
                    for k0 in range(0, KC, step):
                        k1 = min(KC, k0 + step)
                        DMA("sp", t[:, k0:k1, :], src[:, k0:k1, st * ST:(st + 1) * ST], reads=[Rin], pwrites=[Rt])
                for slab in slabs:
                    wv = []
                    for gi_, (ind, Rin, KC, wfn) in enumerate(groups):
                        wt, Rw = wsb[gi_][cnt % wbufs]
                        DMA("pool", wt, wfn(slab), writes=[Rw])
                        wv.append((wt, Rw))
                    odt = out_dt(slab) if callable(out_dt) else out_dt
                    sg, Rsg = (stage_f if odt == F32 else stage_b)[cnt % 2]
                    cnt += 1
                    for tc in range(ST // 512):
                        pss = []
                        for gi_, (ind, Rin, KC, wfn) in enumerate(groups):
                            bi = (u % nbk) * ng + gi_
                            ps = bank(bi)[0:M, :]
                            t, Rt = inT[gi_]
                            wt, Rw = wv[gi_]
                            for kc in range(KC):
                                MM(ps, wt[:, kc, :], t[:, kc, tc * 512:(tc + 1) * 512], kc == 0, kc == KC - 1,
                                   reads=[Rt, Rw], writes=[PSR[bi]])
                            pss.append((ps, PSR[bi]))
                        u += 1
                        epi(slab, st * ST + tc * 512, pss, sg[0:M, tc * 512:(tc + 1) * 512], Rsg)
                    outs = out_fn(slab)
                    if not isinstance(outs, list):
                        outs = [(outs[0], outs[1], slice(0, M))]
                    for od, Rod, rows in outs:
                        DMA("sp", od[:, st * ST:(st + 1) * ST], sg[rows, :], reads=[Rsg], pwrites=[Rod])
            wk.off = wk_mark

        def gemm_tm(ind, Rin, KC, wfn, nslab, WC, epi, STILE):
            wk_mark = wk.off
            ST = min(STILE, S)
            NT = S // ST
            sig = ("tm", id(ind), KC, WC, ST)
            ck = (wk.gen, wk_mark)
            if ck in gemm_cache and gemm_cache[ck][0] == sig:
                _, t, Rt, wsb = gemm_cache[ck]
            else:
                if ck in gemm_cache:
                    sc.barrier_all()
                t = wk.alloc(KC * ST, BF16).rearrange("p (c t) -> p c t", t=ST)
                Rt = Res()
                wsb = [(wk.alloc(KC * WC, BF16).rearrange("p (c m) -> p c m", m=WC), Res()) for _ in range(2)]
                gemm_cache[ck] = (sig, t, Rt, wsb)
            src = ind.rearrange("c p s -> p c s")
            cnt = 0
            u = 0
            for st in range(NT):
                for k0 in range(0, KC, 4):
                    k1 = min(KC, k0 + 4)
                    DMA("sp", t[:, k0:k1, :], src[:, k0:k1, st * ST:(st + 1) * ST], reads=[Rin], pwrites=[Rt])
                for slab in range(nslab):
                    wt, Rw = wsb[cnt % 2]
                    cnt += 1
                    wsrc = wfn(slab)
                    kstep = max(1, 2048 // WC)
                    for k0 in range(0, KC, kstep):
                        k1 = min(KC, k0 + kstep)
                        DMA("pool", wt[:, k0:k1, :], wsrc[:, k0:k1, :], pwrites=[Rw])
                    for tb in range(ST // 128):
                        bi = u % 6
                        u += 1
                        ps = bank(bi)[:, 0:WC]
                        for kc in range(KC):
                            MM(ps, t[:, kc, tb * 128:(tb + 1) * 128], wt[:, kc, :], kc == 0, kc == KC - 1,
                               reads=[Rt, Rw], writes=[PSR[bi]])
                        epi(slab, st * (ST // 128) + tb, ps, PSR[bi])
            wk.off = wk_mark

        def layer(l, x_d, Rx, xn1_d, Rxn1, xn2_d, Rxn2):
            lam_init = 0.8 - 0.6 * math.exp(-0.3 * l)
            load_small(l)
            lo = SM["lam"]
            TT("dve", lamt[:, 0:1], small[:, lo:lo + 1], small[:, lo + 1:lo + 2], ALU.mult, reads=[Rsmall], writes=[Rlam])
            TT("dve", lamt[:, 1:2], small[:, lo + 2:lo + 3], small[:, lo + 3:lo + 4], ALU.mult, reads=[Rsmall], writes=[Rlam])
            psl = bank(7)[:, 0:2]
            MM(psl, ones_f, lamt[:, 0:2], True, True, reads=[Rlam, Rcf], writes=[PSR[7]])
            ACT(lamt[:, 2:4], psl, AF.Exp, reads=[PSR[7]], writes=[Rlam])
            TT("dve", lamt[:, 4:5], lamt[:, 3:4], lamt[:, 2:3], ALU.subtract, reads=[Rlam], writes=[Rlam])
            TS("dve", lamt[:, 5:6], lamt[:, 4:5], -lam_init, None, ALU.add, reads=[Rlam], writes=[Rlam])
            nlam = lamt[:, 5:6]
            TS("dve", small[0:4, SM["nfb"]:SM["nfb"] + 1], small[0:4, SM["fb"]:SM["fb"] + 1], -1.0, None, ALU.mult,
               reads=[Rsmall], writes=[Rsmall])

            norm_phase(x_d, Rx, SM["gmix"])
            sc.barrier_all()
            if upto == "n1":
                return

            wk.reset()
            ectr = [0]
            vst = [(wk.alloc(512, BF16), Res()) for _ in range(3)]
            vctr = [0]

            def epi_copy(slab, t0, pss, dst, Rd):
                ps, Rp_ = pss[0]
                ectr[0] += 1
                if ectr[0] % 2:
                    ACT(dst, ps, AF.Copy, reads=[Rp_], writes=[Rd])
                else:
                    COPY("dve", dst, ps, reads=[Rp_], writes=[Rd])

            def epi_sig(slab, t0, pss, dst, Rd):
                ps, Rp_ = pss[0]
                ACT(dst, ps, AF.Sigmoid, reads=[Rp_], writes=[Rd])

            def epi_rope(slab, t0, pss, dst, Rd):
                ps, Rp_ = pss[0]
                ectr[0] += 1
                k = ectr[0] % 2
                rb, Rrb = ropeb[k]
                rt, Rrt = ropet[k]
                ACT(dst, ps, AF.Copy, reads=[Rp_], writes=[Rd])
                prf = bank(7)
                pr = prf[0:32, :]
                MM(prf, rt_b, dst, True, True, reads=[Rd, Rcb], writes=[PSR[7]])
                TT("dve", rt[0:32, 0:512], ps[0:32, :], cst[0:32, t0:t0 + 512], ALU.mult, reads=[Rp_, Rcs], writes=[Rrt])
                TT("dve", rt[0:32, 512:1024], pr, snt[0:32, t0:t0 + 512], ALU.mult, reads=[PSR[7], Rcs], writes=[Rrt])
                TT("dve", dst[0:32, :], rt[0:32, 0:512], rt[0:32, 512:1024], ALU.add, reads=[Rrt], writes=[Rd])

            def g1_w(sl):
                return wg_in[l, 0] if sl == 80 else wfm_in[l, sl]

            def g1_epi(sl, t0, pss, dst, Rd):
                if 8 <= sl < 16 or 48 <= sl < 80:
                    epi_sig(sl, t0, pss, dst, Rd)
                else:
                    epi_copy(sl, t0, pss, dst, Rd)

            def g1_out(sl):
                if sl < 8:
                    return (mqk_d[sl], R["mqk"])
                if sl < 16:
                    return (sgo_d[sl - 8], R["sgo"])
                if sl < 32:
                    return (aq_d[sl - 16], R["aq"])
                if sl < 48:
                    return (ak_d[sl - 32], R["ak"])
                if sl < 64:
                    return (sgm_d[sl - 48], R["sgm"])
                if sl < 80:
                    return (sga_d[sl - 64], R["sga"])
                return [(gi_d, R["gi"], slice(0, 4)), (gf_d, R["gf"], slice(32, 36))]

            def g1_dt(sl):
                return F32 if (sl < 16 or sl == 80) else BF16

            gemm_fm([(hT_d, R["hT"], KCD, g1_w)], list(range(81)), 128, g1_epi, g1_out, g1_dt, 2048)
            if upto == "g1d":
                return

            def epi_v(slab, tb, ps, Rp_):
                vctr[0] += 1
                sg, Rsg = vst[vctr[0] % 3]
                if vctr[0] % 2:
                    ACT(sg, ps, AF.Copy, reads=[Rp_], writes=[Rsg])
                else:
                    COPY("dve", sg, ps, reads=[Rp_], writes=[Rsg])
                if slab < 2:
                    DMA("sp", mv_d[tb * 128:(tb + 1) * 128, slab * 512:(slab + 1) * 512], sg, reads=[Rsg], pwrites=[R["mv"]])
                else:
                    DMA("sp", av_d[tb * 128:(tb + 1) * 128, (slab - 2) * 512:(slab - 1) * 512], sg, reads=[Rsg], pwrites=[R["av"]])

            gemm_tm(hT_d, R["hT"], KCD, lambda s: wtm_in[l, s], 6, 512, epi_v, 2048)
            sc.barrier_all()
            if upto == "g1":
                return

            wk.reset()
            t_i = wk.alloc(S, F32)
            t_f = wk.alloc(S, F32)
            t_b = wk.alloc(S, F32)
            egs = wk.alloc(4 * TB, F32)
            Rti_, Rtf_, Rtb_, Regs = Res(), Res(), Res(), Res()
            sc.op("pool", lambda: nc.gpsimd.memset(t_i, 0.0), writes=[Rti_])
            sc.op("pool", lambda: nc.gpsimd.memset(t_f, 0.0), writes=[Rtf_])
            DMA("sp", t_i[0:4, :], gi_d, reads=[R["gi"]], writes=[Rti_])
            DMA("sp", t_f[0:4, :], gf_d, reads=[R["gf"]], writes=[Rtf_])
            TS("dve", t_i[0:4, :], t_i[0:4, :], small[0:4, SM["ib"]:SM["ib"] + 1], None, ALU.add, reads=[Rti_, Rsmall], writes=[Rti_])
            ACT(t_f[0:4, :], t_f[0:4, :], AF.Exp, reads=[Rtf_, Rsmall], writes=[Rtf_], scale=-1.0,
                bias=small[0:4, SM["nfb"]:SM["nfb"] + 1])
            ACT(t_f[0:4, :], t_f[0:4, :], AF.Ln, reads=[Rtf_], writes=[Rtf_], bias=1.0)
            for c in range(TB):
                sc.op("dve", lambda c=c: nc.vector.tensor_tensor_scan(out=t_b[0:4, c * 128:(c + 1) * 128], data0=ones4,
                                                                     data1=t_f[0:4, c * 128:(c + 1) * 128], initial=0.0,
                                                                     op0=ALU.mult, op1=ALU.add),
                      reads=[Rtf_, Rcf], writes=[Rtb_])
            ACT(t_f[0:4, :], t_b[0:4, :], AF.Exp, reads=[Rtb_], writes=[Rtf_], scale=-1.0)
            TT("dve", t_i[0:4, :], t_i[0:4, :], t_b[0:4, :], ALU.add, reads=[Rti_, Rtb_], writes=[Rti_])
            ACT(t_i[0:4, :], t_i[0:4, :], AF.Exp, reads=[Rti_], writes=[Rti_], bias=-0.5 * math.log(128.0))
            if upto == "m0a":
                return
            xin_ = [(wk.alloc(S, F32), Res()) for _ in range(2)]
            acc_ = [(wk.alloc(S, F32), Res()) for _ in range(2)]
            ost_ = [(wk.alloc(S, BF16), Res()) for _ in range(2)]
            cw = SM["convw"]
            for slab in range(8):
                xi, Rxi = xin_[slab % 2]
                ac, Rac = acc_[slab % 2]
                os_, Ros = ost_[slab % 2]
                h = slab % 4
                DMA("sp", xi, mqk_d[slab], reads=[R["mqk"]], writes=[Rxi])
                w = lambda j: small[:, cw + slab * 4 + j:cw + slab * 4 + j + 1]
                TS("dve", ac, xi, w(3), None, ALU.mult, reads=[Rxi, Rsmall], writes=[Rac])
                for j in range(3):
                    sh = 3 - j
                    STT(ac[:, sh:S], xi[:, 0:S - sh], w(j), ac[:, sh:S], ALU.mult, ALU.add, reads=[Rxi, Rac, Rsmall], writes=[Rac])
                ACT(ac, ac, AF.Silu, reads=[Rac, Rsmall], writes=[Rac], bias=small[:, SM["convb"] + slab:SM["convb"] + slab + 1])
                if upto == "m0c":
                    DMA("sp", hraw_d[0], ac, reads=[Rac], pwrites=[R["hraw"]])
                    return
                gsrc, Rg = (t_f, Rtf_) if slab < 4 else (t_i, Rti_)
                for tc in range(TC):
                    bi = tc % 8
                    ps = bank(bi)
                    MM(ps, sel_f[:, h, :], gsrc[:, tc * 512:(tc + 1) * 512], True, True, reads=[Rg, Rcf], writes=[PSR[bi]])
                    TT("dve", os_[:, tc * 512:(tc + 1) * 512], ac[:, tc * 512:(tc + 1) * 512], ps, ALU.mult,
                       reads=[Rac, PSR[bi]], writes=[Ros])
                    if slab < 4:
                        src = ps.rearrange("p (c t) -> p c t", t=128)[:, :, 127]
                        ACT(egs[:, h * TB + tc * 4:h * TB + tc * 4 + 4], src, AF.Copy, reads=[PSR[bi]], writes=[Regs])
                DMA("sp", qkt_d[slab], os_, reads=[Ros], pwrites=[R["qkt"]])
                if upto == "m0b":
                    return
            DMA("sp", egb_d, egs, reads=[Regs], writes=[R["egb"]])
            sc.barrier_all()
            if upto == "m0":
                return

            wk.reset()
            egs1 = wk.alloc(4 * TB, F32)
            Regs1 = Res()
            DMA("sp", egs1, egb_d, reads=[R["egb"]], writes=[Regs1])
            hd = []
            for i in range(2):
                d_ = dict(
                    q=wk.alloc(S, BF16), k=wk.alloc(S, BF16),
                    v=wk.alloc(TB * 384, BF16).rearrange("p (c v) -> p c v", v=384),
                    C=wk.alloc(384, F32), Ch=wk.alloc(384, F32), Cb=wk.alloc(384, BF16),
                    kt=[wk.alloc(128, BF16) for _ in range(2)], sm=[wk.alloc(128, BF16) for _ in range(2)],
                    dm=[wk.alloc(128, F32) for _ in range(2)],
                    hst=[wk.alloc(1024, F32).rearrange("p (j t) -> p j t", t=512) for _ in range(2)],
                    Rq=Res(), Rk=Res(), Rv=Res(), RC=Res(), RCh=Res(), RCb=Res(),
                    Rkt=[Res(), Res()], Rsm=[Res(), Res()], Rdm=[Res(), Res()], Rhst=[Res(), Res()],
                )
                hd.append(d_)
                sc.op("pool", lambda v=d_["v"]: nc.gpsimd.memset(v[:, :, 256:384], 1.0), writes=[d_["Rv"]])
            for hp in range(2):
                for i in range(2):
                    h = hp * 2 + i
                    d_ = hd[i]
                    DMA("sp", d_["q"], qkt_d[h], reads=[R["qkt"]], writes=[d_["Rq"]])
                    DMA("sp", d_["k"], qkt_d[4 + h], reads=[R["qkt"]], writes=[d_["Rk"]])
                    for c0 in range(0, TB, 8):
                        DMA("sp", d_["v"][:, c0:c0 + 8, 0:256], mv_d.rearrange("(c p) v -> p c v", p=128)[:, c0:c0 + 8, h * 256:(h + 1) * 256],
                            reads=[R["mv"]], pwrites=[d_["Rv"]])
                    sc.op("dve", lambda C=d_["C"]: nc.vector.memset(C, 0.0), writes=[d_["RC"]])
                    sc.op("dve", lambda C=d_["Ch"]: nc.vector.memset(C, 0.0), writes=[d_["RCh"]])
                    sc.op("dve", lambda C=d_["Cb"]: nc.vector.memset(C, 0.0), writes=[d_["RCb"]])
                for c in range(TB):
                    for i in range(2):
                        h = hp * 2 + i
                        d_ = hd[i]
                        b0 = i * 4
                        cs_ = slice(c * 128, (c + 1) * 128)
                        p2 = c % 2
                        psK = bank(b0 + 0, BF16)[:, 0:128]
                        psS = bank(b0 + 1)[:, 0:128]
                        psU = bank(b0 + 2)[:, 0:384]
                        psN = bank(b0 + 3)[:, 0:384].rearrange("p (j t) -> p j t", t=128)
                        PE(lambda o=psK, i_=d_["k"][:, cs_]: nc.tensor.transpose(o, i_, ident_b), reads=[d_["Rk"], Rcb], writes=[PSR[b0]])
                        ACT(d_["kt"][p2], psK, AF.Copy, reads=[PSR[b0]], writes=[d_["Rkt"][p2]])
                        MM(psS, d_["k"][:, cs_], d_["q"][:, cs_], True, True, reads=[d_["Rk"], d_["Rq"]], writes=[PSR[b0 + 1]])
                        TT("dve", d_["sm"][p2], psS, tri_b, ALU.mult, reads=[PSR[b0 + 1], Rcb], writes=[d_["Rsm"][p2]])
                        MM(psU, d_["kt"][p2], d_["v"][:, c, :], True, True, reads=[d_["Rkt"][p2], d_["Rv"]], writes=[PSR[b0 + 2]])
                        for j in range(3):
                            MM(psN[:, j, :], d_["v"][:, c, j * 128:(j + 1) * 128], d_["sm"][p2], True, False,
                               reads=[d_["Rv"], d_["Rsm"][p2]], writes=[PSR[b0 + 3]])
                            MM(psN[:, j, :], d_["Cb"][:, j * 128:(j + 1) * 128], d_["q"][:, cs_], False, True,
                               reads=[d_["RCb"], d_["Rq"]], writes=[PSR[b0 + 3]])
                        ACT(d_["dm"][p2], psN[:, 2, :], AF.Abs, reads=[PSR[b0 + 3]], writes=[d_["Rdm"][p2]])
                        TS("dve", d_["dm"][p2], d_["dm"][p2], 1.0, None, ALU.max, reads=[d_["Rdm"][p2]], writes=[d_["Rdm"][p2]])
                        sc.op("dve", lambda o=d_["dm"][p2]: nc.vector.reciprocal(out=o, in_=o), reads=[d_["Rdm"][p2]], writes=[d_["Rdm"][p2]])
                        hs_ = (c // 4) % 2
                        for j in range(2):
                            TT("dve", d_["hst"][hs_][:, j, (c % 4) * 128:(c % 4 + 1) * 128], psN[:, j, :], d_["dm"][p2], ALU.mult,
                               reads=[PSR[b0 + 3], d_["Rdm"][p2]], writes=[d_["Rhst"][hs_]])
                        if c % 4 == 3:
                            tc = c // 4
                            DMA("sp", hraw_d[2 * h:2 * h + 2].rearrange("j p s -> p j s")[:, :, tc * 512:(tc + 1) * 512], d_["hst"][hs_],
                                reads=[d_["Rhst"][hs_]], pwrites=[R["hraw"]])
                        eg = egs1[:, h * TB + c:h * TB + c + 1]
                        STT(d_["C"], psU, eg, d_["Ch"], ALU.mult, ALU.add, reads=[PSR[b0 + 2], Regs1, d_["RCh"]], writes=[d_["RC"]])
                        ACT(d_["Cb"], d_["C"], AF.Copy, reads=[d_["RC"]], writes=[d_["RCb"]])
                        if c + 1 < TB:
                            eg2 = egs1[:, h * TB + c + 1:h * TB + c + 2]
                            ACT(d_["Ch"], d_["C"], AF.Copy, reads=[d_["RC"], Regs1], writes=[d_["RCh"]], scale=eg2)
            sc.barrier_all()
            if upto == "m1":
                return

            wk.reset()
            m2 = [dict(h=wk.alloc(1024, F32), g=wk.alloc(1024, F32), sq=wk.alloc(1024, F32), r=wk.alloc(512, F32),
                       o=wk.alloc(1024, BF16), Rh=Res(), Rg=Res(), Rsq=Res(), Rr=Res(), Ro=Res()) for _ in range(3)]
            u = 0
            for h in range(4):
                for tc in range(TC):
                    b_ = m2[u % 3]
                    bi = u % 4
                    u += 1
                    ts_ = slice(tc * 512, (tc + 1) * 512)
                    hv = b_["h"].rearrange("p (j t) -> p j t", t=512)
                    gv = b_["g"].rearrange("p (j t) -> p j t", t=512)
                    ov = b_["o"].rearrange("p (j t) -> p j t", t=512)
                    DMA("sp", hv, hraw_d[2 * h:2 * h + 2].rearrange("j p s -> p j s")[:, :, ts_], reads=[R["hraw"]], writes=[b_["Rh"]])
                    DMA("sp", gv, sgo_d[2 * h:2 * h + 2].rearrange("j p s -> p j s")[:, :, ts_], reads=[R["sgo"]], writes=[b_["Rg"]])
                    ACT(b_["sq"], b_["h"], AF.Square, reads=[b_["Rh"]], writes=[b_["Rsq"]])
                    ps = bank(bi)
                    MM(ps, ones_f, b_["sq"][:, 0:512], True, False, reads=[b_["Rsq"], Rcf], writes=[PSR[bi]])
                    MM(ps, ones_f, b_["sq"][:, 512:1024], False, True, reads=[b_["Rsq"], Rcf], writes=[PSR[bi]])
                    TS("dve", b_["r"], ps, 1.0 / 256, EPS, ALU.mult, ALU.add, reads=[PSR[bi]], writes=[b_["Rr"]])
                    ACT(b_["r"], b_["r"], AF.Ln, reads=[b_["Rr"]], writes=[b_["Rr"]])
                    ACT(b_["r"], b_["r"], AF.Exp, reads=[b_["Rr"]], writes=[b_["Rr"]], scale=-0.5)
                    for j in range(2):
                        gc = small[:, SM["gmh"] + 2 * h + j:SM["gmh"] + 2 * h + j + 1]
                        ACT(gv[:, j, :], gv[:, j, :], AF.Copy, reads=[b_["Rg"], Rsmall], writes=[b_["Rg"]], scale=gc)
                        TT("dve", hv[:, j, :], hv[:, j, :], b_["r"], ALU.mult, reads=[b_["Rh"], b_["Rr"]], writes=[b_["Rh"]])
                        TT("dve", ov[:, j, :], hv[:, j, :], gv[:, j, :], ALU.mult, reads=[b_["Rh"], b_["Rg"]], writes=[b_["Ro"]])
                    DMA("sp", hmT_d[2 * h:2 * h + 2].rearrange("j p s -> p j s")[:, :, ts_], ov, reads=[b_["Ro"]], pwrites=[R["hmT"]])
            sc.barrier_all()
            if upto == "m2":
                return

            wk.reset()
            ab = [dict(q=[wk.alloc(S, BF16) for _ in range(2)], k=[wk.alloc(S, BF16) for _ in range(2)],
                       v=wk.alloc(TB * 256, BF16).rearrange("p (c v) -> p c v", v=256), R=Res()) for _ in range(2)]
            pT = [(wk.alloc(512, BF16), Res()) for _ in range(4)]
            on = [(wk.alloc(1024, F32), Res()) for _ in range(2)]
            rs_ = (wk.alloc(512, F32), Res())
            ot = (wk.alloc(1024, F32), Res())
            sq = (wk.alloc(1024, F32), Res())
            rr = (wk.alloc(512, F32), Res())
            ost = [(wk.alloc(1024, BF16), Res()) for _ in range(2)]
            scale = 128.0 ** -0.5
            cst = wk.alloc(S, F32)
            snt = wk.alloc(S, F32)
            Rcs = Res()
            DMA("sp", cst[0:32, :], cs_d[0], reads=[R["cs"]], pwrites=[Rcs])
            DMA("sp", snt[0:32, :], cs_d[1], reads=[R["cs"]], pwrites=[Rcs])
            xs = wk.alloc(S, BF16)
            Rxs = Res()
            rt1 = wk.alloc(1024, F32)
            rt2 = wk.alloc(1024, F32)
            Rrt12 = Res()
            RC = min(1024, S)
            uu = 0
            oc = 0
            def head_thunks(hh):
                aa = ab[hh % 2]
                th = []
                for c in range(2):
                    th.append(lambda c=c: DMA("sp", aa["q"][c], aq_d[2 * hh + c], reads=[R["aq"]], pwrites=[aa["R"]]))
                    th.append(lambda c=c: DMA("sp", aa["k"][c], ak_d[2 * hh + c], reads=[R["ak"]], pwrites=[aa["R"]]))
                for c0 in range(0, TB, 8):
                    th.append(lambda c0=c0: DMA("sp", aa["v"][:, c0:c0 + 8, :],
                                                av_d.rearrange("(c p) v -> p c v", p=128)[:, c0:c0 + 8, hh * 256:(hh + 1) * 256],
                                                reads=[R["av"]], pwrites=[aa["R"]]))
                for c in range(2):
                    for (buf, src_d, Rsrc) in ((aa["q"][c], aq_d[2 * hh + c], R["aq"]), (aa["k"][c], ak_d[2 * hh + c], R["ak"])):
                        def ld(src_d=src_d, Rsrc=Rsrc):
                            DMA("sp", xs[0:16, :], src_d[16:32, :], reads=[Rsrc], pwrites=[Rxs])
                            DMA("sp", xs[16:32, :], src_d[0:16, :], reads=[Rsrc], pwrites=[Rxs])
                        th.append(ld)
                        for r0_ in range(0, S, RC):
                            def rp(buf=buf, sl=slice(r0_, r0_ + RC)):
                                TT("dve", rt1[0:32, 0:RC], buf[0:32, sl], cst[0:32, sl], ALU.mult, reads=[aa["R"], Rcs], writes=[Rrt12])
                                TT("dve", rt2[0:32, 0:RC], xs[0:32, sl], snt[0:32, sl], ALU.mult, reads=[Rxs, Rcs], writes=[Rrt12])
                                TT("dve", buf[0:32, sl], rt1[0:32, 0:RC], rt2[0:32, 0:RC], ALU.add, reads=[Rrt12], writes=[aa["R"]])
                            th.append(rp)
                return th

            deferred = [None]
            for t_ in head_thunks(0):
                t_()
            for h in range(8):
                a_ = ab[h % 2]
                pend = head_thunks(h + 1) if h + 1 < 8 else []
                per_j = -(-len(pend) // TC) if pend else 0
                for j in range(TC):
                    for c in range(2):
                        bO, bO1, bS = (2, 3, 4) if c == 0 else (5, 6, 7)
                        psO = bank(bO)
                        psO1 = bank(bO1)
                        psSum = bank(bS)
                        nk = 4 * j + 4
                        units = []
                        for m in range(nk):
                            r0 = 128 * max(0, m - 4 * j)
                            units.append((m, r0))

                        def do_s(m, r0, idx):
                            bi = idx % 2
                            MM(bank(bi)[:, r0:512], a_["k"][c][:, m * 128:(m + 1) * 128], a_["q"][c][:, j * 512 + r0:(j + 1) * 512],
                               True, True, reads=[a_["R"]], writes=[PSR[bi]])
                            p_, Rp2 = pT[idx % 4]
                            ACT(p_[:, r0:512], bank(bi)[:, r0:512], AF.Exp, reads=[PSR[bi]], writes=[Rp2], scale=scale)
                            if m >= 4 * j:
                                TT("dve", p_[:, r0:r0 + 128], p_[:, r0:r0 + 128], tri_b, ALU.mult, reads=[Rp2, Rcb], writes=[Rp2])

                        def do_pv(m, r0, idx, first, last):
                            p_, Rp2 = pT[idx % 4]
                            MM(psO[:, r0:512], a_["v"][:, m, 0:128], p_[:, r0:512], first, last, reads=[a_["R"], Rp2], writes=[PSR[bO]])
                            MM(psO1[:, r0:512], a_["v"][:, m, 128:256], p_[:, r0:512], first, last, reads=[a_["R"], Rp2], writes=[PSR[bO1]])
                            MM(psSum[:, r0:512], ones_b, p_[:, r0:512], first, last, reads=[Rcb, Rp2], writes=[PSR[bS]])

                        LA = 2
                        for ui in range(min(LA, nk)):
                            do_s(units[ui][0], units[ui][1], uu + ui)
                        for ui in range(nk):
                            if ui + LA < nk:
                                do_s(units[ui + LA][0], units[ui + LA][1], uu + ui + LA)
                            do_pv(units[ui][0], units[ui][1], uu + ui, ui == 0, ui == nk - 1)
                            if ui == 0 and c == 0 and deferred[0] is not None:
                                deferred[0]()
                                deferred[0] = None
                        uu += nk
                        rt_, Rrt_ = rs_
                        sc.op("dve", lambda o=rt_, i_=psSum: nc.vector.reciprocal(out=o, in_=i_), reads=[PSR[bS]], writes=[Rrt_])
                        on_, Ron = on[c]
                        TT("dve", on_[:, 0:512], psO, rt_, ALU.mult, reads=[PSR[bO], Rrt_], writes=[Ron])
                        TT("dve", on_[:, 512:1024], psO1, rt_, ALU.mult, reads=[PSR[bO1], Rrt_], writes=[Ron])
                    o_, Ro_ = ot
                    STT(o_, on[1][0], nlam, on[0][0], ALU.mult, ALU.add, reads=[on[0][1], on[1][1], Rlam], writes=[Ro_])
                    s_, Rs_ = sq
                    ACT(s_, o_, AF.Square, reads=[Ro_], writes=[Rs_])
                    os2, Ros2 = ost[oc % 2]
                    oc += 1

                    def norm_tail(h=h, j=j, o_=o_, Ro_=Ro_, s_=s_, Rs_=Rs_, os2=os2, Ros2=Ros2):
                        psn = bank(7)
                        MM(psn, ones_f, s_[:, 0:512], True, False, reads=[Rs_, Rcf], writes=[PSR[7]])
                        MM(psn, ones_f, s_[:, 512:1024], False, True, reads=[Rs_, Rcf], writes=[PSR[7]])
                        r_, Rr_ = rr
                        TS("dve", r_, psn, 1.0 / 256, EPS, ALU.mult, ALU.add, reads=[PSR[7]], writes=[Rr_])
                        ACT(r_, r_, AF.Ln, reads=[Rr_], writes=[Rr_])
                        ACT(r_, r_, AF.Exp, reads=[Rr_], writes=[Rr_], scale=-0.5, bias=math.log(1.0 - lam_init))
                        for jj in range(2):
                            gs = small[:, SM["gsub"] + jj:SM["gsub"] + jj + 1]
                            STT(os2[:, jj * 512:(jj + 1) * 512], o_[:, jj * 512:(jj + 1) * 512], gs, r_, ALU.mult, ALU.mult,
                                reads=[Ro_, Rr_, Rsmall], writes=[Ros2])
                        DMA("sp", haT_d[2 * h:2 * h + 2].rearrange("j p s -> p j s")[:, :, j * 512:(j + 1) * 512],
                            os2.rearrange("p (j t) -> p j t", t=512), reads=[Ros2], pwrites=[R["haT"]])

                    deferred[0] = norm_tail
                    for t_ in pend[j * per_j:(j + 1) * per_j]:
                        t_()
            if deferred[0] is not None:
                deferred[0]()
                deferred[0] = None
            sc.barrier_all()
            if upto == "a":
                return

            wk.reset()
            ygm = [(wk.alloc(2048, BF16), Res()) for _ in range(2)]
            yga = [(wk.alloc(2048, BF16), Res()) for _ in range(2)]
            ytm = [(wk.alloc(512, F32), Res()) for _ in range(2)]
            ytm2 = [(wk.alloc(512, F32), Res()) for _ in range(2)]
            ycur = [None, None, 0]

            def epi_y(slab, t0, pss, dst, Rd):
                ST = min(2048, S)
                if t0 % ST == 0:
                    k = ycur[2] % 2
                    ycur[2] += 1
                    ycur[0] = ygm[k]
                    ycur[1] = yga[k]
                    DMA("act", ycur[0][0][:, 0:ST], sgm_d[slab][:, t0:t0 + ST], reads=[R["sgm"]], writes=[ycur[0][1]])
                    DMA("act", ycur[1][0][:, 0:ST], sga_d[slab][:, t0:t0 + ST], reads=[R["sga"]], writes=[ycur[1][1]])
                off = t0 % ST
                (pm_, Rpm), (pa_, Rpa) = pss
                k = (t0 // 512) % 2
                a1, Ra1 = ytm[k]
                a2, Ra2 = ytm2[k]
                TT("dve", a1, pm_, ycur[0][0][:, off:off + 512], ALU.mult, reads=[Rpm, ycur[0][1]], writes=[Ra1])
                TT("dve", a2, pa_, ycur[1][0][:, off:off + 512], ALU.mult, reads=[Rpa, ycur[1][1]], writes=[Ra2])
                TT("dve", dst, a1, a2, ALU.add, reads=[Ra1, Ra2], writes=[Rd])

            gemm_fm([(hmT_d, R["hmT"], 8, lambda s: pm_in[l, s]), (haT_d, R["haT"], 16, lambda s: pa_in[l, s])],
                    range(16), 128, epi_y, lambda s: (yT_d[s], R["yT"]), BF16, 2048, wbufs=2)
            sc.barrier_all()
            if upto == "y":
                return

            wk.reset()
            xr = [(wk.alloc(512, F32), Res()) for _ in range(3)]
            octr = [0]

            def mk_epi_res(xs_d, Rxs, xd_d, Rxd, WC):
                def epi(slab, tb, ps, Rp_):
                    octr[0] += 1
                    xt_, Rxt_ = xr[octr[0] % 3]
                    rows = slice(tb * 128, (tb + 1) * 128)
                    cols = slice(slab * WC, (slab + 1) * WC)
                    DMA("act", xt_[:, 0:WC], xs_d[rows, cols], reads=[Rxs], writes=[Rxt_])
                    TT("dve", xt_[:, 0:WC], ps, xt_[:, 0:WC], ALU.add, reads=[Rp_, Rxt_], writes=[Rxt_])
                    DMA("sp", xd_d[rows, cols], xt_[:, 0:WC], reads=[Rxt_], pwrites=[Rxd])
                return epi

            gemm_tm(yT_d, R["yT"], KCD, lambda s: wo_in[l, s], 4, 512, mk_epi_res(x_d, Rx, xn1_d, Rxn1, 512), 2048)
            sc.barrier_all()
            if upto == "o":
                return

            norm_phase(xn1_d, Rxn1, SM["gffn"])
            sc.barrier_all()
            wk.reset()
            fs = [(wk.alloc(512, F32), Res()) for _ in range(2)]
            fctr = [0]

            def epi_ffn(slab, t0, pss, dst, Rd):
                (pg, Rpg), (pu, Rpu) = pss
                fctr[0] += 1
                s_, Rs_ = fs[fctr[0] % 2]
                ACT(s_, pg, AF.Silu, reads=[Rpg], writes=[Rs_])
                TT("dve", dst, pu, s_, ALU.mult, reads=[Rpu, Rs_], writes=[Rd])

            gemm_fm([(hT_d, R["hT"], KCD, lambda s: wga_in[l, s]), (hT_d, R["hT"], KCD, lambda s: wup_in[l, s])],
                    range(44), 128, epi_ffn, lambda s: (actT_d[s], R["actT"]), BF16, 2048, wbufs=2)
            sc.barrier_all()
            if upto == "f1":
                return
            wk.reset()
            xr[:] = [(wk.alloc(512, F32), Res()) for _ in range(3)]
            gemm_tm(actT_d, R["actT"], KCF, lambda s: wdn_in[l, s], 8, 256, mk_epi_res(xn1_d, Rxn1, xn2_d, Rxn2, 256), 1024)
            sc.barrier_all()

        cur, Rcur = x_in, R["xin"]
        done = False
        for l in range(depth):
            layer(l, cur, Rcur, xa_d, R["xa"], xb_d, R["xb"])
            cur, Rcur = xb_d, R["xb"]
            if upto is not None:
                done = True
                break
        if not done:
            wk.reset()
            gf_ = wk.alloc(D, F32)
            Rgf = Res()
            DMA("sp", gf_, gfin_in, writes=[Rgf])
            xt = [(wk.alloc(D, F32), Res()) for _ in range(2)]
            junk = (wk.alloc(D, BF16), Res())
            yo = [(wk.alloc(D, F32), Res()) for _ in range(2)]
            st = wk.alloc(4 * TB, F32)
            Rst = [Res(), Res()]
            for tb in range(TB):
                s2 = tb % 2
                x_, Rx_ = xt[s2]
                DMA("sp", x_, cur[tb * 128:(tb + 1) * 128, :], reads=[Rcur], writes=[Rx_])
                ss = st[:, 4 * tb:4 * tb + 1]
                ms = st[:, 4 * tb + 1:4 * tb + 2]
                rstd = st[:, 4 * tb + 2:4 * tb + 3]
                ACT(junk[0], x_, AF.Square, reads=[Rx_], writes=[Rst[s2]], accum_out=ss)
                TS("dve", ms, ss, 1.0 / D, EPS, ALU.mult, ALU.add, reads=[Rst[s2]], writes=[Rst[s2]])
                ACT(ms, ms, AF.Ln, reads=[Rst[s2]], writes=[Rst[s2]])
                ACT(rstd, ms, AF.Exp, reads=[Rst[s2]], writes=[Rst[s2]], scale=-0.5)
                y_, Ry_ = yo[s2]
                STT(y_, x_, rstd, gf_, ALU.mult, ALU.mult, reads=[Rx_, Rst[s2], Rgf], writes=[Ry_])
                DMA("sp", out_d[tb * 128:(tb + 1) * 128, :], y_, reads=[Ry_], pwrites=[R["out"]])

        block = es.enter_context(nc.Block())
        sc.emit(block)
    return nc, dbg_outs


def fm_slabs(w, ncol):
    L, K, N = w.shape
    return np.ascontiguousarray(w.reshape(L, K // 128, 128, N // ncol, ncol).transpose(0, 3, 2, 1, 4))


def host_consts():
    cf = np.zeros((128, 1024), np.float32)
    cf[:, 0:128] = 1.0
    for h in range(4):
        cf[h, 128 + h * 128:128 + (h + 1) * 128] = 1.0
    half = 16
    inv = np.power(np.float32(ROPE_THETA), -np.arange(half, dtype=np.float32) * np.float32(2.0 / 32)).astype(np.float32)
    cf[0:16, 640] = inv / np.float32(2 * math.pi)
    cf[16:32, 640] = inv / np.float32(2 * math.pi)
    cf[:, 641] = -0.5
    cf[:, 642] = 0.25
    cf[0:4, 768:896] = 1.0
    cb = np.zeros((128, 512), np.float32)
    cb[:, 0:128] = np.eye(128)
    cb[:, 128:256] = 1.0
    cb[:, 256:384] = (np.arange(128)[None, :] >= np.arange(128)[:, None])
    rt = np.zeros((32, 32), np.float32)
    for m in range(32):
        rt[(m + 16) % 32, m] = 1.0
    cb[0:32, 384:416] = rt
    return cf, cb.astype(ml_dtypes.bfloat16)


def host_layout(inp, depth):
    w_in = np.asarray(inp["w_in"])[:depth]
    o = {}
    c = lambda a, b: w_in[:, :, a:b]
    fam = [c(0, 1024), c(2048, 3072), c(3080, 5128), c(5128, 7176), c(9224, 11272), c(11272, 13320)]
    o["wfm"] = np.concatenate([fm_slabs(f, 128) for f in fam], axis=1)
    wg = np.zeros((depth, 1, 128, KCD, 128), np.float32)
    wg[:, 0, :, :, 0:4] = fm_slabs(c(3072, 3076), 4)[:, 0]
    wg[:, 0, :, :, 32:36] = fm_slabs(c(3076, 3080), 4)[:, 0]
    o["wg"] = wg
    o["wtm"] = np.concatenate([fm_slabs(c(1024, 2048), 512), fm_slabs(c(7176, 9224), 512)], axis=1)
    o["pm"] = fm_slabs(np.asarray(inp["p_m"])[:depth], 128)
    o["pa"] = fm_slabs(np.asarray(inp["p_a"])[:depth], 128)
    o["wo"] = fm_slabs(np.asarray(inp["w_out"])[:depth], 512)
    o["wga"] = fm_slabs(np.asarray(inp["w_gate"])[:depth], 128)
    o["wup"] = fm_slabs(np.asarray(inp["w_up"])[:depth], 128)
    o["wdn"] = fm_slabs(np.asarray(inp["w_down"])[:depth], 256)
    small = np.zeros((depth, 128, NSMALL), np.float32)
    g = lambda k: np.asarray(inp[k])[:depth]
    small[:, :, 0:16] = g("g_mix").reshape(depth, 16, 128).transpose(0, 2, 1)
    small[:, :, 16:32] = g("g_ffn").reshape(depth, 16, 128).transpose(0, 2, 1)
    small[:, :, 32:64] = g("conv_w").reshape(depth, 4, 8, 128).transpose(0, 3, 2, 1).reshape(depth, 128, 32)
    small[:, :, 64:72] = g("conv_b").reshape(depth, 8, 128).transpose(0, 2, 1)
    small[:, :, 72:80] = g("g_mhead").reshape(depth, 8, 128).transpose(0, 2, 1)
    small[:, :, 80] = g("lambda_q1")
    small[:, :, 81] = g("lambda_k1")
    small[:, :, 82] = g("lambda_q2")
    small[:, :, 83] = g("lambda_k2")
    small[:, :, 84:86] = g("g_sub").reshape(depth, 2, 128).transpose(0, 2, 1)
    small[:, 0:4, 86] = g("i_bias")
    small[:, 0:4, 87] = g("f_bias")
    o["small"] = small
    o["gfin"] = np.ascontiguousarray(np.broadcast_to(np.asarray(inp["g_final"])[None, :], (128, D)))
    cf, cb = host_consts()
    o["cf"] = cf
    o["cb"] = cb
    return o


_CACHE = {}


def kernel(**inputs):
    x = np.asarray(inputs["x"])
    pos = np.asarray(inputs["positions"]).astype(np.int32)
    B, S, _ = x.shape
    shared = host_layout(inputs, DEPTH)
    key = (S, DEPTH)
    if key not in _CACHE:
        _CACHE[key] = build(S, DEPTH)[0]
    nc = _CACHE[key]
    in_maps = []
    for b in range(B):
        m = dict(shared)
        m["x"] = np.ascontiguousarray(x[b])
        m["pos"] = np.ascontiguousarray(pos[b:b + 1])
        in_maps.append(m)
    res = run_bass_kernel_spmd(nc, in_maps, core_ids=list(range(B)))
    return np.stack([np.asarray(r["out"]) for r in res.results], axis=0).astype(np.float32)
```
